# Optimizing a Trainium2 kernel written in Bass

```python
import math
import jax, jax.numpy as jnp
from jax import lax
import numpy as np

D_MODEL = 2048
BATCH = 8
SEQ = 2048
DEPTH = 1

NORM_EPS = 1e-6
HG_HEADS = 16
HG_HEAD_DIM = D_MODEL // HG_HEADS
HG_WIDTH = HG_HEADS * HG_HEAD_DIM
HG_CHUNK = 32
ATT_GROUPS = ((128, 1), (512, 4), (2048, 16))
ATT_HEADS_PER_GROUP = 4
ATT_HEAD_DIM = 128
ATT_HEADS = len(ATT_GROUPS) * ATT_HEADS_PER_GROUP
ATT_WIDTH = ATT_HEADS * ATT_HEAD_DIM
ATT_OUT_WIDTH = ATT_HEADS_PER_GROUP * ATT_HEAD_DIM
ATT_BLOCK = 128
REL_BUCKETS = 32
REL_MAX_DIST = 2048
IN_SPLITS = (HG_WIDTH, HG_WIDTH, HG_WIDTH, HG_WIDTH, ATT_WIDTH, ATT_WIDTH, ATT_WIDTH, D_MODEL, D_MODEL)
IN_WIDTH = sum(IN_SPLITS)
MOE_GROUPS = 8
MOE_EXPERTS_PER_GROUP = 8
MOE_EXPERTS = MOE_GROUPS * MOE_EXPERTS_PER_GROUP
MOE_TOPK = 2
MOE_FF = 512
MOE_BLOCK = 128

kernel_name = "hybrid_hgrn2_dilated_attn_hiermoe"


def rms_norm(x, gain):
    xf = x.astype(jnp.float32)
    y = xf * lax.rsqrt(jnp.mean(xf * xf, axis=-1, keepdims=True) + NORM_EPS)
    return (y * gain.astype(jnp.float32)).astype(x.dtype)


def hgrn2(q, f_pre, i_in, g, lower_bound, norm_gain):
    B, S, _ = q.shape
    H, E, C = HG_HEADS, HG_HEAD_DIM, HG_CHUNK
    nc = S // C
    f = lower_bound + (1.0 - lower_bound) * jax.nn.sigmoid(f_pre.astype(jnp.float32))
    log_f = jnp.log(f)
    k = 1.0 - f

    def chunks(t):
        return t.astype(jnp.float32).reshape(B, nc, C, H, E).transpose(1, 0, 3, 2, 4)

    causal = jnp.tril(jnp.ones((C, C), dtype=bool))

    def step(state, inp):
        qc, kc, vc, lc = inp
        b = jnp.cumsum(lc, axis=2)
        o_inter = jnp.einsum("bhtk,bhkv->bhtv", qc * jnp.exp(b), state)
        diff = b[:, :, :, None, :] - b[:, :, None, :, :]
        decay = jnp.exp(jnp.where(causal[:, :, None], diff, -jnp.inf))
        scores = jnp.einsum("bhtk,bhsk,bhtsk->bhts", qc, kc, decay)
        o_intra = jnp.einsum("bhts,bhsv->bhtv", scores, vc)
        b_last = b[:, :, -1]
        state = jnp.exp(b_last)[..., None] * state + jnp.einsum(
            "bhsk,bhsv->bhkv", kc * jnp.exp(b_last[:, :, None] - b), vc)
        return state, o_inter + o_intra

    s0 = jnp.zeros((B, H, E, E), jnp.float32)
    _, o = lax.scan(step, s0, (chunks(q), chunks(k), chunks(i_in), chunks(log_f)))
    o = o.transpose(1, 0, 3, 2, 4).reshape(B, S, H, E)
    o = o * lax.rsqrt(jnp.mean(o * o, axis=-1, keepdims=True) + NORM_EPS)
    o = o.reshape(B, S, H * E) * norm_gain.astype(jnp.float32) * jax.nn.sigmoid(g.astype(jnp.float32))
    return o.astype(q.dtype)


def t5_bucket(dist):
    exact = REL_BUCKETS // 2
    d_f = jnp.maximum(dist, 1).astype(jnp.float32)
    log_b = exact + (jnp.log(d_f / exact) / math.log(REL_MAX_DIST / exact)
                     * (REL_BUCKETS - exact)).astype(jnp.int32)
    return jnp.where(dist < exact, dist, jnp.minimum(log_b, REL_BUCKETS - 1))


def dilated_group_attention(q, k, v, rel_bias, window, dilation):
    B, S, H, E = q.shape
    n_back = window // dilation
    L = S // dilation
    blk = min(ATT_BLOCK, L)
    nb = -(-L // blk)
    Lp = nb * blk

    def to_blocks(t):
        t = t.astype(jnp.float32).reshape(B, L, dilation, H, E).transpose(0, 2, 3, 1, 4)
        t = jnp.pad(t, ((0, 0), (0, 0), (0, 0), (0, Lp - L), (0, 0)))
        return t.reshape(B, dilation, H, nb, blk, E)

    def with_prev(t):
        prev = jnp.pad(t[:, :, :, :-1], ((0, 0), (0, 0), (0, 0), (1, 0), (0, 0), (0, 0)))
        return jnp.concatenate([prev, t], axis=4)

    qb = to_blocks(q)
    kc = with_prev(to_blocks(k))
    vc = with_prev(to_blocks(v))
    i = jnp.arange(blk)[:, None]
    c = jnp.arange(2 * blk)[None, :]
    delta = i + blk - c
    n = jnp.arange(nb)[:, None, None]
    valid = (delta >= 0) & (delta <= n_back) & (n * blk + c - blk >= 0)
    bias = rel_bias[t5_bucket(jnp.maximum(delta, 0) * dilation)]
    bias = jnp.transpose(bias, (2, 0, 1)).astype(jnp.float32)[:, None]
    logits = jnp.einsum("brhnqe,brhnke->brhnqk", qb, kc) * (E ** -0.5) + bias
    logits = jnp.where(valid, logits, -1e30)
    m = jnp.max(logits, axis=-1, keepdims=True)
    p = jnp.exp(logits - m)
    den = jnp.sum(p, axis=-1)
    o = jnp.einsum("brhnqk,brhnke->brhnqe", p, vc) / den[..., None]
    lse = m[..., 0] + jnp.log(den)
    o = o.reshape(B, dilation, H, Lp, E)[:, :, :, :L].transpose(0, 3, 1, 2, 4).reshape(B, S, H, E)
    lse = lse.reshape(B, dilation, H, Lp)[..., :L].transpose(0, 3, 1, 2).reshape(B, S, H)
    return o, lse


def dilated_attention(q, k, v, rel_bias):
    B, S = q.shape[:2]
    outs, lses = [], []
    for gi, (window, dilation) in enumerate(ATT_GROUPS):
        hs = slice(gi * ATT_HEADS_PER_GROUP, (gi + 1) * ATT_HEADS_PER_GROUP)
        o, lse = dilated_group_attention(q[:, :, hs], k[:, :, hs], v[:, :, hs],
                                         rel_bias[:, hs], window, dilation)
        outs.append(o)
        lses.append(lse)
    alpha = jax.nn.softmax(jnp.stack(lses, axis=0), axis=0)
    o = jnp.einsum("gbsh,gbshe->bshe", alpha, jnp.stack(outs, axis=0))
    return o.reshape(B, S, ATT_OUT_WIDTH)


def hybrid_mixer(h, w_in, lower_bound, hg_norm_gain, rel_bias, w_branch_a, w_branch_b, w_out):
    B, S, _ = h.shape
    proj = h @ w_in
    split_points = np.cumsum(IN_SPLITS)[:-1].tolist()
    q_a, f_a, i_a, g_a, q_b, k_b, v_b, gate_a, gate_b = jnp.split(proj, split_points, axis=-1)
    y_a = hgrn2(q_a, f_a, i_a, g_a, lower_bound, hg_norm_gain)
    heads = lambda t: t.reshape(B, S, ATT_HEADS, ATT_HEAD_DIM)
    y_b = dilated_attention(heads(q_b), heads(k_b), heads(v_b), rel_bias).astype(h.dtype)
    merged = jax.nn.sigmoid(gate_a) * (y_a @ w_branch_a) + jax.nn.sigmoid(gate_b) * (y_b @ w_branch_b)
    return merged @ w_out


def hierarchical_moe(h, w_rg, b_rg, w_re, b_re, w_gate, w_up, w_down):
    B, S, D = h.shape
    T = B * S
    hf = h.reshape(T, D)
    h32 = hf.astype(jnp.float32)
    lg_group = h32 @ w_rg.astype(jnp.float32) + b_rg.astype(jnp.float32)
    p_group = jax.nn.softmax(lg_group, axis=-1)
    g_sel = jnp.argmax(lg_group, axis=-1).astype(jnp.int32)
    pg = jnp.take_along_axis(p_group, g_sel[:, None], axis=-1)
    lg_exp = (h32 @ w_re.astype(jnp.float32) + b_re.astype(jnp.float32)).reshape(
        T, MOE_GROUPS, MOE_EXPERTS_PER_GROUP)
    lg_in = jnp.take_along_axis(lg_exp, g_sel[:, None, None], axis=1)[:, 0]
    top_v, top_i = lax.top_k(lg_in, MOE_TOPK)
    gate = pg * jax.nn.softmax(top_v, axis=-1)
    expert = g_sel[:, None] * MOE_EXPERTS_PER_GROUP + top_i

    A = T * MOE_TOPK
    eid = expert.reshape(A)
    tok = jnp.repeat(jnp.arange(T, dtype=jnp.int32), MOE_TOPK)
    wts = gate.reshape(A)
    order = jnp.argsort(eid)
    e_s, tok_s, w_s = eid[order], tok[order], wts[order]
    counts = jnp.bincount(eid, length=MOE_EXPERTS)
    offs = jnp.cumsum(counts) - counts
    pcounts = (counts + MOE_BLOCK - 1) // MOE_BLOCK * MOE_BLOCK
    pends = jnp.cumsum(pcounts)
    poffs = pends - pcounts
    dest = poffs[e_s] + (jnp.arange(A) - offs[e_s])
    n_blocks = -(-A // MOE_BLOCK) + MOE_EXPERTS
    P = n_blocks * MOE_BLOCK
    buf_tok = jnp.full((P,), T, jnp.int32).at[dest].set(tok_s)
    buf_w = jnp.zeros((P,), jnp.float32).at[dest].set(w_s)
    block_expert = jnp.minimum(jnp.searchsorted(pends, jnp.arange(n_blocks) * MOE_BLOCK, side="right"),
                               MOE_EXPERTS - 1).astype(jnp.int32)
    h_pad = jnp.concatenate([hf, jnp.zeros((1, D), hf.dtype)], axis=0)

    def run_block(args):
        e, tk = args
        xb = h_pad[tk]
        u = jax.nn.silu(xb @ w_gate[e]) * (xb @ w_up[e])
        return u @ w_down[e]

    y_blocks = lax.map(run_block, (block_expert, buf_tok.reshape(n_blocks, MOE_BLOCK)))
    y_rows = y_blocks.reshape(P, D) * buf_w[:, None].astype(y_blocks.dtype)
    y = jnp.zeros((T + 1, D), y_blocks.dtype).at[buf_tok].add(y_rows)
    return y[:T].reshape(B, S, D)


def setup_inputs(seed: int = 0) -> dict:
    key = jax.random.key(seed)
    ks = jax.random.split(key, 18)
    f32 = jnp.float32
    D, F, G, E = D_MODEL, MOE_FF, MOE_GROUPS, MOE_EXPERTS

    def nrm(k, shape, scale):
        return jax.random.normal(k, shape, f32) * scale

    return {
        "x": nrm(ks[0], (BATCH, SEQ, D), 1.0),
        "norm1_gain": 1.0 + nrm(ks[1], (DEPTH, D), 0.02),
        "w_in": nrm(ks[2], (DEPTH, D, IN_WIDTH), D ** -0.5),
        "hg_lb_logits": nrm(ks[3], (DEPTH + 1, HG_WIDTH), 0.5),
        "hg_norm_gain": 1.0 + nrm(ks[4], (DEPTH, HG_WIDTH), 0.02),
        "rel_bias": nrm(ks[5], (REL_BUCKETS, ATT_HEADS), 0.5),
        "w_branch_a": nrm(ks[6], (DEPTH, HG_WIDTH, D), HG_WIDTH ** -0.5),
        "w_branch_b": nrm(ks[7], (DEPTH, ATT_OUT_WIDTH, D), ATT_OUT_WIDTH ** -0.5),
        "w_out": nrm(ks[8], (DEPTH, D, D), D ** -0.5),
        "norm2_gain": 1.0 + nrm(ks[9], (DEPTH, D), 0.02),
        "w_router_group": nrm(ks[10], (DEPTH, D, G), D ** -0.5),
        "b_router_group": nrm(ks[11], (DEPTH, G), 0.01),
        "w_router_expert": nrm(ks[12], (DEPTH, D, E), D ** -0.5),
        "b_router_expert": nrm(ks[13], (DEPTH, E), 0.01),
        "w_exp_gate": nrm(ks[14], (DEPTH, E, D, F), D ** -0.5),
        "w_exp_up": nrm(ks[15], (DEPTH, E, D, F), D ** -0.5),
        "w_exp_down": nrm(ks[16], (DEPTH, E, F, D), F ** -0.5),
        "final_norm_gain": 1.0 + nrm(ks[17], (D,), 0.02),
    }


def reference(x, norm1_gain, w_in, hg_lb_logits, hg_norm_gain, rel_bias, w_branch_a, w_branch_b,
              w_out, norm2_gain, w_router_group, b_router_group, w_router_expert, b_router_expert,
              w_exp_gate, w_exp_up, w_exp_down, final_norm_gain):
    lower_bounds = jnp.cumsum(jax.nn.softmax(hg_lb_logits.astype(jnp.float32), axis=0), axis=0)
    for layer in range(DEPTH):
        h = rms_norm(x, norm1_gain[layer])
        x = x + hybrid_mixer(h, w_in[layer], lower_bounds[layer], hg_norm_gain[layer], rel_bias,
                             w_branch_a[layer], w_branch_b[layer], w_out[layer])
        h = rms_norm(x, norm2_gain[layer])
        x = x + hierarchical_moe(h, w_router_group[layer], b_router_group[layer],
                                 w_router_expert[layer], b_router_expert[layer],
                                 w_exp_gate[layer], w_exp_up[layer], w_exp_down[layer])
    return rms_norm(x, final_norm_gain)
```

```python
import numpy as np
import concourse.bass as bass
import concourse.mybir as mybir
from concourse.bass_utils import run_bass_kernel_spmd

F32 = mybir.dt.float32
BF16 = mybir.dt.bfloat16
I32 = mybir.dt.int32
AF = mybir.ActivationFunctionType
ALU = mybir.AluOpType
AX = mybir.AxisListType

D = 2048
S = 2048
NT = S // 128
KC = D // 128
INW = 16896
EPS = 1e-6
NEG = -30000.0
ENGS = ("pe", "act", "dve", "pool", "sp")


class Buf:
    __slots__ = ("name", "w", "r")

    def __init__(self, name):
        self.name = name
        self.w = None
        self.r = {}


class Sched:
    def __init__(self, nc):
        self.nc = nc
        self.q = {e: [] for e in ENGS}
        self.cnt = {e: 0 for e in ENGS}
        self.seen = {e: {} for e in ENGS}
        self.dma_cnt = {}
        self.sems = {}
        self.ndma = 0

    def _deps(self, eng, reads, writes):
        deps = {}
        def add(ev):
            if ev is None:
                return
            k, v = ev
            if deps.get(k, 0) < v:
                deps[k] = v
        for b in reads:
            add(b.w)
        for b in writes:
            add(b.w)
            for k, v in b.r.items():
                add((k, v))
        for k, v in deps.items():
            if k == "pe" and eng == "pe":
                continue
            if self.seen[eng].get(k, 0) >= v:
                continue
            self.seen[eng][k] = v
            self.q[eng].append(("wait", k, v))

    def _mark(self, ev, reads, writes):
        for b in writes:
            b.w = ev
            b.r = {}
        for b in reads:
            if b.r.get(ev[0], 0) < ev[1]:
                b.r[ev[0]] = ev[1]

    def op(self, eng, fn, reads=(), writes=()):
        self._deps(eng, reads, writes)
        self.cnt[eng] += 1
        ev = (eng, self.cnt[eng])
        self.q[eng].append(("op", fn, eng, 1))
        self._mark(ev, reads, writes)
        return ev

    def dma(self, eng, fn, sem, reads=(), writes=()):
        self._deps(eng, reads, writes)
        self.dma_cnt[sem] = self.dma_cnt.get(sem, 0) + 16
        ev = (sem, self.dma_cnt[sem])
        self.q[eng].append(("op", fn, sem, 16))
        self._mark(ev, reads, writes)
        self.ndma += 1
        return ev

    def barrier(self):
        keys = list(ENGS) + sorted(self.dma_cnt.keys())
        for eng in ENGS:
            for k in keys:
                if k == eng:
                    continue
                v = self.cnt[k] if k in self.cnt else self.dma_cnt[k]
                if v > self.seen[eng].get(k, 0):
                    self.seen[eng][k] = v
                    self.q[eng].append(("wait", k, v))

    def wait_all(self, eng, bufs):
        self._deps(eng, bufs, ())

    def sem_names(self):
        return list(ENGS) + sorted(self.dma_cnt.keys())

    def emit(self, block):
        nc = self.nc
        sems = self.sems

        def run(engname):
            def body(e):
                for it in self.q[engname]:
                    if it[0] == "wait":
                        e.wait_ge(sems[it[1]], it[2])
                    else:
                        ins = it[1](e)
                        ins.then_inc(sems[it[2]], it[3])
            return body
        block.tensor(run("pe"))
        block.scalar(run("act"))
        block.vector(run("dve"))
        block.gpsimd(run("pool"))
        block.sync(run("sp"))


def _t5_bucket_np(dist):
    dist = np.asarray(dist, dtype=np.int64)
    exact = 16
    d_f = np.maximum(dist, 1).astype(np.float32)
    lb = exact + (np.log(d_f / np.float32(exact)) / np.float32(np.log(2048 / exact)) * np.float32(32 - exact)).astype(np.int32)
    return np.where(dist < exact, dist, np.minimum(lb, 31))


def _att_onehot():
    groups = ((128, 1), (512, 4), (2048, 16))
    JW = (256, 640, 2048)
    cols = []
    for (win, dil), J in zip(groups, JW):
        W = J + 127
        dist = np.arange(W) - 127
        valid = (dist >= 0) & (dist % dil == 0) & (dist <= win)
        b = _t5_bucket_np(np.maximum(dist, 0))
        oh = np.zeros((33, W), np.float32)
        oh[b[valid], np.nonzero(valid)[0]] = 1.0
        oh[32, ~valid] = 1.0
        cols.append(oh)
    return np.ascontiguousarray(np.concatenate(cols, axis=1))
def build_nc(stage="full"):
    nc = bass.Bass("TRN2", target_bir_lowering=False)
    dr = lambda name, shape, dt, kind: nc.dram_tensor(name, shape, dt, kind=kind).ap()
    x = dr("x", [S, D], F32, "ExternalInput")
    norm1_gain = dr("norm1_gain", [D], F32, "ExternalInput")
    w_in = dr("w_in", [D, INW], F32, "ExternalInput")
    hg_lb_logits = dr("hg_lb_logits", [2, D], F32, "ExternalInput")
    hg_norm_gain = dr("hg_norm_gain", [D], F32, "ExternalInput")
    rel_bias = dr("rel_bias", [32, 12], F32, "ExternalInput")
    att_oh = dr("att_oh", [33, 3325], F32, "ExternalInput")
    w_branch_a = dr("w_branch_a", [D, D], F32, "ExternalInput")
    w_branch_b = dr("w_branch_b", [512, D], F32, "ExternalInput")
    w_out = dr("w_out", [D, D], F32, "ExternalInput")
    norm2_gain = dr("norm2_gain", [D], F32, "ExternalInput")
    final_norm_gain = dr("final_norm_gain", [D], F32, "ExternalInput")
    w_router_group = dr("w_router_group", [D, 8], F32, "ExternalInput")
    b_router_group = dr("b_router_group", [1, 8], F32, "ExternalInput")
    w_router_expert = dr("w_router_expert", [D, 64], F32, "ExternalInput")
    b_router_expert = dr("b_router_expert", [1, 64], F32, "ExternalInput")
    w_exp_gate = dr("w_exp_gate", [64, D, 512], F32, "ExternalInput")
    w_exp_up = dr("w_exp_up", [64, D, 512], F32, "ExternalInput")
    w_exp_down = dr("w_exp_down", [64, 512, D], F32, "ExternalInput")
    out = dr("out", [S, D], F32, "ExternalOutput")

    def scratch(name, shape, dt):
        kind = "ExternalOutput" if stage == "dbg_" + name else "Internal"
        return dr(name, shape, dt, kind)
    sg_dram = scratch("sg", [2 * D, S], BF16)
    ya_dram = scratch("ya", [D, S], BF16)

    yb_dram = scratch("yb", [512, S], BF16)
    zrow_dram = scratch("zrow", [12 * 128 * 2176], BF16)
    x2_dram = scratch("x2", [S, D], F32)
    h2_dram = scratch("h2", [S + 1, D], BF16)
    ye_dram = scratch("ye", [8192 + 128, D], BF16)
    tab_dram = scratch("tab", [8320], I32)
    wbf_parts = [scratch(f"wbf{k}", [16, 3, 2048 * 512], BF16) for k in range(4)]

    class _Wbf:
        def __getitem__(self, key):
            e_, m_ = key
            return wbf_parts[e_ // 16][e_ % 16, m_]
    wbf_dram = _Wbf()
    sc = Sched(nc)
    ctxs = []

    def sb(name, shape, dt):
        cm = nc.sbuf_tensor(name, shape, dt)
        t = cm.__enter__()
        ctxs.append(cm)
        return t

    def ps(name, shape, dt):
        cm = nc.psum_tensor(name, shape, dt)
        t = cm.__enter__()
        ctxs.append(cm)
        return t

    ARENA = 53100
    arena = sb("arena", [128, ARENA], F32)
    top = [0]

    def alloc(words, dt=F32, shape=None):
        o = top[0]
        top[0] += words
        assert top[0] <= ARENA, ("arena overflow", top[0])
        a = arena[:, o:o + words]
        if dt != F32:
            a = a.bitcast(dt)
        if shape is not None and len(shape) == 3:
            a = a.rearrange("p (a b) -> p a b", b=shape[2])
        return a

    pbank = [ps(f"pb{i}", [128, 512], F32) for i in range(8)]
    B_pb = [Buf(f"pb{i}") for i in range(8)]

    ident_f = alloc(128)
    ident_b = alloc(64, BF16)
    mask_ut = alloc(128)
    B_const = Buf("const")
    sc.op("pool", lambda e: e.memset(ident_f, 1.0), writes=[B_const])
    sc.op("pool", lambda e: e.affine_select(out=ident_f, in_=ident_f, pattern=[[-1, 128]],
                                            compare_op=ALU.is_equal, fill=0.0, base=0, channel_multiplier=1),
          reads=[B_const], writes=[B_const])
    sc.op("dve", lambda e: e.tensor_copy(out=ident_b, in_=ident_f), reads=[B_const], writes=[B_const])
    sc.op("pool", lambda e: e.memset(mask_ut, 1.0), writes=[B_const])
    sc.op("pool", lambda e: e.affine_select(out=mask_ut, in_=mask_ut, pattern=[[1, 128]],
                                            compare_op=ALU.is_ge, fill=0.0, base=0, channel_multiplier=-1),
          reads=[B_const], writes=[B_const])
    g1 = alloc(KC)
    epsc = alloc(1)
    zeros512 = alloc(512)
    lbl = alloc(32, F32, [128, 2, 16])
    lbT = alloc(16)
    omlb = alloc(16)
    nomlb = alloc(16)
    gnT = alloc(16)
    sc.dma("sp", lambda e: e.dma_start(out=g1, in_=norm1_gain.rearrange("(kc p) -> p kc", p=128),
                                       allow_slow_non_contiguous=True), "d_const", writes=[B_const])
    sc.dma("sp", lambda e: e.dma_start(out=gnT, in_=hg_norm_gain.rearrange("(kc p) -> p kc", p=128),
                                       allow_slow_non_contiguous=True), "d_const", writes=[B_const])
    sc.dma("sp", lambda e: e.dma_start(out=lbl, in_=hg_lb_logits.rearrange("r (h p) -> p r h", p=128),
                                       allow_slow_non_contiguous=True), "d_const", writes=[B_const])
    sc.op("dve", lambda e: e.memset(epsc, EPS), writes=[B_const])
    sc.op("dve", lambda e: e.memset(zeros512, 0.0), writes=[B_const])
    sc.op("dve", lambda e: e.tensor_tensor(out=lbT, in0=lbl[:, 0, :], in1=lbl[:, 1, :], op=ALU.subtract),
          reads=[B_const], writes=[B_const])
    sc.op("act", lambda e: e.activation(out=omlb, in_=lbT, func=AF.Sigmoid, scale=-1.0), reads=[B_const], writes=[B_const])
    sc.op("act", lambda e: e.activation(out=lbT, in_=lbT, func=AF.Sigmoid), reads=[B_const], writes=[B_const])
    sc.op("dve", lambda e: e.tensor_scalar(out=nomlb, in0=omlb, scalar1=-1.0, scalar2=None, op0=ALU.mult),
          reads=[B_const], writes=[B_const])

    hT = alloc(16384, BF16, [128, KC, S])
    B_hT = [Buf(f"hT{i}") for i in range(NT)]
    NSLAB = 3
    slab_base = top[0]
    slab = [alloc(4096, BF16, [128, KC, 512]) for i in range(NSLAB)]
    B_slab = [Buf(f"slab{i}") for i in range(NSLAB)]
    phase_base = top[0]

    xt = [alloc(2048) for i in range(2)]
    B_xt = [Buf(f"xt{i}") for i in range(2)]
    junk = alloc(1024, BF16)
    B_junk = Buf("junk")
    ssq = alloc(2)
    rstd = alloc(2)
    B_st = [Buf("st0"), Buf("st1")]
    xn = [alloc(1024, BF16) for i in range(2)]
    B_xn = [Buf("xn0"), Buf("xn1")]

    for i in range(NT):
        s_ = i % 2
        sc.dma("sp", lambda e, i=i, s_=s_: e.dma_start(out=xt[s_], in_=x[i * 128:(i + 1) * 128, :]),
               f"d_xt{s_}", writes=[B_xt[s_]])
        sc.op("act", lambda e, s_=s_: e.activation(out=junk, in_=xt[s_], func=AF.Square,
                                                   accum_out=ssq[:, s_:s_ + 1]),
              reads=[B_xt[s_]], writes=[B_junk, B_st[s_]])
        sc.op("act", lambda e, s_=s_: e.activation(out=rstd[:, s_:s_ + 1], in_=ssq[:, s_:s_ + 1], func=AF.Sqrt,
                                                   scale=1.0 / D, bias=epsc[:, 0:1]),
              reads=[B_st[s_], B_const], writes=[B_st[s_]])
        sc.op("dve", lambda e, s_=s_: e.reciprocal(out=rstd[:, s_:s_ + 1], in_=rstd[:, s_:s_ + 1]),
              reads=[B_st[s_]], writes=[B_st[s_]])
        sc.op("dve", lambda e, s_=s_: e.tensor_scalar(out=xn[s_], in0=xt[s_], scalar1=rstd[:, s_:s_ + 1],
                                                      scalar2=None, op0=ALU.mult),
              reads=[B_xt[s_], B_st[s_]], writes=[B_xn[s_]])
        for q4 in range(4):
            bk = (i * 4 + q4) % 4
            pt = pbank[bk][:].bitcast(BF16)
            for j in range(4):
                kc = q4 * 4 + j
                sc.op("pe", lambda e, pt=pt, j=j, kc=kc, s_=s_: e.transpose(
                    out=pt[:, j * 128:(j + 1) * 128], in_=xn[s_][:, kc * 128:(kc + 1) * 128], identity=ident_b),
                    reads=[B_xn[s_], B_const], writes=[B_pb[bk]])
            for j in range(4):
                kc = q4 * 4 + j
                sc.op("act", lambda e, pt=pt, j=j, kc=kc, i=i: e.activation(
                    out=hT[:, kc, i * 128:(i + 1) * 128], in_=pt[:, j * 128:(j + 1) * 128],
                    func=AF.Copy, scale=g1[:, kc:kc + 1]),
                    reads=[B_pb[bk], B_const], writes=[B_hT[i]])

    slab_ctr = [0]
    nslab = [NSLAB]

    def load_slab(pieces, kc_n=KC):
        s_ = slab_ctr[0] % nslab[0]
        slab_ctr[0] += 1
        for ap_, c0, n in pieces:
            sc.dma("pool", lambda e, ap_=ap_, c0=c0, n=n: e.dma_start(
                out=slab[s_][:, 0:kc_n, c0:c0 + n], in_=ap_.rearrange("(kc p) n -> p kc n", p=128)),
                f"d_slab{s_}", writes=[B_slab[s_]])
        return s_

    cvt_list = [(e_, m_) for e_ in range(64) for m_ in range(3)] if stage == "full" else []
    B_wbf = Buf("wbf")
    w_exp = (w_exp_gate, w_exp_up, w_exp_down)

    def issue_cvt(n):
        for _ in range(n):
            if not cvt_list:
                return
            e_, m_ = cvt_list.pop(0)
            sc.dma("pool", lambda e, e_=e_, m_=m_: e.dma_start(
                out=wbf_dram[e_, m_].rearrange("(a b) -> a b", b=w_exp[m_].shape[2]), in_=w_exp[m_][e_]),
                "d_cvt", writes=[B_wbf])

    pb_ctr = [0]

    def next_bank(lo=0, n=4):
        b = lo + pb_ctr[0] % n
        pb_ctr[0] += 1
        return b

    def mm_fm(s_, c0, tc, bk):
        for kc in range(KC):
            sc.op("pe", lambda e, kc=kc: e.matmul(
                pbank[bk][:], lhsT=slab[s_][:, kc, c0:c0 + 128],
                rhs=hT[:, kc, tc * 512:(tc + 1) * 512], start=(kc == 0), stop=(kc == KC - 1)),
                reads=[B_slab[s_]] + B_hT[tc * 4:(tc + 1) * 4], writes=[B_pb[bk]])

    def mm_tm(s_, c0, n, i, bk):
        for kc in range(KC):
            sc.op("pe", lambda e, kc=kc: e.matmul(
                pbank[bk][:, 0:n], lhsT=hT[:, kc, i * 128:(i + 1) * 128],
                rhs=slab[s_][:, kc, c0:c0 + n], start=(kc == 0), stop=(kc == KC - 1)),
                reads=[B_slab[s_], B_hT[i]], writes=[B_pb[bk]])

    sc.barrier()
    top[0] = phase_base
    GA0 = 4 * 2048 + 3 * 1536
    sgrow = [alloc(1024, BF16) for i in range(2)]
    B_sgrow = [Buf("sgrow0"), Buf("sgrow1")]
    B_sgd = Buf("sg_dram")
    rowctr = 0
    if stage not in ("dbg_ya", "dbg_yb"):
        for sl in range(8):
            s_ = load_slab([(w_in[:, GA0 + sl * 512:GA0 + (sl + 1) * 512], 0, 512)])
            issue_cvt(3)
            for fb in range(4):
                r_ = rowctr % 2
                rowctr += 1
                for tc in range(4):
                    bk = next_bank()
                    mm_fm(s_, fb * 128, tc, bk)
                    sc.op("act", lambda e, bk=bk, r_=r_, tc=tc: e.activation(
                        out=sgrow[r_][:, tc * 512:(tc + 1) * 512], in_=pbank[bk][:], func=AF.Sigmoid),
                        reads=[B_pb[bk]], writes=[B_sgrow[r_]])
                n0 = sl * 512 + fb * 128
                sc.dma("sp", lambda e, r_=r_, n0=n0: e.dma_start(out=sg_dram[n0:n0 + 128, :], in_=sgrow[r_]),
                       f"d_sgrow{r_}", reads=[B_sgrow[r_]], writes=[B_sgd])

    sc.barrier()
    top[0] = phase_base
    NH = 16 if stage not in ("dbg_ya", "dbg_yb") else (3 if stage == "dbg_ya" else 0)
    qT = [alloc(1024, BF16) for _ in range(2)]
    ktT = [alloc(1024, BF16) for _ in range(2)]
    itok = [alloc(1024, BF16, [128, NT, 128]) for _ in range(2)]
    sgtok = [alloc(1024, BF16, [128, NT, 128]) for _ in range(2)]
    B_qT = [[Buf(f"qT{s}_{c}") for c in range(4)] for s in range(2)]
    B_ktT = [[Buf(f"ktT{s}_{c}") for c in range(4)] for s in range(2)]
    B_itok = [[Buf(f"itok{s}_{c}") for c in range(NT)] for s in range(2)]
    B_sgtok = [[Buf(f"sgtok{s}_{c}") for c in range(NT)] for s in range(2)]
    Bc = alloc(2048)
    B_Bc = Buf("Bc")
    t_sig, t_kk, t_lf, t_b, t_eq, t_ek = [alloc(512) for _ in range(6)]
    B_t = {n: Buf(n) for n in ("sig", "kk", "lf", "b", "eq", "ek")}
    Dm = [alloc(16) for _ in range(2)]
    B_Dm = [Buf("Dm0"), Buf("Dm1")]
    dref = alloc(16)
    Sf = alloc(128)
    Stmp = alloc(128)
    Sbf = alloc(64, BF16)
    B_S = Buf("S")
    B_Sbf = Buf("Sbf")
    sctmp = alloc(128)
    scT = alloc(64, BF16)
    B_scT = Buf("scT")
    ktok = alloc(64, BF16)
    B_ktok = Buf("ktok")
    hss = alloc(1)
    hrs = alloc(1)
    B_hs = Buf("hs")
    hjunk = alloc(64, BF16)
    ytok = alloc(64, BF16)
    B_ytok = Buf("ytok")
    yaT = [alloc(1024, BF16) for _ in range(2)]
    B_yaT = [Buf("yaT0"), Buf("yaT1")]
    B_yad = Buf("ya_dram")
    pS_sc = pbank[4][:, 0:128]
    pS_tr = pbank[5][:].bitcast(BF16)
    pS_o = pbank[6][:, 0:128]
    pS_st = pbank[7][:, 0:128]
    B_psc, B_ptr1, B_ptr2, B_po, B_pst = Buf("psc"), Buf("ptr1"), Buf("ptr2"), Buf("po"), Buf("pst")

    def hg_inproj_units(h):
        sl = h % 2
        units = []
        holder = {}

        def u_load():
            holder["s"] = load_slab([(w_in[:, h * 128:(h + 1) * 128], 0, 128),
                                     (w_in[:, 2048 + h * 128:2048 + (h + 1) * 128], 128, 128),
                                     (w_in[:, 4096 + h * 128:4096 + (h + 1) * 128], 256, 128),
                                     (w_in[:, 6144 + h * 128:6144 + (h + 1) * 128], 384, 128)])
            issue_cvt(6)
        units.append(u_load)

        def u_q(tc):
            def f():
                s_ = holder["s"]
                bk = next_bank()
                mm_fm(s_, 0, tc, bk)
                sc.op("act", lambda e: e.activation(out=qT[sl][:, tc * 512:(tc + 1) * 512], in_=pbank[bk][:],
                                                    func=AF.Copy),
                      reads=[B_pb[bk]], writes=[B_qT[sl][tc]])
            return f

        def u_f(tc):
            def f():
                s_ = holder["s"]
                bk = next_bank()
                mm_fm(s_, 128, tc, bk)
                cs = slice(tc * 512, (tc + 1) * 512)
                sc.op("act", lambda e: e.activation(out=t_sig, in_=pbank[bk][:], func=AF.Exp, scale=-1.0),
                      reads=[B_pb[bk]], writes=[B_t["sig"]])
                sc.op("dve", lambda e: e.tensor_scalar(out=t_sig, in0=t_sig, scalar1=1.0, scalar2=None, op0=ALU.add),
                      reads=[B_t["sig"]], writes=[B_t["sig"]])
                sc.op("dve", lambda e: e.reciprocal(out=t_sig, in_=t_sig), reads=[B_t["sig"]], writes=[B_t["sig"]])
                sc.op("dve", lambda e: e.tensor_scalar(out=t_kk, in0=t_sig, scalar1=nomlb[:, h:h + 1],
                                                       scalar2=omlb[:, h:h + 1], op0=ALU.mult, op1=ALU.add),
                      reads=[B_t["sig"], B_const], writes=[B_t["kk"]])
                sc.op("act", lambda e: e.activation(out=t_lf, in_=t_sig, func=AF.Ln, scale=omlb[:, h:h + 1],
                                                    bias=lbT[:, h:h + 1]),
                      reads=[B_t["sig"], B_const], writes=[B_t["lf"]])
                init = 0.0 if tc == 0 else Bc[:, tc * 512 - 1:tc * 512]
                sc.op("dve", lambda e: e.tensor_tensor_scan(out=Bc[:, cs], data0=t_lf, data1=zeros512,
                                                            initial=init, op0=ALU.add, op1=ALU.add),
                      reads=[B_t["lf"], B_const, B_Bc], writes=[B_Bc])
                bview = Bc[:, cs].rearrange("p (a b) -> p a b", b=128)
                sc.op("dve", lambda e: e.tensor_tensor(
                    out=t_b.rearrange("p (a b) -> p a b", b=128), in0=bview,
                    in1=bview[:, :, 63:64].to_broadcast([128, 4, 128]), op=ALU.subtract),
                    reads=[B_Bc], writes=[B_t["b"]])
                sc.op("act", lambda e: e.activation(out=t_eq, in_=t_b, func=AF.Exp), reads=[B_t["b"]], writes=[B_t["eq"]])
                sc.op("act", lambda e: e.activation(out=t_ek, in_=t_b, func=AF.Exp, scale=-1.0),
                      reads=[B_t["b"]], writes=[B_t["ek"]])
                sc.op("dve", lambda e: e.tensor_tensor(out=qT[sl][:, cs], in0=qT[sl][:, cs], in1=t_eq, op=ALU.mult),
                      reads=[B_t["eq"], B_qT[sl][tc]], writes=[B_qT[sl][tc]])
                sc.op("dve", lambda e: e.tensor_tensor(out=ktT[sl][:, cs], in0=t_kk, in1=t_ek, op=ALU.mult),
                      reads=[B_t["ek"], B_t["kk"]], writes=[B_ktT[sl][tc]])
                if tc == 3:
                    refs = Bc.rearrange("p (a b) -> p a b", b=128)[:, :, 63]
                    sc.op("dve", lambda e: e.tensor_tensor(out=dref[:, 0:15], in0=refs[:, 1:16], in1=refs[:, 0:15],
                                                           op=ALU.subtract),
                          reads=[B_Bc], writes=[B_t["b"]])
                    sc.op("act", lambda e: e.activation(out=Dm[sl][:, 0:15], in_=dref[:, 0:15], func=AF.Exp),
                          reads=[B_t["b"]], writes=[B_Dm[sl]])
            return f

        def u_ig(i):
            def f():
                s_ = holder["s"]
                bk = next_bank()
                mm_tm(s_, 256, 256, i, bk)
                sc.op("act", lambda e: e.activation(out=itok[sl][:, i, :], in_=pbank[bk][:, 0:128], func=AF.Copy),
                      reads=[B_pb[bk]], writes=[B_itok[sl][i]])
                sc.op("act", lambda e: e.activation(out=sgtok[sl][:, i, :], in_=pbank[bk][:, 128:256], func=AF.Exp, scale=-1.0),
                      reads=[B_pb[bk]], writes=[B_sgtok[sl][i]])
                if i == NT - 1:
                    sc.op("dve", lambda e: e.tensor_scalar(out=sgtok[sl], in0=sgtok[sl], scalar1=1.0, scalar2=None, op0=ALU.add),
                          reads=B_sgtok[sl], writes=B_sgtok[sl])
                    def _rcp(e):
                        with nc.allow_low_precision("bf16 storage of the sigmoid gate"):
                            return e.reciprocal(out=sgtok[sl], in_=sgtok[sl])
                    sc.op("dve", _rcp, reads=B_sgtok[sl], writes=B_sgtok[sl])
            return f
        for tc in range(4):
            units.append(u_q(tc))
        for tc in range(4):
            units.append(u_f(tc))
        for i in range(NT):
            units.append(u_ig(i))
        return units

    scT2 = [scT, alloc(64, BF16)]
    ktok2 = [ktok, alloc(64, BF16)]
    ytok2 = [ytok, alloc(64, BF16)]
    sctmp2 = [sctmp, alloc(128)]
    hss2 = [hss, alloc(1)]
    hrs2 = [hrs, alloc(1)]
    B_scT2 = [Buf("scT0"), Buf("scT1")]
    B_ktok2 = [Buf("ktok0"), Buf("ktok1")]
    B_ytok2 = [Buf("ytok0"), Buf("ytok1")]
    B_hs2 = [Buf("hs0"), Buf("hs1")]
    pSsc2 = [pbank[4][:, 0:128], pbank[4][:, 128:256]]
    pStr_k = [pS_tr[:, 0:128], pS_tr[:, 128:256]]
    pStr_y = [pS_tr[:, 256:384], pS_tr[:, 384:512]]
    pSo2 = [pbank[6][:, 0:128], pbank[6][:, 128:256]]
    pSst2 = [pbank[7][:, 0:128], pbank[7][:, 128:256]]
    pStr_y = [pbank[7][:].bitcast(BF16)[:, 0:128], pbank[7][:].bitcast(BF16)[:, 128:256]]
    pSst2 = [pbank[6][:, 0:128], pbank[6][:, 128:256]]
    pSo2 = [pbank[5][:, 0:128], pbank[5][:, 128:256]]
    pStr_k = [pbank[4][:].bitcast(BF16)[:, 512:640], pbank[4][:].bitcast(BF16)[:, 640:768]]
    B_psc2 = [B_pb[4], B_pb[4]]
    B_ptrk = [B_pb[4], B_pb[4]]
    B_po2 = [B_pb[5], B_pb[5]]
    B_pst2 = [B_pb[6], B_pb[6]]
    B_ptry = [B_pb[7], B_pb[7]]

    def hg_rec_steps(h):
        sl = h % 2

        def stA(i):
            d = i % 2
            ts = slice(i * 128, (i + 1) * 128)
            tc = i // 4
            sc.op("pe", lambda e: e.matmul(pSsc2[d], lhsT=ktT[sl][:, ts], rhs=qT[sl][:, ts], start=True, stop=True),
                  reads=[B_ktT[sl][tc], B_qT[sl][tc]], writes=[B_psc2[d]])
            if i < NT - 1:
                sc.op("pe", lambda e: e.transpose(out=pStr_k[d], in_=ktT[sl][:, ts], identity=ident_b),
                      reads=[B_ktT[sl][tc], B_const], writes=[B_ptrk[d]])
            sc.op("dve", lambda e: e.tensor_scalar(out=sctmp2[d], in0=pSsc2[d], scalar1=1e30, scalar2=-1e30,
                                                   op0=ALU.min, op1=ALU.max),
                  reads=[B_psc2[d]], writes=[B_scT2[d], B_psc2[d]])
            sc.op("dve", lambda e: e.tensor_tensor(out=scT2[d], in0=sctmp2[d], in1=mask_ut, op=ALU.mult),
                  reads=[B_scT2[d], B_const], writes=[B_scT2[d]])
            if i < NT - 1:
                sc.op("act", lambda e: e.activation(out=ktok2[d], in_=pStr_k[d], func=AF.Copy),
                      reads=[B_ptrk[d]], writes=[B_ktok2[d], B_ptrk[d]])

        def stB(i):
            d = i % 2
            ts = slice(i * 128, (i + 1) * 128)
            tc = i // 4
            sc.op("pe", lambda e: e.matmul(pSo2[d], lhsT=scT2[d], rhs=itok[sl][:, i, :], start=True, stop=(i == 0)),
                  reads=[B_scT2[d], B_itok[sl][i]], writes=[B_po2[d]])
            if i > 0:
                sc.op("pe", lambda e: e.matmul(pSo2[d], lhsT=qT[sl][:, ts], rhs=Sbf, start=False, stop=True),
                      reads=[B_qT[sl][tc], B_Sbf], writes=[B_po2[d]])
            if i < NT - 1:
                sc.op("pe", lambda e: e.matmul(pSst2[d], lhsT=ktok2[d], rhs=itok[sl][:, i, :], start=True, stop=True),
                      reads=[B_ktok2[d], B_itok[sl][i]], writes=[B_pst2[d]])
                if i == 0:
                    sc.op("dve", lambda e: e.tensor_scalar(out=Sf, in0=pSst2[d], scalar1=Dm[sl][:, 0:1], scalar2=None,
                                                           op0=ALU.mult),
                          reads=[B_pst2[d], B_Dm[sl]], writes=[B_S, B_pst2[d]])
                else:
                    sc.op("dve", lambda e: e.tensor_scalar(out=Stmp, in0=Sf, scalar1=Dm[sl][:, i:i + 1], scalar2=None,
                                                           op0=ALU.mult),
                          reads=[B_S, B_Dm[sl]], writes=[B_S])
                    sc.op("dve", lambda e: e.scalar_tensor_tensor(out=Sf, in0=pSst2[d], scalar=Dm[sl][:, i:i + 1],
                                                                  in1=Stmp, op0=ALU.mult, op1=ALU.add),
                          reads=[B_pst2[d], B_S, B_Dm[sl]], writes=[B_S, B_pst2[d]])
                sc.op("act", lambda e: e.activation(out=Sbf, in_=Sf, func=AF.Copy), reads=[B_S], writes=[B_Sbf])
            sc.op("act", lambda e: e.activation(out=hjunk, in_=pSo2[d], func=AF.Square, accum_out=hss2[d]),
                  reads=[B_po2[d]], writes=[B_hs2[d], B_po2[d]])
            sc.op("act", lambda e: e.activation(out=hrs2[d], in_=hss2[d], func=AF.Ln, scale=1.0 / 128, bias=epsc[:, 0:1]),
                  reads=[B_hs2[d], B_const], writes=[B_hs2[d]])
            sc.op("act", lambda e: e.activation(out=hrs2[d], in_=hrs2[d], func=AF.Exp, scale=-0.5),
                  reads=[B_hs2[d]], writes=[B_hs2[d]])
            sc.op("dve", lambda e: e.scalar_tensor_tensor(out=ytok2[d], in0=pSo2[d], scalar=hrs2[d][:, 0:1],
                                                          in1=sgtok[sl][:, i, :], op0=ALU.mult, op1=ALU.mult),
                  reads=[B_po2[d], B_hs2[d], B_sgtok[sl][i]], writes=[B_ytok2[d], B_po2[d]])

        def stC(i):
            d = i % 2
            ts = slice(i * 128, (i + 1) * 128)
            sc.op("pe", lambda e: e.transpose(out=pStr_y[d], in_=ytok2[d], identity=ident_b),
                  reads=[B_ytok2[d], B_const], writes=[B_ptry[d]])
            sc.op("act", lambda e: e.activation(out=yaT[sl][:, ts], in_=pStr_y[d], func=AF.Copy,
                                                scale=gnT[:, h:h + 1]),
                  reads=[B_ptry[d], B_const], writes=[B_yaT[sl], B_ptry[d]])
            if i == NT - 1:
                sc.dma("sp", lambda e: e.dma_start(out=ya_dram[h * 128:(h + 1) * 128, :], in_=yaT[sl]),
                       f"d_yaT{sl}", reads=[B_yaT[sl]], writes=[B_yad])

        def slot(u):
            def f():
                if u < NT:
                    stA(u)
                if 0 <= u - 1 < NT:
                    stB(u - 1)
                if 0 <= u - 2 < NT:
                    stC(u - 2)
            return f
        return [slot(u) for u in range(NT + 2)]

    prev_steps = []
    for h in range(NH + 1):
        units = hg_inproj_units(h) if h < NH else []
        n = max(len(units), len(prev_steps))
        for u in range(n):
            if u < len(units):
                units[u]()
            if u < len(prev_steps):
                prev_steps[u]()
        prev_steps = hg_rec_steps(h) if h < NH else []

    sc.barrier()
    top[0] = phase_base
    ATT_G = ((128, 1), (512, 4), (2048, 16))
    DMAX = (1, 4, 15)
    JW = (256, 640, 2048)
    WW = tuple(j + 127 for j in JW)
    WOFF = (0, WW[0], WW[0] + WW[1])
    NSLOT = 4 if stage != "dbg_yb" else 1
    rb33 = alloc(12)
    rbx = alloc(12 * 128, F32, [128, 12, 128])
    ohs = alloc(WW[0] + WW[1] + WW[2])
    zst = alloc(1088, BF16)
    B_att = Buf("attc")
    B_zst = Buf("zst")
    B_zd = Buf("zrow_dram")
    Btoe = [[alloc(JW[g] // 2, BF16) for s in range(4)] for g in range(3)]
    B_toe = Buf("toe")
    sc.dma("sp", lambda e: e.dma_start(out=rb33[0:32, :], in_=rel_bias), "d_const", writes=[B_att])
    sc.op("dve", lambda e: e.memset(rb33[32:33, :], NEG), writes=[B_att])
    sc.dma("sp", lambda e: e.dma_start(out=ohs[0:33, :], in_=att_oh), "d_const", writes=[B_att])
    sc.op("dve", lambda e: e.tensor_copy(out=rbx[0:33, :, :], in_=rb33[0:33, :].unsqueeze(2).to_broadcast([33, 12, 128])),
          reads=[B_att], writes=[B_att])
    for g in range(3):
        for s in range(NSLOT):
            hd = g * 4 + s
            W = WW[g]
            for c0 in range(0, W, 512):
                n = min(512, W - c0)
                sc.op("pe", lambda e, c0=c0, n=n, hd=hd, g=g: e.matmul(
                    pbank[7][:, 0:n], lhsT=rbx[0:33, hd, :], rhs=ohs[0:33, WOFF[g] + c0:WOFF[g] + c0 + n],
                    start=True, stop=True), reads=[B_att], writes=[B_pb[7]])
                sc.op("act", lambda e, c0=c0, n=n: e.activation(out=zst[:, c0:c0 + n], in_=pbank[7][:, 0:n], func=AF.Copy),
                      reads=[B_pb[7]], writes=[B_zst])
            zoff = hd * 128 * 2176
            sc.dma("sp", lambda e, W=W, zoff=zoff: e.dma_start(
                out=zrow_dram[zoff:zoff + 128 * W].rearrange("(c w) -> c w", w=W), in_=zst[:, 0:W]),
                "d_zst", reads=[B_zst], writes=[B_zd])
            src = bass.AP(tensor=zrow_dram.tensor, offset=zrow_dram.offset + zoff + 127,
                          ap=[[W - 1, 128], [1, JW[g]]])
            sc.dma("sp", lambda e, src=src, g=g, s=s: e.dma_start(out=Btoe[g][s], in_=src),
                   "d_toe", reads=[B_zd], writes=[B_toe])

    qTs = [alloc(1024, BF16) for g in range(3)]
    kTs = [alloc(1024, BF16) for g in range(3)]
    vtok = alloc(16 * 3 * 130 // 2, BF16).rearrange("p (t g c) -> p t g c", g=3, c=130)
    B_qTs = [[Buf(f"qTs{g}_{c}") for c in range(4)] for g in range(3)]
    B_kTs = [[Buf(f"kTs{g}_{c}") for c in range(4)] for g in range(3)]
    B_vtok = [Buf(f"vtok{i}") for i in range(NT)]
    PT = [alloc(256, BF16) for _ in range(2)]
    B_PT = [Buf("PT0"), Buf("PT1")]
    rden = alloc(1)
    B_rden = Buf("rden")
    obt = alloc(64, BF16)
    B_obt = Buf("obt")
    ybT = alloc(1024, BF16)
    B_ybT = Buf("ybT")
    B_ybd = Buf("yb_dram")
    sc.op("dve", lambda e: e.memset(vtok[:, :, :, 128:130], 1.0), writes=B_vtok)
    po_ap = [pbank[4 + j][:, 0:129] for j in range(4)]
    B_poa = [B_pb[4 + j] for j in range(4)]
    pt_ctr = 0
    AQ0 = 8192
    for s in range(NSLOT):
        pieces_q = [(w_in[:, AQ0 + (g * 4 + s) * 128:AQ0 + (g * 4 + s + 1) * 128], g * 128, 128) for g in range(3)]
        pieces_k = [(w_in[:, AQ0 + 1536 + (g * 4 + s) * 128:AQ0 + 1536 + (g * 4 + s + 1) * 128], g * 128, 128) for g in range(3)]
        pieces_v = [(w_in[:, AQ0 + 3072 + (g * 4 + s) * 128:AQ0 + 3072 + (g * 4 + s + 1) * 128], g * 128, 128) for g in range(3)]
        s_q = load_slab(pieces_q)
        s_k = load_slab(pieces_k)
        s_v = load_slab(pieces_v)
        issue_cvt(12)

        def inproj_units(tc, s_q=s_q, s_k=s_k, s_v=s_v):
            us = []
            for g in range(3):
                def uq(g=g):
                    bk = next_bank()
                    mm_fm(s_q, g * 128, tc, bk)
                    sc.op("act", lambda e: e.activation(
                        out=qTs[g][:, tc * 512:(tc + 1) * 512], in_=pbank[bk][:], func=AF.Copy, scale=128.0 ** -0.5),
                        reads=[B_pb[bk]], writes=[B_qTs[g][tc]])
                us.append(uq)

                def uk(g=g):
                    bk = next_bank()
                    mm_fm(s_k, g * 128, tc, bk)
                    sc.op("dve", lambda e: e.tensor_copy(out=kTs[g][:, tc * 512:(tc + 1) * 512], in_=pbank[bk][:]),
                          reads=[B_pb[bk]], writes=[B_kTs[g][tc]])
                us.append(uk)
            for i in range(4 * tc, 4 * tc + 4):
                def uv(i=i):
                    bk = next_bank()
                    mm_tm(s_v, 0, 384, i, bk)
                    sc.op("act", lambda e: e.activation(
                        out=vtok[:, i, :, 0:128], in_=pbank[bk][:, 0:384].rearrange("p (g c) -> p g c", c=128), func=AF.Copy),
                        reads=[B_pb[bk]], writes=[B_vtok[i]])
                us.append(uv)
            return us

        for u_ in inproj_units(0):
            u_()
        for Q in range(4):
            pend = inproj_units(Q + 1) if Q < 3 else []
            contribs = []
            for g in range(3):
                for m in range(max(0, 4 * Q - DMAX[g]), 4 * Q + 4):
                    T_lo = max(m, 4 * Q)
                    T_hi = min(4 * Q + 3, m + DMAX[g])
                    if T_hi >= T_lo:
                        contribs.append((g, m, T_lo, T_hi))
            firstT, lastT = {}, {}
            for ci, (g, m, T_lo, T_hi) in enumerate(contribs):
                for T in range(T_lo, T_hi + 1):
                    firstT.setdefault(T, ci)
                    lastT[T] = ci
            every = max(1, len(contribs) // (len(pend) + 1)) if pend else 0
            for ci, (g, m, T_lo, T_hi) in enumerate(contribs):
                if pend and ci % every == every - 1:
                    pend.pop(0)()
                ncols = (T_hi - T_lo + 1) * 128
                bk = next_bank()
                qbufs = [B_qTs[g][T // 4] for T in range(T_lo, T_hi + 1)]
                sc.op("pe", lambda e, g=g, m=m, T_lo=T_lo, T_hi=T_hi, ncols=ncols, bk=bk: e.matmul(
                    pbank[bk][:, 0:ncols], lhsT=kTs[g][:, m * 128:(m + 1) * 128],
                    rhs=qTs[g][:, T_lo * 128:(T_hi + 1) * 128], start=True, stop=False),
                    reads=[B_kTs[g][m // 4]] + qbufs, writes=[B_pb[bk]])
                sc.op("pe", lambda e, g=g, m=m, T_lo=T_lo, T_hi=T_hi, ncols=ncols, bk=bk, s=s: e.matmul(
                    pbank[bk][:, 0:ncols], lhsT=ident_b,
                    rhs=Btoe[g][s][:, (T_lo - m) * 128:(T_hi - m + 1) * 128], start=False, stop=True),
                    reads=[B_toe, B_const], writes=[B_pb[bk]])
                p_ = pt_ctr % 2
                pt_ctr += 1
                sc.op("act", lambda e, p_=p_, ncols=ncols, bk=bk: e.activation(
                    out=PT[p_][:, 0:ncols], in_=pbank[bk][:, 0:ncols], func=AF.Exp),
                    reads=[B_pb[bk]], writes=[B_PT[p_]])
                for T in range(T_lo, T_hi + 1):
                    j = T - 4 * Q
                    st_, sp_ = (firstT[T] == ci), (lastT[T] == ci)
                    sc.op("pe", lambda e, p_=p_, T=T, T_lo=T_lo, j=j, m=m, g=g, st_=st_, sp_=sp_: e.matmul(
                        po_ap[j], lhsT=PT[p_][:, (T - T_lo) * 128:(T - T_lo + 1) * 128], rhs=vtok[:, m, g, 0:129],
                        start=st_, stop=sp_),
                        reads=[B_PT[p_], B_vtok[m]], writes=[B_poa[j]])
            while pend:
                pend.pop(0)()
            for j in range(4):
                T = 4 * Q + j
                sc.op("dve", lambda e, j=j: e.reciprocal(out=rden, in_=po_ap[j][:, 128:129]),
                      reads=[B_poa[j]], writes=[B_rden])
                sc.op("dve", lambda e, j=j: e.tensor_scalar(out=obt, in0=po_ap[j][:, 0:128], scalar1=rden[:, 0:1],
                                                            scalar2=None, op0=ALU.mult),
                      reads=[B_poa[j], B_rden], writes=[B_obt])
                bk = next_bank()
                ptr_att = pbank[bk][:].bitcast(BF16)
                sc.op("pe", lambda e, ptr_att=ptr_att: e.transpose(out=ptr_att[:, 0:128], in_=obt, identity=ident_b),
                      reads=[B_obt, B_const], writes=[B_pb[bk]])
                sc.op("act", lambda e, T=T, ptr_att=ptr_att: e.activation(out=ybT[:, T * 128:(T + 1) * 128], in_=ptr_att[:, 0:128],
                                                         func=AF.Copy),
                      reads=[B_pb[bk]], writes=[B_ybT])
        sc.dma("sp", lambda e, s=s: e.dma_start(out=yb_dram[s * 128:(s + 1) * 128, :], in_=ybT),
               "d_ybT", reads=[B_ybT], writes=[B_ybd])
    fin = [B_sgd, B_yad, B_ybd]
    if stage in ('full', 'dbg_full'):
        sc.barrier()
        top[0] = phase_base
        yaT_all = hT
        sc.dma("sp", lambda e: e.dma_start(out=yaT_all[:, 0:8, :], in_=ya_dram[0:1024, :].rearrange("(c p) t -> p c t", p=128)),
               "d_big", reads=[B_yad], writes=B_hT)
        sc.dma("sp", lambda e: e.dma_start(out=yaT_all[:, 8:16, :], in_=ya_dram[1024:2048, :].rearrange("(c p) t -> p c t", p=128)),
               "d_big", reads=[B_yad], writes=B_hT)
        mergedT = alloc(16384, BF16, [128, KC, S])
        ybT_all = alloc(4096, BF16, [128, 4, S])
        B_ybTa = Buf("ybT_all")
        sc.dma("sp", lambda e: e.dma_start(out=ybT_all, in_=yb_dram.rearrange("(c p) t -> p c t", p=128)),
               "d_big", reads=[B_ybd], writes=[B_ybTa])
        B_mT = [Buf(f"mT{c}") for c in range(4)]
        sga = alloc(1024, BF16)
        sgb = alloc(1024, BF16)
        B_sga, B_sgb = Buf("sga"), Buf("sgb")
        tmpA = slab[2][:, 0:2, :].rearrange("p a b -> p (a b)").bitcast(F32)
        tmpB = slab[2][:, 2:4, :].rearrange("p a b -> p (a b)").bitcast(F32)
        B_tA, B_tB = Buf("tmpA"), Buf("tmpB")
        nslab[0] = 2
        slab_ctr[0] = 0
        for ns in range(4):
            s_a = load_slab([(w_branch_a[:, ns * 512:(ns + 1) * 512], 0, 512)])
            s_b = load_slab([(w_branch_b[:, ns * 512:(ns + 1) * 512], 0, 512)], kc_n=4)
            issue_cvt(6 if ns < 3 else 200)
            for fb in range(4):
                nb = ns * 4 + fb
                sc.dma("sp", lambda e, nb=nb: e.dma_start(out=sga, in_=sg_dram[nb * 128:(nb + 1) * 128, :]),
                       "d_sga", reads=[B_sgd], writes=[B_sga])
                sc.dma("sp", lambda e, nb=nb: e.dma_start(out=sgb, in_=sg_dram[2048 + nb * 128:2048 + (nb + 1) * 128, :]),
                       "d_sgb", reads=[B_sgd], writes=[B_sgb])
                for tc in range(4):
                    bka = next_bank()
                    for kc in range(KC):
                        sc.op("pe", lambda e, kc=kc, bka=bka, s_a=s_a, fb=fb, tc=tc: e.matmul(
                            pbank[bka][:], lhsT=slab[s_a][:, kc, fb * 128:(fb + 1) * 128],
                            rhs=yaT_all[:, kc, tc * 512:(tc + 1) * 512], start=(kc == 0), stop=(kc == KC - 1)),
                            reads=[B_slab[s_a]] + B_hT[tc * 4:(tc + 1) * 4], writes=[B_pb[bka]])
                    bkb = next_bank()
                    for kc in range(4):
                        sc.op("pe", lambda e, kc=kc, bkb=bkb, s_b=s_b, fb=fb, tc=tc: e.matmul(
                            pbank[bkb][:], lhsT=slab[s_b][:, kc, fb * 128:(fb + 1) * 128],
                            rhs=ybT_all[:, kc, tc * 512:(tc + 1) * 512], start=(kc == 0), stop=(kc == 3)),
                            reads=[B_slab[s_b], B_ybTa], writes=[B_pb[bkb]])
                    cs = slice(tc * 512, (tc + 1) * 512)
                    sc.op("dve", lambda e, bka=bka, cs=cs: e.tensor_tensor(out=tmpA, in0=pbank[bka][:], in1=sga[:, cs], op=ALU.mult),
                          reads=[B_pb[bka], B_sga], writes=[B_tA])
                    sc.op("dve", lambda e, bkb=bkb, cs=cs: e.tensor_tensor(out=tmpB, in0=pbank[bkb][:], in1=sgb[:, cs], op=ALU.mult),
                          reads=[B_pb[bkb], B_sgb], writes=[B_tB])
                    sc.op("pool", lambda e, nb=nb, cs=cs: e.tensor_tensor(out=mergedT[:, nb, cs], in0=tmpA, in1=tmpB, op=ALU.add),
                          reads=[B_tA, B_tB], writes=[B_mT[tc]])

        sc.barrier()
        wo = hT
        B_wo = Buf("wo")
        for c in range(4):
            sc.dma("pool", lambda e, c=c: e.dma_start(
                out=wo[:, c * 4:(c + 1) * 4, :], in_=w_out[c * 512:(c + 1) * 512, :].rearrange("(kc p) n -> p kc n", p=128)),
                "d_big", writes=[B_wo])
        top[0] = phase_base + 16384
        g2b = alloc(2048)
        gfb = alloc(2048)
        B_gb = Buf("gb")
        bcast = lambda ap_: bass.AP(tensor=ap_.tensor, offset=ap_.offset, ap=[[0, 128], [1, ap_.shape[0]]])
        sc.dma("sp", lambda e: e.dma_start(out=g2b, in_=bcast(norm2_gain)), "d_const", writes=[B_gb])
        sc.dma("sp", lambda e: e.dma_start(out=gfb, in_=bcast(final_norm_gain)), "d_const", writes=[B_gb])
        top2 = [slab_base]

        def alloc2(words, dt=F32, shape=None):
            save = top[0]
            top[0] = top2[0]
            a = alloc(words, dt, shape)
            top2[0] = top[0]
            top[0] = save
            assert top2[0] <= slab_base + 12288
            return a
        xt2 = alloc2(2048)
        x2t = alloc2(2048)
        h2f = alloc2(2048)
        h2T = alloc2(2048, F32, [128, KC, 128])
        h2b = alloc2(1024, BF16)
        wr = alloc2(16 * 72, F32, [128, KC, 72])
        lg_all = alloc2(16 * 72, F32, [128, NT, 72])
        brow = alloc2(72)
        onesrow = alloc2(128)
        ss2 = alloc2(1)
        rs2 = alloc2(1)
        B_xt2, B_x2t, B_h2f, B_h2T, B_h2b, B_wr, B_lg, B_s2 = (Buf(n) for n in ("xt2", "x2t", "h2f", "h2T", "h2b", "wr", "lg", "s2"))
        B_x2d, B_h2d = Buf("x2_dram"), Buf("h2_dram")
        sc.dma("sp", lambda e: e.dma_start(out=wr[:, :, 0:8], in_=w_router_group.rearrange("(kc p) n -> p kc n", p=128),
                                           allow_slow_non_contiguous=True), "d_const", writes=[B_wr])
        sc.dma("sp", lambda e: e.dma_start(out=wr[:, :, 8:72], in_=w_router_expert.rearrange("(kc p) n -> p kc n", p=128),
                                           allow_slow_non_contiguous=True), "d_const", writes=[B_wr])
        sc.dma("sp", lambda e: e.dma_start(out=brow[0:1, 0:8], in_=b_router_group), "d_const", writes=[B_wr])
        sc.dma("sp", lambda e: e.dma_start(out=brow[0:1, 8:72], in_=b_router_expert), "d_const", writes=[B_wr])
        sc.op("dve", lambda e: e.memset(onesrow[0:1, :], 1.0), writes=[B_wr])
        sc.op("dve", lambda e: e.memset(h2f, 0.0), writes=[B_h2f])
        sc.op("dve", lambda e: e.memset(h2b, 0.0), writes=[B_h2b])
        sc.dma("sp", lambda e: e.dma_start(out=h2_dram[2048:2049, :], in_=h2b[0:1, :]), "d_h2b", reads=[B_h2b], writes=[B_h2d])
        B_yed = Buf("ye_dram")
        sc.dma("sp", lambda e: e.dma_start(out=ye_dram[8192:8320, :], in_=h2b), "d_h2b", reads=[B_h2b], writes=[B_yed])
        for i in range(NT):
            ts = slice(i * 128, (i + 1) * 128)
            sc.dma("sp", lambda e, ts=ts: e.dma_start(out=xt2, in_=x[ts, :]), "d_xt2", writes=[B_xt2])
            for dsl in range(4):
                for kc in range(KC):
                    sc.op("pe", lambda e, kc=kc, dsl=dsl, ts=ts: e.matmul(
                        pbank[dsl][:], lhsT=mergedT[:, kc, ts], rhs=wo[:, kc, dsl * 512:(dsl + 1) * 512],
                        start=(kc == 0), stop=(kc == KC - 1)),
                        reads=[B_mT[i // 4], B_wo], writes=[B_pb[dsl]])
                sc.op("dve", lambda e, dsl=dsl: e.tensor_tensor(out=x2t[:, dsl * 512:(dsl + 1) * 512], in0=pbank[dsl][:],
                                                               in1=xt2[:, dsl * 512:(dsl + 1) * 512], op=ALU.add),
                      reads=[B_pb[dsl], B_xt2], writes=[B_x2t])
            sc.dma("sp", lambda e, ts=ts: e.dma_start(out=x2_dram[ts, :], in_=x2t), "d_x2t", reads=[B_x2t], writes=[B_x2d])
            sc.op("act", lambda e: e.activation(out=h2f, in_=x2t, func=AF.Square, accum_out=ss2), reads=[B_x2t], writes=[B_h2f, B_s2])
            sc.op("act", lambda e: e.activation(out=rs2, in_=ss2, func=AF.Sqrt, scale=1.0 / D, bias=epsc[:, 0:1]),
                  reads=[B_s2, B_const], writes=[B_s2])
            sc.op("dve", lambda e: e.reciprocal(out=rs2, in_=rs2), reads=[B_s2], writes=[B_s2])
            sc.op("dve", lambda e: e.scalar_tensor_tensor(out=h2f, in0=x2t, scalar=rs2[:, 0:1], in1=g2b, op0=ALU.mult, op1=ALU.mult),
                  reads=[B_x2t, B_s2, B_gb], writes=[B_h2f])
            sc.op("act", lambda e: e.activation(out=h2b, in_=h2f, func=AF.Copy), reads=[B_h2f], writes=[B_h2b])
            sc.dma("sp", lambda e, ts=ts: e.dma_start(out=h2_dram[ts, :], in_=h2b), "d_h2b", reads=[B_h2b], writes=[B_h2d])
            for q4 in range(4):
                bk = 4 + q4
                for j in range(4):
                    kc = q4 * 4 + j
                    sc.op("pe", lambda e, bk=bk, j=j, kc=kc: e.transpose(
                        out=pbank[bk][:, j * 128:(j + 1) * 128], in_=h2f[:, kc * 128:(kc + 1) * 128], identity=ident_f),
                        reads=[B_h2f, B_const], writes=[B_pb[bk]])
                sc.op("act" if q4 % 2 else "dve", (lambda e, bk=bk, q4=q4: e.activation(
                    out=h2T[:, q4 * 4:(q4 + 1) * 4, :], in_=pbank[bk][:].rearrange("p (a b) -> p a b", b=128), func=AF.Copy))
                    if q4 % 2 else (lambda e, bk=bk, q4=q4: e.tensor_copy(
                        out=h2T[:, q4 * 4:(q4 + 1) * 4, :], in_=pbank[bk][:].rearrange("p (a b) -> p a b", b=128))),
                    reads=[B_pb[bk]], writes=[B_h2T])
            for kc in range(KC):
                sc.op("pe", lambda e, kc=kc: e.matmul(pbank[0][:, 0:72], lhsT=h2T[:, kc, :], rhs=wr[:, kc, :],
                                                      start=(kc == 0), stop=False),
                      reads=[B_h2T, B_wr], writes=[B_pb[0]])
            sc.op("pe", lambda e: e.matmul(pbank[0][:, 0:72], lhsT=onesrow[0:1, :], rhs=brow[0:1, :], start=False, stop=True),
                  reads=[B_wr], writes=[B_pb[0]])
            sc.op("dve", lambda e, i=i: e.tensor_copy(out=lg_all[:, i, :], in_=pbank[0][:, 0:72]), reads=[B_pb[0]], writes=[B_lg])

        sc.barrier()
        top[0] = phase_base
        B_r = Buf("route")

        def R(eng, fn):
            sc.op(eng, fn, reads=[B_r, B_lg, B_const], writes=[B_r])
        lgG = lg_all[:, :, 0:8]
        lgE = lg_all[:, :, 8:72].rearrange("p t (g e) -> p t g e", e=8)
        mG, sumG, pg, m1, m2, w1 = (alloc(16) for _ in range(6))
        ohG, eG, lgin, oh1, oh2, msk = (alloc(128, F32, [128, 16, 8]) for _ in range(6))
        sel = alloc(1024).rearrange("p (t g e) -> p t g e", g=8, e=8)
        O1 = alloc(1024).rearrange("p (t g e) -> p t g e", g=8, e=8)
        O2 = alloc(1024).rearrange("p (t g e) -> p t g e", g=8, e=8)
        Osum = alloc(1024, F32, [128, 16, 64])
        Ob = alloc(512, BF16, [128, 16, 64])
        cum = alloc(1024, F32, [128, 16, 64])
        tmp64 = alloc(1024, F32, [128, 16, 64])
        gates = alloc(32, F32, [128, 16, 2])
        slot_ = alloc(32, F32, [128, 16, 2])
        eid_ = alloc(32, F32, [128, 16, 2])
        tixf = alloc(32, F32, [128, 16, 2])
        rixf = alloc(32, F32, [128, 16, 2])
        tix = alloc(32, I32, [128, 16, 2])
        rix = alloc(32, I32, [128, 16, 2])
        iota_i = alloc(64, I32)
        iota_e = alloc(64)
        tokid = alloc(16, I32)
        fill_i = alloc(65, I32)
        ones_b = alloc(64, BF16)
        mst_b = alloc(64, BF16)
        mstf = alloc(128)
        tabs = alloc(64, I32)
        bc3 = lambda a: a.unsqueeze(2).to_broadcast([128, 16, 8])
        R("dve", lambda e: e.tensor_reduce(out=mG, in_=lgG, axis=AX.X, op=ALU.max))
        R("dve", lambda e: e.tensor_tensor(out=ohG, in0=lgG, in1=bc3(mG), op=ALU.is_equal))
        R("dve", lambda e: e.tensor_tensor(out=eG, in0=lgG, in1=bc3(mG), op=ALU.subtract))
        R("act", lambda e: e.activation(out=eG, in_=eG, func=AF.Exp))
        R("dve", lambda e: e.tensor_reduce(out=sumG, in_=eG, axis=AX.X, op=ALU.add))
        R("dve", lambda e: e.reciprocal(out=pg, in_=sumG))
        R("dve", lambda e: e.tensor_tensor(out=sel, in0=lgE, in1=ohG.unsqueeze(3).to_broadcast([128, 16, 8, 8]), op=ALU.mult))
        R("dve", lambda e: e.tensor_reduce(out=lgin, in_=sel.rearrange("p t g e -> p t e g"), axis=AX.X, op=ALU.add))
        R("dve", lambda e: e.tensor_reduce(out=m1, in_=lgin, axis=AX.X, op=ALU.max))
        R("dve", lambda e: e.tensor_tensor(out=oh1, in0=lgin, in1=bc3(m1), op=ALU.is_equal))
        R("dve", lambda e: e.scalar_tensor_tensor(out=msk, in0=oh1, scalar=-1e30, in1=lgin, op0=ALU.mult, op1=ALU.add))
        R("dve", lambda e: e.tensor_reduce(out=m2, in_=msk, axis=AX.X, op=ALU.max))
        R("dve", lambda e: e.tensor_tensor(out=oh2, in0=msk, in1=bc3(m2), op=ALU.is_equal))
        R("dve", lambda e: e.tensor_tensor(out=w1, in0=m2, in1=m1, op=ALU.subtract))
        R("act", lambda e: e.activation(out=w1, in_=w1, func=AF.Exp))
        R("dve", lambda e: e.tensor_scalar(out=w1, in0=w1, scalar1=1.0, scalar2=None, op0=ALU.add))
        R("dve", lambda e: e.reciprocal(out=w1, in_=w1))
        R("dve", lambda e: e.tensor_tensor(out=gates[:, :, 0], in0=pg, in1=w1, op=ALU.mult))
        R("dve", lambda e: e.tensor_tensor(out=gates[:, :, 1], in0=pg, in1=gates[:, :, 0], op=ALU.subtract))
        for O_, oh_ in ((O1, oh1), (O2, oh2)):
            R("dve", lambda e, O_=O_: e.tensor_copy(out=O_, in_=ohG.unsqueeze(3).to_broadcast([128, 16, 8, 8])))
            R("dve", lambda e, O_=O_, oh_=oh_: e.tensor_tensor(out=O_, in0=O_, in1=oh_.unsqueeze(2).to_broadcast([128, 16, 8, 8]),
                                                             op=ALU.mult))
        O1f = O1.rearrange("p t g e -> p t (g e)")
        O2f = O2.rearrange("p t g e -> p t (g e)")
        R("dve", lambda e: e.tensor_tensor(out=Osum, in0=O1f, in1=O2f, op=ALU.add))
        R("dve", lambda e: e.tensor_copy(out=Ob, in_=Osum))
        R("dve", lambda e: e.memset(ones_b, 1.0))
        R("dve", lambda e: e.tensor_tensor(out=mstf, in0=mask_ut, in1=ident_f, op=ALU.subtract))
        R("dve", lambda e: e.tensor_copy(out=mst_b, in_=mstf))
        for i in range(NT):
            bk = 4 + (i // 8)
            cols = slice((i % 8) * 64, (i % 8 + 1) * 64)
            for j in range(i):
                sc.op("pe", lambda e, bk=bk, cols=cols, j=j: e.matmul(pbank[bk][:, cols], lhsT=ones_b, rhs=Ob[:, j, :],
                                                                     start=(j == 0), stop=False),
                      reads=[B_r], writes=[B_pb[bk]])
            sc.op("pe", lambda e, bk=bk, cols=cols, i=i: e.matmul(pbank[bk][:, cols], lhsT=mst_b, rhs=Ob[:, i, :],
                                                                 start=(i == 0), stop=True),
                  reads=[B_r], writes=[B_pb[bk]])
        sc.op("dve", lambda e: e.tensor_copy(out=cum[:, 0:8, :], in_=pbank[4][:].rearrange("p (t e) -> p t e", e=64)),
              reads=[B_pb[4], B_r], writes=[B_r])
        sc.op("dve", lambda e: e.tensor_copy(out=cum[:, 8:16, :], in_=pbank[5][:].rearrange("p (t e) -> p t e", e=64)),
              reads=[B_pb[5], B_r], writes=[B_r])
        R("pool", lambda e: e.iota(iota_i, pattern=[[1, 64]], base=0, channel_multiplier=0))
        R("pool", lambda e: e.iota(tokid, pattern=[[128, 16]], base=0, channel_multiplier=1))
        R("pool", lambda e: e.iota(fill_i, pattern=[[0, 65]], base=2048, channel_multiplier=0))
        R("dve", lambda e: e.tensor_copy(out=iota_e, in_=iota_i))
        for jj, Of in ((0, O1f), (1, O2f)):
            R("dve", lambda e, Of=Of: e.tensor_tensor(out=tmp64, in0=Of, in1=cum, op=ALU.mult))
            R("dve", lambda e, jj=jj: e.tensor_reduce(out=slot_[:, :, jj], in_=tmp64, axis=AX.X, op=ALU.add))
            R("dve", lambda e, Of=Of: e.tensor_tensor(out=tmp64, in0=Of, in1=iota_e.unsqueeze(1).to_broadcast([128, 16, 64]),
                                                     op=ALU.mult))
            R("dve", lambda e, jj=jj: e.tensor_reduce(out=eid_[:, :, jj], in_=tmp64, axis=AX.X, op=ALU.add))
        R("dve", lambda e: e.tensor_scalar(out=tixf, in0=slot_, scalar1=128.0, scalar2=None, op0=ALU.min))
        R("dve", lambda e: e.scalar_tensor_tensor(out=tixf, in0=eid_, scalar=129.0, in1=tixf, op0=ALU.mult, op1=ALU.add))
        R("dve", lambda e: e.tensor_scalar(out=rixf, in0=slot_, scalar1=128.0, scalar2=1e6, op0=ALU.is_ge, op1=ALU.mult))
        R("dve", lambda e: e.tensor_tensor(out=rixf, in0=rixf, in1=slot_, op=ALU.add))
        R("dve", lambda e: e.scalar_tensor_tensor(out=rixf, in0=eid_, scalar=128.0, in1=rixf, op0=ALU.mult, op1=ALU.add))
        R("dve", lambda e: e.tensor_scalar(out=rixf, in0=rixf, scalar1=8192.0, scalar2=None, op0=ALU.min))
        R("dve", lambda e: e.tensor_copy(out=tix, in_=tixf))
        R("dve", lambda e: e.tensor_copy(out=rix, in_=rixf))
        B_tabd = Buf("tab_dram")
        sc.dma("sp", lambda e: e.dma_start(out=tab_dram.rearrange("(p c) -> p c", c=65), in_=fill_i), "d_tab",
               reads=[B_r], writes=[B_tabd])
        tab2 = tab_dram.rearrange("(r c) -> r c", c=1)
        for i in range(NT):
            for jj in range(2):
                sc.dma("pool", lambda e, i=i, jj=jj: e.indirect_dma_start(
                    out=tab2, out_offset=bass.IndirectOffsetOnAxis(ap=tix[:, i, jj:jj + 1], axis=0),
                    in_=tokid[:, i:i + 1], in_offset=None), "d_tabs", reads=[B_r, B_tabd], writes=[B_tabd])
        B_tabs = Buf("tabs")
        tab_src = bass.AP(tensor=tab_dram.tensor, offset=tab_dram.offset, ap=[[1, 128], [129, 64]])
        sc.dma("sp", lambda e: e.dma_start(out=tabs, in_=tab_src, allow_slow_non_contiguous=True), "d_tab2",
               reads=[B_tabd], writes=[B_tabs])

        sc.barrier()
        wreg = [slab_base - 16384 + 4096 * k for k in range(6)]
        def wview(k):
            a = arena[:, wreg[k]:wreg[k] + 4096].bitcast(BF16)
            return a
        Wg = [wview(0).rearrange("p (a b) -> p a b", b=512), wview(3).rearrange("p (a b) -> p a b", b=512)]
        Wu = [wview(1).rearrange("p (a b) -> p a b", b=512), wview(4).rearrange("p (a b) -> p a b", b=512)]
        Wd = [wview(2).rearrange("p (a b) -> p a b", b=2048), wview(5).rearrange("p (a b) -> p a b", b=2048)]
        B_W = [Buf("W0"), Buf("W1")]
        xb = [alloc(1024, BF16) for _ in range(2)]
        B_xb = [Buf("xb0"), Buf("xb1")]
        xbT = alloc(1024, BF16, [128, KC, 128])
        B_xbT = Buf("xbT")
        sgu = alloc(512)
        ub = alloc(256, BF16)
        uT = alloc(256, BF16, [128, 4, 128])
        B_sgu, B_ub, B_uT = Buf("sgu"), Buf("ub"), Buf("uT")
        yeb = [alloc(1024, BF16) for _ in range(2)]
        B_yeb = [Buf("yeb0"), Buf("yeb1")]
        NE = 64 if stage == "full" else 2
        for ex in range(NE):
            p_ = ex % 2
            sc.dma("sp", lambda e, ex=ex, p_=p_: e.dma_start(
                out=Wg[p_], in_=wbf_dram[ex, 0].rearrange("(kc p n) -> p kc n", p=128, n=512)),
                f"d_W{p_}", reads=[B_wbf], writes=[B_W[p_]])
            sc.dma("sp", lambda e, ex=ex, p_=p_: e.dma_start(
                out=Wu[p_], in_=wbf_dram[ex, 1].rearrange("(kc p n) -> p kc n", p=128, n=512)),
                f"d_W{p_}", reads=[B_wbf], writes=[B_W[p_]])
            sc.dma("sp", lambda e, ex=ex, p_=p_: e.dma_start(
                out=Wd[p_], in_=wbf_dram[ex, 2].rearrange("(kc p n) -> p kc n", p=128, n=2048)),
                f"d_W{p_}", reads=[B_wbf], writes=[B_W[p_]])
            sc.dma("pool", lambda e, ex=ex, p_=p_: e.indirect_dma_start(
                out=xb[p_], out_offset=None, in_=h2_dram,
                in_offset=bass.IndirectOffsetOnAxis(ap=tabs[:, ex:ex + 1], axis=0)),
                f"d_xb{p_}", reads=[B_tabs, B_h2d], writes=[B_xb[p_]])
            for q2 in range(2):
                bk = q2
                pt = pbank[bk][:].bitcast(BF16)
                for j in range(8):
                    kc = q2 * 8 + j
                    sc.op("pe", lambda e, pt=pt, j=j, kc=kc, p_=p_: e.transpose(
                        out=pt[:, j * 128:(j + 1) * 128], in_=xb[p_][:, kc * 128:(kc + 1) * 128], identity=ident_b),
                        reads=[B_xb[p_], B_const], writes=[B_pb[bk]])
                sc.op("act" if q2 else "dve", (lambda e, pt=pt, q2=q2: e.activation(
                    out=xbT[:, q2 * 8:(q2 + 1) * 8, :], in_=pt.rearrange("p (a b) -> p a b", b=128), func=AF.Copy))
                    if q2 else (lambda e, pt=pt, q2=q2: e.tensor_copy(
                        out=xbT[:, q2 * 8:(q2 + 1) * 8, :], in_=pt.rearrange("p (a b) -> p a b", b=128))),
                    reads=[B_pb[bk]], writes=[B_xbT])
            for kc in range(KC):
                sc.op("pe", lambda e, kc=kc, p_=p_: e.matmul(pbank[2][:], lhsT=xbT[:, kc, :], rhs=Wg[p_][:, kc, :],
                                                            start=(kc == 0), stop=(kc == KC - 1)),
                      reads=[B_xbT, B_W[p_]], writes=[B_pb[2]])
            for kc in range(KC):
                sc.op("pe", lambda e, kc=kc, p_=p_: e.matmul(pbank[3][:], lhsT=xbT[:, kc, :], rhs=Wu[p_][:, kc, :],
                                                            start=(kc == 0), stop=(kc == KC - 1)),
                      reads=[B_xbT, B_W[p_]], writes=[B_pb[3]])
            sc.op("act", lambda e: e.activation(out=sgu, in_=pbank[2][:], func=AF.Silu), reads=[B_pb[2]], writes=[B_sgu])
            sc.op("dve", lambda e: e.tensor_tensor(out=ub, in0=sgu, in1=pbank[3][:], op=ALU.mult),
                  reads=[B_sgu, B_pb[3]], writes=[B_ub])
            ptu = pbank[0][:].bitcast(BF16)
            for fc in range(4):
                sc.op("pe", lambda e, fc=fc, ptu=ptu: e.transpose(out=ptu[:, fc * 128:(fc + 1) * 128],
                                                                 in_=ub[:, fc * 128:(fc + 1) * 128], identity=ident_b),
                      reads=[B_ub, B_const], writes=[B_pb[0]])
            sc.op("act", lambda e, ptu=ptu: e.activation(out=uT, in_=ptu[:, 0:512].rearrange("p (a b) -> p a b", b=128), func=AF.Copy),
                  reads=[B_pb[0]], writes=[B_uT])
            for dsl in range(4):
                bk = 4 + dsl
                for fc in range(4):
                    sc.op("pe", lambda e, fc=fc, dsl=dsl, bk=bk, p_=p_: e.matmul(
                        pbank[bk][:], lhsT=uT[:, fc, :], rhs=Wd[p_][:, fc, dsl * 512:(dsl + 1) * 512],
                        start=(fc == 0), stop=(fc == 3)),
                        reads=[B_uT, B_W[p_]], writes=[B_pb[bk]])
                if dsl % 2:
                    sc.op("act", lambda e, dsl=dsl, bk=bk, p_=p_: e.activation(out=yeb[p_][:, dsl * 512:(dsl + 1) * 512],
                                                                              in_=pbank[bk][:], func=AF.Copy),
                          reads=[B_pb[bk]], writes=[B_yeb[p_]])
                else:
                    sc.op("dve", lambda e, dsl=dsl, bk=bk, p_=p_: e.tensor_copy(out=yeb[p_][:, dsl * 512:(dsl + 1) * 512],
                                                                               in_=pbank[bk][:]),
                          reads=[B_pb[bk]], writes=[B_yeb[p_]])
            sc.dma("sp", lambda e, ex=ex, p_=p_: e.dma_start(out=ye_dram[ex * 128:(ex + 1) * 128, :], in_=yeb[p_]),
                   f"d_yeb{p_}", reads=[B_yeb[p_]], writes=[B_yed])

        sc.barrier()
        top[0] = slab_base - 16384
        fx = [alloc(2048) for _ in range(2)]
        fa = [alloc(1024, BF16) for _ in range(2)]
        fb_ = [alloc(1024, BF16) for _ in range(2)]
        fo = [alloc(2048) for _ in range(2)]
        B_fx, B_fa, B_fb, B_fo = ([Buf(f"{n}{k}") for k in range(2)] for n in ("fx", "fa", "fb", "fo"))
        fss = alloc(2)
        B_fs = [Buf("fs0"), Buf("fs1")]
        B_out = Buf("out")
        for i in range(NT):
            p_ = i % 2
            ts = slice(i * 128, (i + 1) * 128)
            sc.dma("sp", lambda e, ts=ts, p_=p_: e.dma_start(out=fx[p_], in_=x2_dram[ts, :]), f"d_fx{p_}",
                   reads=[B_x2d], writes=[B_fx[p_]])
            sc.dma("pool", lambda e, i=i, p_=p_: e.indirect_dma_start(
                out=fa[p_], out_offset=None, in_=ye_dram, in_offset=bass.IndirectOffsetOnAxis(ap=rix[:, i, 0:1], axis=0)),
                f"d_fa{p_}", reads=[B_yed, B_r], writes=[B_fa[p_]])
            sc.dma("pool", lambda e, i=i, p_=p_: e.indirect_dma_start(
                out=fb_[p_], out_offset=None, in_=ye_dram, in_offset=bass.IndirectOffsetOnAxis(ap=rix[:, i, 1:2], axis=0)),
                f"d_fb{p_}", reads=[B_yed, B_r], writes=[B_fb[p_]])
            sc.op("dve", lambda e, i=i, p_=p_: e.scalar_tensor_tensor(out=fx[p_], in0=fa[p_], scalar=gates[:, i, 0:1], in1=fx[p_],
                                                                      op0=ALU.mult, op1=ALU.add),
                  reads=[B_fa[p_], B_fx[p_], B_r], writes=[B_fx[p_]])
            sc.op("dve", lambda e, i=i, p_=p_: e.scalar_tensor_tensor(out=fx[p_], in0=fb_[p_], scalar=gates[:, i, 1:2], in1=fx[p_],
                                                                      op0=ALU.mult, op1=ALU.add),
                  reads=[B_fb[p_], B_fx[p_], B_r], writes=[B_fx[p_]])
            sc.op("act", lambda e, p_=p_: e.activation(out=fo[p_], in_=fx[p_], func=AF.Square, accum_out=fss[:, p_:p_ + 1]),
                  reads=[B_fx[p_]], writes=[B_fo[p_], B_fs[p_]])
            sc.op("act", lambda e, p_=p_: e.activation(out=fss[:, p_:p_ + 1], in_=fss[:, p_:p_ + 1], func=AF.Sqrt, scale=1.0 / D,
                                                       bias=epsc[:, 0:1]),
                  reads=[B_fs[p_], B_const], writes=[B_fs[p_]])
            sc.op("dve", lambda e, p_=p_: e.reciprocal(out=fss[:, p_:p_ + 1], in_=fss[:, p_:p_ + 1]), reads=[B_fs[p_]], writes=[B_fs[p_]])
            sc.op("dve", lambda e, p_=p_: e.scalar_tensor_tensor(out=fo[p_], in0=fx[p_], scalar=fss[:, p_:p_ + 1], in1=gfb,
                                                                 op0=ALU.mult, op1=ALU.mult),
                  reads=[B_fx[p_], B_fs[p_], B_gb], writes=[B_fo[p_]])
            sc.dma("sp", lambda e, ts=ts, p_=p_: e.dma_start(out=out[ts, :], in_=fo[p_]), f"d_fo{p_}",
                   reads=[B_fo[p_]], writes=[B_out])
        fin = [B_out]

    sc.wait_all("sp", fin)

    names = sc.sem_names()
    sem_cms = [nc.semaphore(n) for n in names]
    for n, cm in zip(names, sem_cms):
        sc.sems[n] = cm.__enter__()
    with nc.Block() as block:
        sc.emit(block)
    for cm in reversed(sem_cms):
        cm.__exit__(None, None, None)
    for cm in reversed(ctxs):
        cm.__exit__(None, None, None)
    print("instr counts:", sc.cnt, "dma:", sc.ndma, "sems:", len(names), "arena top:", top[0])
    return nc


_IN_NAMES = ("norm1_gain", "w_in", "hg_norm_gain", "w_branch_a", "w_branch_b", "w_out", "norm2_gain",
             "w_router_group", "b_router_group", "w_router_expert", "b_router_expert",
             "w_exp_gate", "w_exp_up", "w_exp_down")


def make_in_map(inputs, c):
    m = {"x": np.ascontiguousarray(inputs["x"][c])}
    for k in _IN_NAMES:
        m[k] = np.ascontiguousarray(np.asarray(inputs[k])[0])
    m["hg_lb_logits"] = np.ascontiguousarray(inputs["hg_lb_logits"])
    m["rel_bias"] = np.ascontiguousarray(inputs["rel_bias"])
    m["final_norm_gain"] = np.ascontiguousarray(inputs["final_norm_gain"])
    m["att_oh"] = _att_onehot()
    return m


def kernel(**inputs):
    n = 8
    nc = build_nc()
    inputs = {k: np.asarray(v) for k, v in inputs.items()}
    in_maps = [make_in_map(inputs, c) for c in range(n)]
    res = run_bass_kernel_spmd(nc, in_maps, core_ids=list(range(n)))
    return np.stack([np.asarray(r["out"]) for r in res.results], axis=0).astype(np.float32)
```

```python
import numpy as np
import concourse.bass as bass
import concourse.mybir as mybir
from concourse.bass_utils import run_bass_kernel_spmd

F32 = mybir.dt.float32
BF16 = mybir.dt.bfloat16
I32 = mybir.dt.int32
AF = mybir.ActivationFunctionType
ALU = mybir.AluOpType
AX = mybir.AxisListType

D = 2048
S = 2048
NT = S // 128
KC = D // 128
INW = 16896
EPS = 1e-6
NEG = -30000.0
ENGS = ("pe", "act", "dve", "pool", "sp")


class Buf:
    __slots__ = ("name", "w", "r")

    def __init__(self, name):
        self.name = name
        self.w = None
        self.r = {}


class Sched:
    def __init__(self, nc):
        self.nc = nc
        self.q = {e: [] for e in ENGS}
        self.cnt = {e: 0 for e in ENGS}
        self.seen = {e: {} for e in ENGS}
        self.dma_cnt = {}
        self.sems = {}
        self.ndma = 0

    def _deps(self, eng, reads, writes):
        deps = {}
        def add(ev):
            if ev is None:
                return
            k, v = ev
            if deps.get(k, 0) < v:
                deps[k] = v
        for b in reads:
            add(b.w)
        for b in writes:
            add(b.w)
            for k, v in b.r.items():
                add((k, v))
        for k, v in deps.items():
            if k == "pe" and eng == "pe":
                continue
            if self.seen[eng].get(k, 0) >= v:
                continue
            self.seen[eng][k] = v
            self.q[eng].append(("wait", k, v))

    def _mark(self, ev, reads, writes):
        for b in writes:
            b.w = ev
            b.r = {}
        for b in reads:
            if b.r.get(ev[0], 0) < ev[1]:
                b.r[ev[0]] = ev[1]

    def op(self, eng, fn, reads=(), writes=()):
        self._deps(eng, reads, writes)
        self.cnt[eng] += 1
        ev = (eng, self.cnt[eng])
        self.q[eng].append(("op", fn, eng, 1))
        self._mark(ev, reads, writes)
        return ev

    def dma(self, eng, fn, sem, reads=(), writes=()):
        self._deps(eng, reads, writes)
        self.dma_cnt[sem] = self.dma_cnt.get(sem, 0) + 16
        ev = (sem, self.dma_cnt[sem])
        self.q[eng].append(("op", fn, sem, 16))
        self._mark(ev, reads, writes)
        self.ndma += 1
        return ev

    def barrier(self):
        keys = list(ENGS) + sorted(self.dma_cnt.keys())
        for eng in ENGS:
            for k in keys:
                if k == eng:
                    continue
                v = self.cnt[k] if k in self.cnt else self.dma_cnt[k]
                if v > self.seen[eng].get(k, 0):
                    self.seen[eng][k] = v
                    self.q[eng].append(("wait", k, v))

    def wait_all(self, eng, bufs):
        self._deps(eng, bufs, ())

    def sem_names(self):
        return list(ENGS) + sorted(self.dma_cnt.keys())

    def emit(self, block):
        nc = self.nc
        sems = self.sems

        def run(engname):
            def body(e):
                for it in self.q[engname]:
                    if it[0] == "wait":
                        e.wait_ge(sems[it[1]], it[2])
                    else:
                        ins = it[1](e)
                        ins.then_inc(sems[it[2]], it[3])
            return body
        block.tensor(run("pe"))
        block.scalar(run("act"))
        block.vector(run("dve"))
        block.gpsimd(run("pool"))
        block.sync(run("sp"))


def _t5_bucket_np(dist):
    dist = np.asarray(dist, dtype=np.int64)
    exact = 16
    d_f = np.maximum(dist, 1).astype(np.float32)
    lb = exact + (np.log(d_f / np.float32(exact)) / np.float32(np.log(2048 / exact)) * np.float32(32 - exact)).astype(np.int32)
    return np.where(dist < exact, dist, np.minimum(lb, 31))


def _att_onehot():
    groups = ((128, 1), (512, 4), (2048, 16))
    JW = (256, 640, 2048)
    cols = []
    for (win, dil), J in zip(groups, JW):
        W = J + 127
        dist = np.arange(W) - 127
        valid = (dist >= 0) & (dist % dil == 0) & (dist <= win)
        b = _t5_bucket_np(np.maximum(dist, 0))
        oh = np.zeros((33, W), np.float32)
        oh[b[valid], np.nonzero(valid)[0]] = 1.0
        oh[32, ~valid] = 1.0
        cols.append(oh)
    return np.ascontiguousarray(np.concatenate(cols, axis=1))
def build_nc(stage="full"):
    nc = bass.Bass("TRN2", target_bir_lowering=False)
    dr = lambda name, shape, dt, kind: nc.dram_tensor(name, shape, dt, kind=kind).ap()
    x = dr("x", [S, D], F32, "ExternalInput")
    norm1_gain = dr("norm1_gain", [D], F32, "ExternalInput")
    w_in = dr("w_in", [D, INW], F32, "ExternalInput")
    hg_lb_logits = dr("hg_lb_logits", [2, D], F32, "ExternalInput")
    hg_norm_gain = dr("hg_norm_gain", [D], F32, "ExternalInput")
    rel_bias = dr("rel_bias", [32, 12], F32, "ExternalInput")
    att_oh = dr("att_oh", [33, 3325], F32, "ExternalInput")
    w_branch_a = dr("w_branch_a", [D, D], F32, "ExternalInput")
    w_branch_b = dr("w_branch_b", [512, D], F32, "ExternalInput")
    w_out = dr("w_out", [D, D], F32, "ExternalInput")
    norm2_gain = dr("norm2_gain", [D], F32, "ExternalInput")
    final_norm_gain = dr("final_norm_gain", [D], F32, "ExternalInput")
    w_router_group = dr("w_router_group", [D, 8], F32, "ExternalInput")
    b_router_group = dr("b_router_group", [1, 8], F32, "ExternalInput")
    w_router_expert = dr("w_router_expert", [D, 64], F32, "ExternalInput")
    b_router_expert = dr("b_router_expert", [1, 64], F32, "ExternalInput")
    w_exp_gate = dr("w_exp_gate", [64, D, 512], F32, "ExternalInput")
    w_exp_up = dr("w_exp_up", [64, D, 512], F32, "ExternalInput")
    w_exp_down = dr("w_exp_down", [64, 512, D], F32, "ExternalInput")
    out = dr("out", [S, D], F32, "ExternalOutput")

    def scratch(name, shape, dt):
        kind = "ExternalOutput" if stage == "dbg_" + name else "Internal"
        return dr(name, shape, dt, kind)
    sg_dram = scratch("sg", [2 * D, S], BF16)
    ya_dram = scratch("ya", [D, S], BF16)

    yb_dram = scratch("yb", [512, S], BF16)
    zrow_dram = scratch("zrow", [12 * 128 * 2176], BF16)
    x2_dram = scratch("x2", [S, D], F32)
    h2_dram = scratch("h2", [S + 1, D], BF16)
    ye_dram = scratch("ye", [8192 + 128, D], BF16)
    tab_dram = scratch("tab", [8320], I32)
    wbf_parts = [scratch(f"wbf{k}", [16, 3, 2048 * 512], BF16) for k in range(4)]

    class _Wbf:
        def __getitem__(self, key):
            e_, m_ = key
            return wbf_parts[e_ // 16][e_ % 16, m_]
    wbf_dram = _Wbf()
    sc = Sched(nc)
    ctxs = []

    def sb(name, shape, dt):
        cm = nc.sbuf_tensor(name, shape, dt)
        t = cm.__enter__()
        ctxs.append(cm)
        return t

    def ps(name, shape, dt):
        cm = nc.psum_tensor(name, shape, dt)
        t = cm.__enter__()
        ctxs.append(cm)
        return t

    ARENA = 53100
    arena = sb("arena", [128, ARENA], F32)
    top = [0]

    def alloc(words, dt=F32, shape=None):
        o = top[0]
        top[0] += words
        assert top[0] <= ARENA, ("arena overflow", top[0])
        a = arena[:, o:o + words]
        if dt != F32:
            a = a.bitcast(dt)
        if shape is not None and len(shape) == 3:
            a = a.rearrange("p (a b) -> p a b", b=shape[2])
        return a

    pbank = [ps(f"pb{i}", [128, 512], F32) for i in range(8)]
    B_pb = [Buf(f"pb{i}") for i in range(8)]

    ident_f = alloc(128)
    ident_b = alloc(64, BF16)
    mask_ut = alloc(128)
    B_const = Buf("const")
    sc.op("pool", lambda e: e.memset(ident_f, 1.0), writes=[B_const])
    sc.op("pool", lambda e: e.affine_select(out=ident_f, in_=ident_f, pattern=[[-1, 128]],
                                            compare_op=ALU.is_equal, fill=0.0, base=0, channel_multiplier=1),
          reads=[B_const], writes=[B_const])
    sc.op("dve", lambda e: e.tensor_copy(out=ident_b, in_=ident_f), reads=[B_const], writes=[B_const])
    sc.op("pool", lambda e: e.memset(mask_ut, 1.0), writes=[B_const])
    sc.op("pool", lambda e: e.affine_select(out=mask_ut, in_=mask_ut, pattern=[[1, 128]],
                                            compare_op=ALU.is_ge, fill=0.0, base=0, channel_multiplier=-1),
          reads=[B_const], writes=[B_const])
    g1 = alloc(KC)
    epsc = alloc(1)
    zeros512 = alloc(512)
    lbl = alloc(32, F32, [128, 2, 16])
    lbT = alloc(16)
    omlb = alloc(16)
    nomlb = alloc(16)
    gnT = alloc(16)
    sc.dma("sp", lambda e: e.dma_start(out=g1, in_=norm1_gain.rearrange("(kc p) -> p kc", p=128),
                                       allow_slow_non_contiguous=True), "d_const", writes=[B_const])
    sc.dma("sp", lambda e: e.dma_start(out=gnT, in_=hg_norm_gain.rearrange("(kc p) -> p kc", p=128),
                                       allow_slow_non_contiguous=True), "d_const", writes=[B_const])
    sc.dma("sp", lambda e: e.dma_start(out=lbl, in_=hg_lb_logits.rearrange("r (h p) -> p r h", p=128),
                                       allow_slow_non_contiguous=True), "d_const", writes=[B_const])
    sc.op("dve", lambda e: e.memset(epsc, EPS), writes=[B_const])
    sc.op("dve", lambda e: e.memset(zeros512, 0.0), writes=[B_const])
    sc.op("dve", lambda e: e.tensor_tensor(out=lbT, in0=lbl[:, 0, :], in1=lbl[:, 1, :], op=ALU.subtract),
          reads=[B_const], writes=[B_const])
    sc.op("act", lambda e: e.activation(out=omlb, in_=lbT, func=AF.Sigmoid, scale=-1.0), reads=[B_const], writes=[B_const])
    sc.op("act", lambda e: e.activation(out=lbT, in_=lbT, func=AF.Sigmoid), reads=[B_const], writes=[B_const])
    sc.op("dve", lambda e: e.tensor_scalar(out=nomlb, in0=omlb, scalar1=-1.0, scalar2=None, op0=ALU.mult),
          reads=[B_const], writes=[B_const])

    hT = alloc(16384, BF16, [128, KC, S])
    B_hT = [Buf(f"hT{i}") for i in range(NT)]
    NSLAB = 3
    slab_base = top[0]
    slab = [alloc(4096, BF16, [128, KC, 512]) for i in range(NSLAB)]
    B_slab = [Buf(f"slab{i}") for i in range(NSLAB)]
    phase_base = top[0]

    xt = [alloc(2048) for i in range(2)]
    B_xt = [Buf(f"xt{i}") for i in range(2)]
    junk = alloc(1024, BF16)
    B_junk = Buf("junk")
    ssq = alloc(2)
    rstd = alloc(2)
    B_st = [Buf("st0"), Buf("st1")]
    xn = [alloc(1024, BF16) for i in range(2)]
    B_xn = [Buf("xn0"), Buf("xn1")]

    for i in range(NT):
        s_ = i % 2
        sc.dma("sp", lambda e, i=i, s_=s_: e.dma_start(out=xt[s_], in_=x[i * 128:(i + 1) * 128, :]),
               f"d_xt{s_}", writes=[B_xt[s_]])
        sc.op("act", lambda e, s_=s_: e.activation(out=junk, in_=xt[s_], func=AF.Square,
                                                   accum_out=ssq[:, s_:s_ + 1]),
              reads=[B_xt[s_]], writes=[B_junk, B_st[s_]])
        sc.op("act", lambda e, s_=s_: e.activation(out=rstd[:, s_:s_ + 1], in_=ssq[:, s_:s_ + 1], func=AF.Sqrt,
                                                   scale=1.0 / D, bias=epsc[:, 0:1]),
              reads=[B_st[s_], B_const], writes=[B_st[s_]])
        sc.op("dve", lambda e, s_=s_: e.reciprocal(out=rstd[:, s_:s_ + 1], in_=rstd[:, s_:s_ + 1]),
              reads=[B_st[s_]], writes=[B_st[s_]])
        sc.op("dve", lambda e, s_=s_: e.tensor_scalar(out=xn[s_], in0=xt[s_], scalar1=rstd[:, s_:s_ + 1],
                                                      scalar2=None, op0=ALU.mult),
              reads=[B_xt[s_], B_st[s_]], writes=[B_xn[s_]])
        for q4 in range(4):
            bk = (i * 4 + q4) % 4
            pt = pbank[bk][:].bitcast(BF16)
            for j in range(4):
                kc = q4 * 4 + j
                sc.op("pe", lambda e, pt=pt, j=j, kc=kc, s_=s_: e.transpose(
                    out=pt[:, j * 128:(j + 1) * 128], in_=xn[s_][:, kc * 128:(kc + 1) * 128], identity=ident_b),
                    reads=[B_xn[s_], B_const], writes=[B_pb[bk]])
            for j in range(4):
                kc = q4 * 4 + j
                sc.op("act", lambda e, pt=pt, j=j, kc=kc, i=i: e.activation(
                    out=hT[:, kc, i * 128:(i + 1) * 128], in_=pt[:, j * 128:(j + 1) * 128],
                    func=AF.Copy, scale=g1[:, kc:kc + 1]),
                    reads=[B_pb[bk], B_const], writes=[B_hT[i]])

    slab_ctr = [0]
    nslab = [NSLAB]

    def load_slab(pieces, kc_n=KC):
        s_ = slab_ctr[0] % nslab[0]
        slab_ctr[0] += 1
        for ap_, c0, n in pieces:
            sc.dma("pool", lambda e, ap_=ap_, c0=c0, n=n: e.dma_start(
                out=slab[s_][:, 0:kc_n, c0:c0 + n], in_=ap_.rearrange("(kc p) n -> p kc n", p=128)),
                f"d_slab{s_}", writes=[B_slab[s_]])
        return s_

    cvt_list = [(e_, m_) for e_ in range(64) for m_ in range(3)] if stage == "full" else []
    B_wbf = Buf("wbf")
    w_exp = (w_exp_gate, w_exp_up, w_exp_down)

    def issue_cvt(n):
        for _ in range(n):
            if not cvt_list:
                return
            e_, m_ = cvt_list.pop(0)
            sc.dma("pool", lambda e, e_=e_, m_=m_: e.dma_start(
                out=wbf_dram[e_, m_].rearrange("(a b) -> a b", b=w_exp[m_].shape[2]), in_=w_exp[m_][e_]),
                "d_cvt", writes=[B_wbf])

    pb_ctr = [0]

    def next_bank(lo=0, n=4):
        b = lo + pb_ctr[0] % n
        pb_ctr[0] += 1
        return b

    def mm_fm(s_, c0, tc, bk):
        for kc in range(KC):
            sc.op("pe", lambda e, kc=kc: e.matmul(
                pbank[bk][:], lhsT=slab[s_][:, kc, c0:c0 + 128],
                rhs=hT[:, kc, tc * 512:(tc + 1) * 512], start=(kc == 0), stop=(kc == KC - 1)),
                reads=[B_slab[s_]] + B_hT[tc * 4:(tc + 1) * 4], writes=[B_pb[bk]])

    def mm_tm(s_, c0, n, i, bk):
        for kc in range(KC):
            sc.op("pe", lambda e, kc=kc: e.matmul(
                pbank[bk][:, 0:n], lhsT=hT[:, kc, i * 128:(i + 1) * 128],
                rhs=slab[s_][:, kc, c0:c0 + n], start=(kc == 0), stop=(kc == KC - 1)),
                reads=[B_slab[s_], B_hT[i]], writes=[B_pb[bk]])

    sc.barrier()
    top[0] = phase_base
    GA0 = 4 * 2048 + 3 * 1536
    sgrow = [alloc(1024, BF16) for i in range(2)]
    B_sgrow = [Buf("sgrow0"), Buf("sgrow1")]
    B_sgd = Buf("sg_dram")
    rowctr = 0
    if stage not in ("dbg_ya", "dbg_yb"):
        for sl in range(8):
            s_ = load_slab([(w_in[:, GA0 + sl * 512:GA0 + (sl + 1) * 512], 0, 512)])
            issue_cvt(3)
            for fb in range(4):
                r_ = rowctr % 2
                rowctr += 1
                for tc in range(4):
                    bk = next_bank()
                    mm_fm(s_, fb * 128, tc, bk)
                    sc.op("act", lambda e, bk=bk, r_=r_, tc=tc: e.activation(
                        out=sgrow[r_][:, tc * 512:(tc + 1) * 512], in_=pbank[bk][:], func=AF.Sigmoid),
                        reads=[B_pb[bk]], writes=[B_sgrow[r_]])
                n0 = sl * 512 + fb * 128
                sc.dma("sp", lambda e, r_=r_, n0=n0: e.dma_start(out=sg_dram[n0:n0 + 128, :], in_=sgrow[r_]),
                       f"d_sgrow{r_}", reads=[B_sgrow[r_]], writes=[B_sgd])

    sc.barrier()
    top[0] = phase_base
    NH = 16 if stage not in ("dbg_ya", "dbg_yb") else (3 if stage == "dbg_ya" else 0)
    qT = [alloc(1024, BF16) for _ in range(2)]
    ktT = [alloc(1024, BF16) for _ in range(2)]
    itok = [alloc(1024, BF16, [128, NT, 128]) for _ in range(2)]
    sgtok = [alloc(1024, BF16, [128, NT, 128]) for _ in range(2)]
    B_qT = [[Buf(f"qT{s}_{c}") for c in range(4)] for s in range(2)]
    B_ktT = [[Buf(f"ktT{s}_{c}") for c in range(4)] for s in range(2)]
    B_itok = [[Buf(f"itok{s}_{c}") for c in range(NT)] for s in range(2)]
    B_sgtok = [[Buf(f"sgtok{s}_{c}") for c in range(NT)] for s in range(2)]
    Bc = alloc(2048)
    B_Bc = Buf("Bc")
    t_sig, t_kk, t_lf, t_b, t_eq, t_ek = [alloc(512) for _ in range(6)]
    B_t = {n: Buf(n) for n in ("sig", "kk", "lf", "b", "eq", "ek")}
    Dm = [alloc(16) for _ in range(2)]
    B_Dm = [Buf("Dm0"), Buf("Dm1")]
    dref = alloc(16)
    Sf = alloc(128)
    Stmp = alloc(128)
    Sbf = alloc(64, BF16)
    B_S = Buf("S")
    B_Sbf = Buf("Sbf")
    sctmp = alloc(128)
    scT = alloc(64, BF16)
    B_scT = Buf("scT")
    ktok = alloc(64, BF16)
    B_ktok = Buf("ktok")
    hss = alloc(1)
    hrs = alloc(1)
    B_hs = Buf("hs")
    hjunk = alloc(64, BF16)
    ytok = alloc(64, BF16)
    B_ytok = Buf("ytok")
    yaT = [alloc(1024, BF16) for _ in range(2)]
    B_yaT = [Buf("yaT0"), Buf("yaT1")]
    B_yad = Buf("ya_dram")
    pS_sc = pbank[4][:, 0:128]
    pS_tr = pbank[5][:].bitcast(BF16)
    pS_o = pbank[6][:, 0:128]
    pS_st = pbank[7][:, 0:128]
    B_psc, B_ptr1, B_ptr2, B_po, B_pst = Buf("psc"), Buf("ptr1"), Buf("ptr2"), Buf("po"), Buf("pst")

    def hg_inproj_units(h):
        sl = h % 2
        units = []
        holder = {}

        def u_load():
            holder["s"] = load_slab([(w_in[:, h * 128:(h + 1) * 128], 0, 128),
                                     (w_in[:, 2048 + h * 128:2048 + (h + 1) * 128], 128, 128),
                                     (w_in[:, 4096 + h * 128:4096 + (h + 1) * 128], 256, 128),
                                     (w_in[:, 6144 + h * 128:6144 + (h + 1) * 128], 384, 128)])
            issue_cvt(7)
        units.append(u_load)

        def u_q(tc):
            def f():
                s_ = holder["s"]
                bk = next_bank()
                mm_fm(s_, 0, tc, bk)
                sc.op("act", lambda e: e.activation(out=qT[sl][:, tc * 512:(tc + 1) * 512], in_=pbank[bk][:],
                                                    func=AF.Copy),
                      reads=[B_pb[bk]], writes=[B_qT[sl][tc]])
            return f

        def u_f(tc):
            def f():
                s_ = holder["s"]
                bk = next_bank()
                mm_fm(s_, 128, tc, bk)
                cs = slice(tc * 512, (tc + 1) * 512)
                sc.op("act", lambda e: e.activation(out=t_sig, in_=pbank[bk][:], func=AF.Exp, scale=-1.0),
                      reads=[B_pb[bk]], writes=[B_t["sig"]])
                sc.op("dve", lambda e: e.tensor_scalar(out=t_sig, in0=t_sig, scalar1=1.0, scalar2=None, op0=ALU.add),
                      reads=[B_t["sig"]], writes=[B_t["sig"]])
                sc.op("dve", lambda e: e.reciprocal(out=t_sig, in_=t_sig), reads=[B_t["sig"]], writes=[B_t["sig"]])
                sc.op("dve", lambda e: e.tensor_scalar(out=t_kk, in0=t_sig, scalar1=nomlb[:, h:h + 1],
                                                       scalar2=omlb[:, h:h + 1], op0=ALU.mult, op1=ALU.add),
                      reads=[B_t["sig"], B_const], writes=[B_t["kk"]])
                sc.op("act", lambda e: e.activation(out=t_lf, in_=t_sig, func=AF.Ln, scale=omlb[:, h:h + 1],
                                                    bias=lbT[:, h:h + 1]),
                      reads=[B_t["sig"], B_const], writes=[B_t["lf"]])
                init = 0.0 if tc == 0 else Bc[:, tc * 512 - 1:tc * 512]
                sc.op("dve", lambda e: e.tensor_tensor_scan(out=Bc[:, cs], data0=t_lf, data1=zeros512,
                                                            initial=init, op0=ALU.add, op1=ALU.add),
                      reads=[B_t["lf"], B_const, B_Bc], writes=[B_Bc])
                bview = Bc[:, cs].rearrange("p (a b) -> p a b", b=128)
                sc.op("dve", lambda e: e.tensor_tensor(
                    out=t_b.rearrange("p (a b) -> p a b", b=128), in0=bview,
                    in1=bview[:, :, 63:64].to_broadcast([128, 4, 128]), op=ALU.subtract),
                    reads=[B_Bc], writes=[B_t["b"]])
                sc.op("act", lambda e: e.activation(out=t_eq, in_=t_b, func=AF.Exp), reads=[B_t["b"]], writes=[B_t["eq"]])
                sc.op("act", lambda e: e.activation(out=t_ek, in_=t_b, func=AF.Exp, scale=-1.0),
                      reads=[B_t["b"]], writes=[B_t["ek"]])
                sc.op("dve", lambda e: e.tensor_tensor(out=qT[sl][:, cs], in0=qT[sl][:, cs], in1=t_eq, op=ALU.mult),
                      reads=[B_t["eq"], B_qT[sl][tc]], writes=[B_qT[sl][tc]])
                sc.op("dve", lambda e: e.tensor_tensor(out=ktT[sl][:, cs], in0=t_kk, in1=t_ek, op=ALU.mult),
                      reads=[B_t["ek"], B_t["kk"]], writes=[B_ktT[sl][tc]])
                if tc == 3:
                    refs = Bc.rearrange("p (a b) -> p a b", b=128)[:, :, 63]
                    sc.op("dve", lambda e: e.tensor_tensor(out=dref[:, 0:15], in0=refs[:, 1:16], in1=refs[:, 0:15],
                                                           op=ALU.subtract),
                          reads=[B_Bc], writes=[B_t["b"]])
                    sc.op("act", lambda e: e.activation(out=Dm[sl][:, 0:15], in_=dref[:, 0:15], func=AF.Exp),
                          reads=[B_t["b"]], writes=[B_Dm[sl]])
            return f

        def u_ig(i):
            def f():
                s_ = holder["s"]
                bk = next_bank()
                mm_tm(s_, 256, 256, i, bk)
                sc.op("act", lambda e: e.activation(out=itok[sl][:, i, :], in_=pbank[bk][:, 0:128], func=AF.Copy),
                      reads=[B_pb[bk]], writes=[B_itok[sl][i]])
                sc.op("act", lambda e: e.activation(out=sgtok[sl][:, i, :], in_=pbank[bk][:, 128:256], func=AF.Exp, scale=-1.0),
                      reads=[B_pb[bk]], writes=[B_sgtok[sl][i]])
                if i == NT - 1:
                    sc.op("dve", lambda e: e.tensor_scalar(out=sgtok[sl], in0=sgtok[sl], scalar1=1.0, scalar2=None, op0=ALU.add),
                          reads=B_sgtok[sl], writes=B_sgtok[sl])
                    def _rcp(e):
                        with nc.allow_low_precision("bf16 storage of the sigmoid gate"):
                            return e.reciprocal(out=sgtok[sl], in_=sgtok[sl])
                    sc.op("dve", _rcp, reads=B_sgtok[sl], writes=B_sgtok[sl])
            return f
        for tc in range(4):
            units.append(u_q(tc))
        for tc in range(4):
            units.append(u_f(tc))
        for i in range(NT):
            units.append(u_ig(i))
        return units

    scT2 = [scT, alloc(64, BF16)]
    ktok2 = [ktok, alloc(64, BF16)]
    ytok2 = [ytok, alloc(64, BF16)]
    sctmp2 = [sctmp, alloc(128)]
    hss2 = [hss, alloc(1)]
    hrs2 = [hrs, alloc(1)]
    B_scT2 = [Buf("scT0"), Buf("scT1")]
    B_ktok2 = [Buf("ktok0"), Buf("ktok1")]
    B_ytok2 = [Buf("ytok0"), Buf("ytok1")]
    B_hs2 = [Buf("hs0"), Buf("hs1")]
    pSsc2 = [pbank[4][:, 0:128], pbank[4][:, 128:256]]
    pStr_k = [pS_tr[:, 0:128], pS_tr[:, 128:256]]
    pStr_y = [pS_tr[:, 256:384], pS_tr[:, 384:512]]
    pSo2 = [pbank[6][:, 0:128], pbank[6][:, 128:256]]
    pSst2 = [pbank[7][:, 0:128], pbank[7][:, 128:256]]
    pStr_y = [pbank[7][:].bitcast(BF16)[:, 0:128], pbank[7][:].bitcast(BF16)[:, 128:256]]
    pSst2 = [pbank[6][:, 0:128], pbank[6][:, 128:256]]
    pSo2 = [pbank[5][:, 0:128], pbank[5][:, 128:256]]
    pStr_k = [pbank[4][:].bitcast(BF16)[:, 512:640], pbank[4][:].bitcast(BF16)[:, 640:768]]
    B_psc2 = [B_pb[4], B_pb[4]]
    B_ptrk = [B_pb[4], B_pb[4]]
    B_po2 = [B_pb[5], B_pb[5]]
    B_pst2 = [B_pb[6], B_pb[6]]
    B_ptry = [B_pb[7], B_pb[7]]

    def hg_rec_steps(h):
        sl = h % 2

        def stA(i):
            d = i % 2
            ts = slice(i * 128, (i + 1) * 128)
            tc = i // 4
            sc.op("pe", lambda e: e.matmul(pSsc2[d], lhsT=ktT[sl][:, ts], rhs=qT[sl][:, ts], start=True, stop=True),
                  reads=[B_ktT[sl][tc], B_qT[sl][tc]], writes=[B_psc2[d]])
            if i < NT - 1:
                sc.op("pe", lambda e: e.transpose(out=pStr_k[d], in_=ktT[sl][:, ts], identity=ident_b),
                      reads=[B_ktT[sl][tc], B_const], writes=[B_ptrk[d]])
            sc.op("dve", lambda e: e.tensor_scalar(out=sctmp2[d], in0=pSsc2[d], scalar1=1e30, scalar2=-1e30,
                                                   op0=ALU.min, op1=ALU.max),
                  reads=[B_psc2[d]], writes=[B_scT2[d], B_psc2[d]])
            sc.op("dve", lambda e: e.tensor_tensor(out=scT2[d], in0=sctmp2[d], in1=mask_ut, op=ALU.mult),
                  reads=[B_scT2[d], B_const], writes=[B_scT2[d]])
            if i < NT - 1:
                sc.op("act", lambda e: e.activation(out=ktok2[d], in_=pStr_k[d], func=AF.Copy),
                      reads=[B_ptrk[d]], writes=[B_ktok2[d], B_ptrk[d]])

        def stB(i):
            d = i % 2
            ts = slice(i * 128, (i + 1) * 128)
            tc = i // 4
            sc.op("pe", lambda e: e.matmul(pSo2[d], lhsT=scT2[d], rhs=itok[sl][:, i, :], start=True, stop=(i == 0)),
                  reads=[B_scT2[d], B_itok[sl][i]], writes=[B_po2[d]])
            if i > 0:
                sc.op("pe", lambda e: e.matmul(pSo2[d], lhsT=qT[sl][:, ts], rhs=Sbf, start=False, stop=True),
                      reads=[B_qT[sl][tc], B_Sbf], writes=[B_po2[d]])
            if i < NT - 1:
                sc.op("pe", lambda e: e.matmul(pSst2[d], lhsT=ktok2[d], rhs=itok[sl][:, i, :], start=True, stop=True),
                      reads=[B_ktok2[d], B_itok[sl][i]], writes=[B_pst2[d]])
                if i == 0:
                    sc.op("dve", lambda e: e.tensor_scalar(out=Sf, in0=pSst2[d], scalar1=Dm[sl][:, 0:1], scalar2=None,
                                                           op0=ALU.mult),
                          reads=[B_pst2[d], B_Dm[sl]], writes=[B_S, B_pst2[d]])
                else:
                    sc.op("dve", lambda e: e.tensor_scalar(out=Stmp, in0=Sf, scalar1=Dm[sl][:, i:i + 1], scalar2=None,
                                                           op0=ALU.mult),
                          reads=[B_S, B_Dm[sl]], writes=[B_S])
                    sc.op("dve", lambda e: e.scalar_tensor_tensor(out=Sf, in0=pSst2[d], scalar=Dm[sl][:, i:i + 1],
                                                                  in1=Stmp, op0=ALU.mult, op1=ALU.add),
                          reads=[B_pst2[d], B_S, B_Dm[sl]], writes=[B_S, B_pst2[d]])
                sc.op("act", lambda e: e.activation(out=Sbf, in_=Sf, func=AF.Copy), reads=[B_S], writes=[B_Sbf])
            sc.op("act", lambda e: e.activation(out=hjunk, in_=pSo2[d], func=AF.Square, accum_out=hss2[d]),
                  reads=[B_po2[d]], writes=[B_hs2[d], B_po2[d]])
            sc.op("act", lambda e: e.activation(out=hrs2[d], in_=hss2[d], func=AF.Ln, scale=1.0 / 128, bias=epsc[:, 0:1]),
                  reads=[B_hs2[d], B_const], writes=[B_hs2[d]])
            sc.op("act", lambda e: e.activation(out=hrs2[d], in_=hrs2[d], func=AF.Exp, scale=-0.5),
                  reads=[B_hs2[d]], writes=[B_hs2[d]])
            sc.op("dve", lambda e: e.scalar_tensor_tensor(out=ytok2[d], in0=pSo2[d], scalar=hrs2[d][:, 0:1],
                                                          in1=sgtok[sl][:, i, :], op0=ALU.mult, op1=ALU.mult),
                  reads=[B_po2[d], B_hs2[d], B_sgtok[sl][i]], writes=[B_ytok2[d], B_po2[d]])

        def stC(i):
            d = i % 2
            ts = slice(i * 128, (i + 1) * 128)
            sc.op("pe", lambda e: e.transpose(out=pStr_y[d], in_=ytok2[d], identity=ident_b),
                  reads=[B_ytok2[d], B_const], writes=[B_ptry[d]])
            sc.op("act", lambda e: e.activation(out=yaT[sl][:, ts], in_=pStr_y[d], func=AF.Copy,
                                                scale=gnT[:, h:h + 1]),
                  reads=[B_ptry[d], B_const], writes=[B_yaT[sl], B_ptry[d]])
            if i == NT - 1:
                sc.dma("sp", lambda e: e.dma_start(out=ya_dram[h * 128:(h + 1) * 128, :], in_=yaT[sl]),
                       f"d_yaT{sl}", reads=[B_yaT[sl]], writes=[B_yad])

        def slot(u):
            def f():
                if u < NT:
                    stA(u)
                if 0 <= u - 1 < NT:
                    stB(u - 1)
                if 0 <= u - 2 < NT:
                    stC(u - 2)
            return f
        return [slot(u) for u in range(NT + 2)]

    prev_steps = []
    for h in range(NH + 1):
        units = hg_inproj_units(h) if h < NH else []
        n = max(len(units), len(prev_steps))
        for u in range(n):
            if u < len(units):
                units[u]()
            if u < len(prev_steps):
                prev_steps[u]()
        prev_steps = hg_rec_steps(h) if h < NH else []

    sc.barrier()
    top[0] = phase_base
    ATT_G = ((128, 1), (512, 4), (2048, 16))
    DMAX = (1, 4, 15)
    JW = (256, 640, 2048)
    WW = tuple(j + 127 for j in JW)
    WOFF = (0, WW[0], WW[0] + WW[1])
    NSLOT = 4 if stage != "dbg_yb" else 1
    rb33 = alloc(12)
    rbx = alloc(12 * 128, F32, [128, 12, 128])
    ohs = alloc(WW[0] + WW[1] + WW[2])
    zst = alloc(1088, BF16)
    B_att = Buf("attc")
    B_zst = Buf("zst")
    B_zd = Buf("zrow_dram")
    Btoe = [[alloc(JW[g] // 2, BF16) for s in range(4)] for g in range(3)]
    B_toe = Buf("toe")
    sc.dma("sp", lambda e: e.dma_start(out=rb33[0:32, :], in_=rel_bias), "d_const", writes=[B_att])
    sc.op("dve", lambda e: e.memset(rb33[32:33, :], NEG), writes=[B_att])
    sc.dma("sp", lambda e: e.dma_start(out=ohs[0:33, :], in_=att_oh), "d_const", writes=[B_att])
    sc.op("dve", lambda e: e.tensor_copy(out=rbx[0:33, :, :], in_=rb33[0:33, :].unsqueeze(2).to_broadcast([33, 12, 128])),
          reads=[B_att], writes=[B_att])
    for g in range(3):
        for s in range(NSLOT):
            hd = g * 4 + s
            W = WW[g]
            for c0 in range(0, W, 512):
                n = min(512, W - c0)
                sc.op("pe", lambda e, c0=c0, n=n, hd=hd, g=g: e.matmul(
                    pbank[7][:, 0:n], lhsT=rbx[0:33, hd, :], rhs=ohs[0:33, WOFF[g] + c0:WOFF[g] + c0 + n],
                    start=True, stop=True), reads=[B_att], writes=[B_pb[7]])
                sc.op("act", lambda e, c0=c0, n=n: e.activation(out=zst[:, c0:c0 + n], in_=pbank[7][:, 0:n], func=AF.Copy),
                      reads=[B_pb[7]], writes=[B_zst])
            zoff = hd * 128 * 2176
            sc.dma("sp", lambda e, W=W, zoff=zoff: e.dma_start(
                out=zrow_dram[zoff:zoff + 128 * W].rearrange("(c w) -> c w", w=W), in_=zst[:, 0:W]),
                "d_zst", reads=[B_zst], writes=[B_zd])
            src = bass.AP(tensor=zrow_dram.tensor, offset=zrow_dram.offset + zoff + 127,
                          ap=[[W - 1, 128], [1, JW[g]]])
            sc.dma("sp", lambda e, src=src, g=g, s=s: e.dma_start(out=Btoe[g][s], in_=src),
                   "d_toe", reads=[B_zd], writes=[B_toe])

    qTs = [alloc(1024, BF16) for g in range(3)]
    kTs = [alloc(1024, BF16) for g in range(3)]
    vtok = alloc(16 * 3 * 130 // 2, BF16).rearrange("p (t g c) -> p t g c", g=3, c=130)
    B_qTs = [[Buf(f"qTs{g}_{c}") for c in range(4)] for g in range(3)]
    B_kTs = [[Buf(f"kTs{g}_{c}") for c in range(4)] for g in range(3)]
    B_vtok = [Buf(f"vtok{i}") for i in range(NT)]
    PT = [alloc(256, BF16) for _ in range(2)]
    B_PT = [Buf("PT0"), Buf("PT1")]
    rden = alloc(1)
    B_rden = Buf("rden")
    obt = alloc(64, BF16)
    B_obt = Buf("obt")
    ybT = alloc(1024, BF16)
    B_ybT = Buf("ybT")
    B_ybd = Buf("yb_dram")
    sc.op("dve", lambda e: e.memset(vtok[:, :, :, 128:130], 1.0), writes=B_vtok)
    po_ap = [pbank[4 + j][:, 0:129] for j in range(4)]
    B_poa = [B_pb[4 + j] for j in range(4)]
    pt_ctr = 0
    AQ0 = 8192
    for s in range(NSLOT):
        pieces_q = [(w_in[:, AQ0 + (g * 4 + s) * 128:AQ0 + (g * 4 + s + 1) * 128], g * 128, 128) for g in range(3)]
        pieces_k = [(w_in[:, AQ0 + 1536 + (g * 4 + s) * 128:AQ0 + 1536 + (g * 4 + s + 1) * 128], g * 128, 128) for g in range(3)]
        pieces_v = [(w_in[:, AQ0 + 3072 + (g * 4 + s) * 128:AQ0 + 3072 + (g * 4 + s + 1) * 128], g * 128, 128) for g in range(3)]
        s_q = load_slab(pieces_q)
        s_k = load_slab(pieces_k)
        s_v = load_slab(pieces_v)
        issue_cvt(14)

        def inproj_units(tc, s_q=s_q, s_k=s_k, s_v=s_v):
            us = []
            for g in range(3):
                def uq(g=g):
                    bk = next_bank()
                    mm_fm(s_q, g * 128, tc, bk)
                    sc.op("act", lambda e: e.activation(
                        out=qTs[g][:, tc * 512:(tc + 1) * 512], in_=pbank[bk][:], func=AF.Copy, scale=128.0 ** -0.5),
                        reads=[B_pb[bk]], writes=[B_qTs[g][tc]])
                us.append(uq)

                def uk(g=g):
                    bk = next_bank()
                    mm_fm(s_k, g * 128, tc, bk)
                    sc.op("dve", lambda e: e.tensor_copy(out=kTs[g][:, tc * 512:(tc + 1) * 512], in_=pbank[bk][:]),
                          reads=[B_pb[bk]], writes=[B_kTs[g][tc]])
                us.append(uk)
            for i in range(4 * tc, 4 * tc + 4):
                def uv(i=i):
                    bk = next_bank()
                    mm_tm(s_v, 0, 384, i, bk)
                    sc.op("act", lambda e: e.activation(
                        out=vtok[:, i, :, 0:128], in_=pbank[bk][:, 0:384].rearrange("p (g c) -> p g c", c=128), func=AF.Copy),
                        reads=[B_pb[bk]], writes=[B_vtok[i]])
                us.append(uv)
            return us

        for u_ in inproj_units(0):
            u_()
        for Q in range(4):
            pend = inproj_units(Q + 1) if Q < 3 else []
            contribs = []
            for g in range(3):
                for m in range(max(0, 4 * Q - DMAX[g]), 4 * Q + 4):
                    T_lo = max(m, 4 * Q)
                    T_hi = min(4 * Q + 3, m + DMAX[g])
                    if T_hi >= T_lo:
                        contribs.append((g, m, T_lo, T_hi))
            firstT, lastT = {}, {}
            for ci, (g, m, T_lo, T_hi) in enumerate(contribs):
                for T in range(T_lo, T_hi + 1):
                    firstT.setdefault(T, ci)
                    lastT[T] = ci
            every = max(1, len(contribs) // (len(pend) + 1)) if pend else 0
            for ci, (g, m, T_lo, T_hi) in enumerate(contribs):
                if pend and ci % every == every - 1:
                    pend.pop(0)()
                ncols = (T_hi - T_lo + 1) * 128
                bk = next_bank()
                qbufs = [B_qTs[g][T // 4] for T in range(T_lo, T_hi + 1)]
                sc.op("pe", lambda e, g=g, m=m, T_lo=T_lo, T_hi=T_hi, ncols=ncols, bk=bk: e.matmul(
                    pbank[bk][:, 0:ncols], lhsT=kTs[g][:, m * 128:(m + 1) * 128],
                    rhs=qTs[g][:, T_lo * 128:(T_hi + 1) * 128], start=True, stop=False),
                    reads=[B_kTs[g][m // 4]] + qbufs, writes=[B_pb[bk]])
                sc.op("pe", lambda e, g=g, m=m, T_lo=T_lo, T_hi=T_hi, ncols=ncols, bk=bk, s=s: e.matmul(
                    pbank[bk][:, 0:ncols], lhsT=ident_b,
                    rhs=Btoe[g][s][:, (T_lo - m) * 128:(T_hi - m + 1) * 128], start=False, stop=True),
                    reads=[B_toe, B_const], writes=[B_pb[bk]])
                p_ = pt_ctr % 2
                pt_ctr += 1
                sc.op("act", lambda e, p_=p_, ncols=ncols, bk=bk: e.activation(
                    out=PT[p_][:, 0:ncols], in_=pbank[bk][:, 0:ncols], func=AF.Exp),
                    reads=[B_pb[bk]], writes=[B_PT[p_]])
                for T in range(T_lo, T_hi + 1):
                    j = T - 4 * Q
                    st_, sp_ = (firstT[T] == ci), (lastT[T] == ci)
                    sc.op("pe", lambda e, p_=p_, T=T, T_lo=T_lo, j=j, m=m, g=g, st_=st_, sp_=sp_: e.matmul(
                        po_ap[j], lhsT=PT[p_][:, (T - T_lo) * 128:(T - T_lo + 1) * 128], rhs=vtok[:, m, g, 0:129],
                        start=st_, stop=sp_),
                        reads=[B_PT[p_], B_vtok[m]], writes=[B_poa[j]])
            while pend:
                pend.pop(0)()
            for j in range(4):
                T = 4 * Q + j
                sc.op("dve", lambda e, j=j: e.reciprocal(out=rden, in_=po_ap[j][:, 128:129]),
                      reads=[B_poa[j]], writes=[B_rden])
                sc.op("dve", lambda e, j=j: e.tensor_scalar(out=obt, in0=po_ap[j][:, 0:128], scalar1=rden[:, 0:1],
                                                            scalar2=None, op0=ALU.mult),
                      reads=[B_poa[j], B_rden], writes=[B_obt])
                bk = next_bank()
                ptr_att = pbank[bk][:].bitcast(BF16)
                sc.op("pe", lambda e, ptr_att=ptr_att: e.transpose(out=ptr_att[:, 0:128], in_=obt, identity=ident_b),
                      reads=[B_obt, B_const], writes=[B_pb[bk]])
                sc.op("act", lambda e, T=T, ptr_att=ptr_att: e.activation(out=ybT[:, T * 128:(T + 1) * 128], in_=ptr_att[:, 0:128],
                                                         func=AF.Copy),
                      reads=[B_pb[bk]], writes=[B_ybT])
        sc.dma("sp", lambda e, s=s: e.dma_start(out=yb_dram[s * 128:(s + 1) * 128, :], in_=ybT),
               "d_ybT", reads=[B_ybT], writes=[B_ybd])
    fin = [B_sgd, B_yad, B_ybd]
    if stage in ('full', 'dbg_full'):
        sc.barrier()
        top[0] = phase_base
        yaT_all = hT
        sc.dma("sp", lambda e: e.dma_start(out=yaT_all[:, 0:8, :], in_=ya_dram[0:1024, :].rearrange("(c p) t -> p c t", p=128)),
               "d_big", reads=[B_yad], writes=B_hT)
        sc.dma("sp", lambda e: e.dma_start(out=yaT_all[:, 8:16, :], in_=ya_dram[1024:2048, :].rearrange("(c p) t -> p c t", p=128)),
               "d_big", reads=[B_yad], writes=B_hT)
        mergedT = alloc(16384, BF16, [128, KC, S])
        ybT_all = alloc(4096, BF16, [128, 4, S])
        B_ybTa = Buf("ybT_all")
        sc.dma("sp", lambda e: e.dma_start(out=ybT_all, in_=yb_dram.rearrange("(c p) t -> p c t", p=128)),
               "d_big", reads=[B_ybd], writes=[B_ybTa])
        B_mT = [Buf(f"mT{c}") for c in range(4)]
        sga = alloc(1024, BF16)
        sgb = alloc(1024, BF16)
        B_sga, B_sgb = Buf("sga"), Buf("sgb")
        tmpA = slab[2][:, 0:2, :].rearrange("p a b -> p (a b)").bitcast(F32)
        tmpB = slab[2][:, 2:4, :].rearrange("p a b -> p (a b)").bitcast(F32)
        B_tA, B_tB = Buf("tmpA"), Buf("tmpB")
        nslab[0] = 2
        slab_ctr[0] = 0
        for ns in range(4):
            s_a = load_slab([(w_branch_a[:, ns * 512:(ns + 1) * 512], 0, 512)])
            s_b = load_slab([(w_branch_b[:, ns * 512:(ns + 1) * 512], 0, 512)], kc_n=4)
            issue_cvt(200)
            for fb in range(4):
                nb = ns * 4 + fb
                sc.dma("sp", lambda e, nb=nb: e.dma_start(out=sga, in_=sg_dram[nb * 128:(nb + 1) * 128, :]),
                       "d_sga", reads=[B_sgd], writes=[B_sga])
                sc.dma("sp", lambda e, nb=nb: e.dma_start(out=sgb, in_=sg_dram[2048 + nb * 128:2048 + (nb + 1) * 128, :]),
                       "d_sgb", reads=[B_sgd], writes=[B_sgb])
                for tc in range(4):
                    bka = next_bank()
                    for kc in range(KC):
                        sc.op("pe", lambda e, kc=kc, bka=bka, s_a=s_a, fb=fb, tc=tc: e.matmul(
                            pbank[bka][:], lhsT=slab[s_a][:, kc, fb * 128:(fb + 1) * 128],
                            rhs=yaT_all[:, kc, tc * 512:(tc + 1) * 512], start=(kc == 0), stop=(kc == KC - 1)),
                            reads=[B_slab[s_a]] + B_hT[tc * 4:(tc + 1) * 4], writes=[B_pb[bka]])
                    bkb = next_bank()
                    for kc in range(4):
                        sc.op("pe", lambda e, kc=kc, bkb=bkb, s_b=s_b, fb=fb, tc=tc: e.matmul(
                            pbank[bkb][:], lhsT=slab[s_b][:, kc, fb * 128:(fb + 1) * 128],
                            rhs=ybT_all[:, kc, tc * 512:(tc + 1) * 512], start=(kc == 0), stop=(kc == 3)),
                            reads=[B_slab[s_b], B_ybTa], writes=[B_pb[bkb]])
                    cs = slice(tc * 512, (tc + 1) * 512)
                    sc.op("dve", lambda e, bka=bka, cs=cs: e.tensor_tensor(out=tmpA, in0=pbank[bka][:], in1=sga[:, cs], op=ALU.mult),
                          reads=[B_pb[bka], B_sga], writes=[B_tA])
                    sc.op("dve", lambda e, bkb=bkb, cs=cs: e.tensor_tensor(out=tmpB, in0=pbank[bkb][:], in1=sgb[:, cs], op=ALU.mult),
                          reads=[B_pb[bkb], B_sgb], writes=[B_tB])
                    sc.op("pool", lambda e, nb=nb, cs=cs: e.tensor_tensor(out=mergedT[:, nb, cs], in0=tmpA, in1=tmpB, op=ALU.add),
                          reads=[B_tA, B_tB], writes=[B_mT[tc]])

        sc.barrier()
        wo = hT
        B_wo = Buf("wo")
        for c in range(4):
            sc.dma("pool", lambda e, c=c: e.dma_start(
                out=wo[:, c * 4:(c + 1) * 4, :], in_=w_out[c * 512:(c + 1) * 512, :].rearrange("(kc p) n -> p kc n", p=128)),
                "d_big", writes=[B_wo])
        top[0] = phase_base + 16384
        g2b = alloc(2048)
        gfb = alloc(2048)
        B_gb = Buf("gb")
        bcast = lambda ap_: bass.AP(tensor=ap_.tensor, offset=ap_.offset, ap=[[0, 128], [1, ap_.shape[0]]])
        sc.dma("sp", lambda e: e.dma_start(out=g2b, in_=bcast(norm2_gain)), "d_const", writes=[B_gb])
        sc.dma("sp", lambda e: e.dma_start(out=gfb, in_=bcast(final_norm_gain)), "d_const", writes=[B_gb])
        top2 = [slab_base]

        def alloc2(words, dt=F32, shape=None):
            save = top[0]
            top[0] = top2[0]
            a = alloc(words, dt, shape)
            top2[0] = top[0]
            top[0] = save
            assert top2[0] <= slab_base + 12288
            return a
        xt2 = alloc2(2048)
        x2t = alloc2(2048)
        h2f = alloc2(2048)
        h2T = alloc2(2048, F32, [128, KC, 128])
        h2b = alloc2(1024, BF16)
        wr = alloc2(16 * 72, F32, [128, KC, 72])
        lg_all = alloc2(16 * 72, F32, [128, NT, 72])
        brow = alloc2(72)
        onesrow = alloc2(128)
        ss2 = alloc2(1)
        rs2 = alloc2(1)
        B_xt2, B_x2t, B_h2f, B_h2T, B_h2b, B_wr, B_lg, B_s2 = (Buf(n) for n in ("xt2", "x2t", "h2f", "h2T", "h2b", "wr", "lg", "s2"))
        B_x2d, B_h2d = Buf("x2_dram"), Buf("h2_dram")
        sc.dma("sp", lambda e: e.dma_start(out=wr[:, :, 0:8], in_=w_router_group.rearrange("(kc p) n -> p kc n", p=128),
                                           allow_slow_non_contiguous=True), "d_const", writes=[B_wr])
        sc.dma("sp", lambda e: e.dma_start(out=wr[:, :, 8:72], in_=w_router_expert.rearrange("(kc p) n -> p kc n", p=128),
                                           allow_slow_non_contiguous=True), "d_const", writes=[B_wr])
        sc.dma("sp", lambda e: e.dma_start(out=brow[0:1, 0:8], in_=b_router_group), "d_const", writes=[B_wr])
        sc.dma("sp", lambda e: e.dma_start(out=brow[0:1, 8:72], in_=b_router_expert), "d_const", writes=[B_wr])
        sc.op("dve", lambda e: e.memset(onesrow[0:1, :], 1.0), writes=[B_wr])
        sc.op("dve", lambda e: e.memset(h2f, 0.0), writes=[B_h2f])
        sc.op("dve", lambda e: e.memset(h2b, 0.0), writes=[B_h2b])
        sc.dma("sp", lambda e: e.dma_start(out=h2_dram[2048:2049, :], in_=h2b[0:1, :]), "d_h2b", reads=[B_h2b], writes=[B_h2d])
        B_yed = Buf("ye_dram")
        sc.dma("sp", lambda e: e.dma_start(out=ye_dram[8192:8320, :], in_=h2b), "d_h2b", reads=[B_h2b], writes=[B_yed])
        for i in range(NT):
            ts = slice(i * 128, (i + 1) * 128)
            sc.dma("sp", lambda e, ts=ts: e.dma_start(out=xt2, in_=x[ts, :]), "d_xt2", writes=[B_xt2])
            for dsl in range(4):
                for kc in range(KC):
                    sc.op("pe", lambda e, kc=kc, dsl=dsl, ts=ts: e.matmul(
                        pbank[dsl][:], lhsT=mergedT[:, kc, ts], rhs=wo[:, kc, dsl * 512:(dsl + 1) * 512],
                        start=(kc == 0), stop=(kc == KC - 1)),
                        reads=[B_mT[i // 4], B_wo], writes=[B_pb[dsl]])
                sc.op("dve", lambda e, dsl=dsl: e.tensor_tensor(out=x2t[:, dsl * 512:(dsl + 1) * 512], in0=pbank[dsl][:],
                                                               in1=xt2[:, dsl * 512:(dsl + 1) * 512], op=ALU.add),
                      reads=[B_pb[dsl], B_xt2], writes=[B_x2t])
            sc.dma("sp", lambda e, ts=ts: e.dma_start(out=x2_dram[ts, :], in_=x2t), "d_x2t", reads=[B_x2t], writes=[B_x2d])
            sc.op("act", lambda e: e.activation(out=h2f, in_=x2t, func=AF.Square, accum_out=ss2), reads=[B_x2t], writes=[B_h2f, B_s2])
            sc.op("act", lambda e: e.activation(out=rs2, in_=ss2, func=AF.Sqrt, scale=1.0 / D, bias=epsc[:, 0:1]),
                  reads=[B_s2, B_const], writes=[B_s2])
            sc.op("dve", lambda e: e.reciprocal(out=rs2, in_=rs2), reads=[B_s2], writes=[B_s2])
            sc.op("dve", lambda e: e.scalar_tensor_tensor(out=h2f, in0=x2t, scalar=rs2[:, 0:1], in1=g2b, op0=ALU.mult, op1=ALU.mult),
                  reads=[B_x2t, B_s2, B_gb], writes=[B_h2f])
            sc.op("act", lambda e: e.activation(out=h2b, in_=h2f, func=AF.Copy), reads=[B_h2f], writes=[B_h2b])
            sc.dma("sp", lambda e, ts=ts: e.dma_start(out=h2_dram[ts, :], in_=h2b), "d_h2b", reads=[B_h2b], writes=[B_h2d])
            for q4 in range(4):
                bk = 4 + q4
                for j in range(4):
                    kc = q4 * 4 + j
                    sc.op("pe", lambda e, bk=bk, j=j, kc=kc: e.transpose(
                        out=pbank[bk][:, j * 128:(j + 1) * 128], in_=h2f[:, kc * 128:(kc + 1) * 128], identity=ident_f),
                        reads=[B_h2f, B_const], writes=[B_pb[bk]])
                sc.op("act" if q4 % 2 else "dve", (lambda e, bk=bk, q4=q4: e.activation(
                    out=h2T[:, q4 * 4:(q4 + 1) * 4, :], in_=pbank[bk][:].rearrange("p (a b) -> p a b", b=128), func=AF.Copy))
                    if q4 % 2 else (lambda e, bk=bk, q4=q4: e.tensor_copy(
                        out=h2T[:, q4 * 4:(q4 + 1) * 4, :], in_=pbank[bk][:].rearrange("p (a b) -> p a b", b=128))),
                    reads=[B_pb[bk]], writes=[B_h2T])
            for kc in range(KC):
                sc.op("pe", lambda e, kc=kc: e.matmul(pbank[0][:, 0:72], lhsT=h2T[:, kc, :], rhs=wr[:, kc, :],
                                                      start=(kc == 0), stop=False),
                      reads=[B_h2T, B_wr], writes=[B_pb[0]])
            sc.op("pe", lambda e: e.matmul(pbank[0][:, 0:72], lhsT=onesrow[0:1, :], rhs=brow[0:1, :], start=False, stop=True),
                  reads=[B_wr], writes=[B_pb[0]])
            sc.op("dve", lambda e, i=i: e.tensor_copy(out=lg_all[:, i, :], in_=pbank[0][:, 0:72]), reads=[B_pb[0]], writes=[B_lg])

        sc.barrier()
        top[0] = phase_base
        B_r = Buf("route")

        def R(eng, fn):
            sc.op(eng, fn, reads=[B_r, B_lg, B_const], writes=[B_r])
        lgG = lg_all[:, :, 0:8]
        lgE = lg_all[:, :, 8:72].rearrange("p t (g e) -> p t g e", e=8)
        mG, sumG, pg, m1, m2, w1 = (alloc(16) for _ in range(6))
        ohG, eG, lgin, oh1, oh2, msk = (alloc(128, F32, [128, 16, 8]) for _ in range(6))
        sel = alloc(1024).rearrange("p (t g e) -> p t g e", g=8, e=8)
        O1 = alloc(1024).rearrange("p (t g e) -> p t g e", g=8, e=8)
        O2 = alloc(1024).rearrange("p (t g e) -> p t g e", g=8, e=8)
        Osum = alloc(1024, F32, [128, 16, 64])
        Ob = alloc(512, BF16, [128, 16, 64])
        cum = alloc(1024, F32, [128, 16, 64])
        tmp64 = alloc(1024, F32, [128, 16, 64])
        gates = alloc(32, F32, [128, 16, 2])
        slot_ = alloc(32, F32, [128, 16, 2])
        eid_ = alloc(32, F32, [128, 16, 2])
        tixf = alloc(32, F32, [128, 16, 2])
        rixf = alloc(32, F32, [128, 16, 2])
        tix = alloc(32, I32, [128, 16, 2])
        rix = alloc(32, I32, [128, 16, 2])
        iota_i = alloc(64, I32)
        iota_e = alloc(64)
        tokid = alloc(16, I32)
        fill_i = alloc(65, I32)
        ones_b = alloc(64, BF16)
        mst_b = alloc(64, BF16)
        mstf = alloc(128)
        tabs = alloc(64, I32)
        bc3 = lambda a: a.unsqueeze(2).to_broadcast([128, 16, 8])
        R("dve", lambda e: e.tensor_reduce(out=mG, in_=lgG, axis=AX.X, op=ALU.max))
        R("dve", lambda e: e.tensor_tensor(out=ohG, in0=lgG, in1=bc3(mG), op=ALU.is_equal))
        R("dve", lambda e: e.tensor_tensor(out=eG, in0=lgG, in1=bc3(mG), op=ALU.subtract))
        R("act", lambda e: e.activation(out=eG, in_=eG, func=AF.Exp))
        R("dve", lambda e: e.tensor_reduce(out=sumG, in_=eG, axis=AX.X, op=ALU.add))
        R("dve", lambda e: e.reciprocal(out=pg, in_=sumG))
        R("dve", lambda e: e.tensor_tensor(out=sel, in0=lgE, in1=ohG.unsqueeze(3).to_broadcast([128, 16, 8, 8]), op=ALU.mult))
        R("dve", lambda e: e.tensor_reduce(out=lgin, in_=sel.rearrange("p t g e -> p t e g"), axis=AX.X, op=ALU.add))
        R("dve", lambda e: e.tensor_reduce(out=m1, in_=lgin, axis=AX.X, op=ALU.max))
        R("dve", lambda e: e.tensor_tensor(out=oh1, in0=lgin, in1=bc3(m1), op=ALU.is_equal))
        R("dve", lambda e: e.scalar_tensor_tensor(out=msk, in0=oh1, scalar=-1e30, in1=lgin, op0=ALU.mult, op1=ALU.add))
        R("dve", lambda e: e.tensor_reduce(out=m2, in_=msk, axis=AX.X, op=ALU.max))
        R("dve", lambda e: e.tensor_tensor(out=oh2, in0=msk, in1=bc3(m2), op=ALU.is_equal))
        R("dve", lambda e: e.tensor_tensor(out=w1, in0=m2, in1=m1, op=ALU.subtract))
        R("act", lambda e: e.activation(out=w1, in_=w1, func=AF.Exp))
        R("dve", lambda e: e.tensor_scalar(out=w1, in0=w1, scalar1=1.0, scalar2=None, op0=ALU.add))
        R("dve", lambda e: e.reciprocal(out=w1, in_=w1))
        R("dve", lambda e: e.tensor_tensor(out=gates[:, :, 0], in0=pg, in1=w1, op=ALU.mult))
        R("dve", lambda e: e.tensor_tensor(out=gates[:, :, 1], in0=pg, in1=gates[:, :, 0], op=ALU.subtract))
        for O_, oh_ in ((O1, oh1), (O2, oh2)):
            R("dve", lambda e, O_=O_: e.tensor_copy(out=O_, in_=ohG.unsqueeze(3).to_broadcast([128, 16, 8, 8])))
            R("dve", lambda e, O_=O_, oh_=oh_: e.tensor_tensor(out=O_, in0=O_, in1=oh_.unsqueeze(2).to_broadcast([128, 16, 8, 8]),
                                                             op=ALU.mult))
        O1f = O1.rearrange("p t g e -> p t (g e)")
        O2f = O2.rearrange("p t g e -> p t (g e)")
        R("dve", lambda e: e.tensor_tensor(out=Osum, in0=O1f, in1=O2f, op=ALU.add))
        R("dve", lambda e: e.tensor_copy(out=Ob, in_=Osum))
        R("dve", lambda e: e.memset(ones_b, 1.0))
        R("dve", lambda e: e.tensor_tensor(out=mstf, in0=mask_ut, in1=ident_f, op=ALU.subtract))
        R("dve", lambda e: e.tensor_copy(out=mst_b, in_=mstf))
        for i in range(NT):
            bk = 4 + (i // 8)
            cols = slice((i % 8) * 64, (i % 8 + 1) * 64)
            for j in range(i):
                sc.op("pe", lambda e, bk=bk, cols=cols, j=j: e.matmul(pbank[bk][:, cols], lhsT=ones_b, rhs=Ob[:, j, :],
                                                                     start=(j == 0), stop=False),
                      reads=[B_r], writes=[B_pb[bk]])
            sc.op("pe", lambda e, bk=bk, cols=cols, i=i: e.matmul(pbank[bk][:, cols], lhsT=mst_b, rhs=Ob[:, i, :],
                                                                 start=(i == 0), stop=True),
                  reads=[B_r], writes=[B_pb[bk]])
        sc.op("dve", lambda e: e.tensor_copy(out=cum[:, 0:8, :], in_=pbank[4][:].rearrange("p (t e) -> p t e", e=64)),
              reads=[B_pb[4], B_r], writes=[B_r])
        sc.op("dve", lambda e: e.tensor_copy(out=cum[:, 8:16, :], in_=pbank[5][:].rearrange("p (t e) -> p t e", e=64)),
              reads=[B_pb[5], B_r], writes=[B_r])
        R("pool", lambda e: e.iota(iota_i, pattern=[[1, 64]], base=0, channel_multiplier=0))
        R("pool", lambda e: e.iota(tokid, pattern=[[128, 16]], base=0, channel_multiplier=1))
        R("pool", lambda e: e.iota(fill_i, pattern=[[0, 65]], base=2048, channel_multiplier=0))
        R("dve", lambda e: e.tensor_copy(out=iota_e, in_=iota_i))
        for jj, Of in ((0, O1f), (1, O2f)):
            R("dve", lambda e, Of=Of: e.tensor_tensor(out=tmp64, in0=Of, in1=cum, op=ALU.mult))
            R("dve", lambda e, jj=jj: e.tensor_reduce(out=slot_[:, :, jj], in_=tmp64, axis=AX.X, op=ALU.add))
            R("dve", lambda e, Of=Of: e.tensor_tensor(out=tmp64, in0=Of, in1=iota_e.unsqueeze(1).to_broadcast([128, 16, 64]),
                                                     op=ALU.mult))
            R("dve", lambda e, jj=jj: e.tensor_reduce(out=eid_[:, :, jj], in_=tmp64, axis=AX.X, op=ALU.add))
        R("dve", lambda e: e.tensor_scalar(out=tixf, in0=slot_, scalar1=128.0, scalar2=None, op0=ALU.min))
        R("dve", lambda e: e.scalar_tensor_tensor(out=tixf, in0=eid_, scalar=129.0, in1=tixf, op0=ALU.mult, op1=ALU.add))
        R("dve", lambda e: e.tensor_scalar(out=rixf, in0=slot_, scalar1=128.0, scalar2=1e6, op0=ALU.is_ge, op1=ALU.mult))
        R("dve", lambda e: e.tensor_tensor(out=rixf, in0=rixf, in1=slot_, op=ALU.add))
        R("dve", lambda e: e.scalar_tensor_tensor(out=rixf, in0=eid_, scalar=128.0, in1=rixf, op0=ALU.mult, op1=ALU.add))
        R("dve", lambda e: e.tensor_scalar(out=rixf, in0=rixf, scalar1=8192.0, scalar2=None, op0=ALU.min))
        R("dve", lambda e: e.tensor_copy(out=tix, in_=tixf))
        R("dve", lambda e: e.tensor_copy(out=rix, in_=rixf))
        B_tabd = Buf("tab_dram")
        sc.dma("sp", lambda e: e.dma_start(out=tab_dram.rearrange("(p c) -> p c", c=65), in_=fill_i), "d_tab",
               reads=[B_r], writes=[B_tabd])
        tab2 = tab_dram.rearrange("(r c) -> r c", c=1)
        for i in range(NT):
            for jj in range(2):
                sc.dma("pool", lambda e, i=i, jj=jj: e.indirect_dma_start(
                    out=tab2, out_offset=bass.IndirectOffsetOnAxis(ap=tix[:, i, jj:jj + 1], axis=0),
                    in_=tokid[:, i:i + 1], in_offset=None), "d_tabs", reads=[B_r, B_tabd], writes=[B_tabd])
        B_tabs = Buf("tabs")
        tab_src = bass.AP(tensor=tab_dram.tensor, offset=tab_dram.offset, ap=[[1, 128], [129, 64]])
        sc.dma("sp", lambda e: e.dma_start(out=tabs, in_=tab_src, allow_slow_non_contiguous=True), "d_tab2",
               reads=[B_tabd], writes=[B_tabs])

        sc.barrier()
        wreg = [slab_base - 16384 + 4096 * k for k in range(6)]
        def wview(k):
            a = arena[:, wreg[k]:wreg[k] + 4096].bitcast(BF16)
            return a
        Wg = [wview(0).rearrange("p (a b) -> p a b", b=512), wview(3).rearrange("p (a b) -> p a b", b=512)]
        Wu = [wview(1).rearrange("p (a b) -> p a b", b=512), wview(4).rearrange("p (a b) -> p a b", b=512)]
        Wd = [wview(2).rearrange("p (a b) -> p a b", b=2048), wview(5).rearrange("p (a b) -> p a b", b=2048)]
        B_W = [Buf("W0"), Buf("W1")]
        xb = [alloc(1024, BF16) for _ in range(2)]
        B_xb = [Buf("xb0"), Buf("xb1")]
        xbT = alloc(1024, BF16, [128, KC, 128])
        B_xbT = Buf("xbT")
        sgu = alloc(512)
        ub = alloc(256, BF16)
        uT = alloc(256, BF16, [128, 4, 128])
        B_sgu, B_ub, B_uT = Buf("sgu"), Buf("ub"), Buf("uT")
        yeb = [alloc(1024, BF16) for _ in range(2)]
        B_yeb = [Buf("yeb0"), Buf("yeb1")]
        NE = 64 if stage == "full" else 2
        def moe_loads(ex):
            p_ = ex % 2
            sc.dma("sp", lambda e, ex=ex, p_=p_: e.dma_start(
                out=Wg[p_], in_=wbf_dram[ex, 0].rearrange("(kc p n) -> p kc n", p=128, n=512)),
                f"d_W{p_}", reads=[B_wbf], writes=[B_W[p_]])
            sc.dma("sp", lambda e, ex=ex, p_=p_: e.dma_start(
                out=Wu[p_], in_=wbf_dram[ex, 1].rearrange("(kc p n) -> p kc n", p=128, n=512)),
                f"d_W{p_}", reads=[B_wbf], writes=[B_W[p_]])
            sc.dma("sp", lambda e, ex=ex, p_=p_: e.dma_start(
                out=Wd[p_], in_=wbf_dram[ex, 2].rearrange("(kc p n) -> p kc n", p=128, n=2048)),
                f"d_W{p_}", reads=[B_wbf], writes=[B_W[p_]])
            sc.dma("pool", lambda e, ex=ex, p_=p_: e.indirect_dma_start(
                out=xb[p_], out_offset=None, in_=h2_dram,
                in_offset=bass.IndirectOffsetOnAxis(ap=tabs[:, ex:ex + 1], axis=0)),
                f"d_xb{p_}", reads=[B_tabs, B_h2d], writes=[B_xb[p_]])
        moe_loads(0)
        for ex in range(NE):
            p_ = ex % 2
            if ex + 1 < NE:
                moe_loads(ex + 1)
            for q2 in range(2):
                bk = q2
                pt = pbank[bk][:].bitcast(BF16)
                for j in range(8):
                    kc = q2 * 8 + j
                    sc.op("pe", lambda e, pt=pt, j=j, kc=kc, p_=p_: e.transpose(
                        out=pt[:, j * 128:(j + 1) * 128], in_=xb[p_][:, kc * 128:(kc + 1) * 128], identity=ident_b),
                        reads=[B_xb[p_], B_const], writes=[B_pb[bk]])
                sc.op("act" if q2 else "dve", (lambda e, pt=pt, q2=q2: e.activation(
                    out=xbT[:, q2 * 8:(q2 + 1) * 8, :], in_=pt.rearrange("p (a b) -> p a b", b=128), func=AF.Copy))
                    if q2 else (lambda e, pt=pt, q2=q2: e.tensor_copy(
                        out=xbT[:, q2 * 8:(q2 + 1) * 8, :], in_=pt.rearrange("p (a b) -> p a b", b=128))),
                    reads=[B_pb[bk]], writes=[B_xbT])
            for kc in range(KC):
                sc.op("pe", lambda e, kc=kc, p_=p_: e.matmul(pbank[2][:], lhsT=xbT[:, kc, :], rhs=Wg[p_][:, kc, :],
                                                            start=(kc == 0), stop=(kc == KC - 1)),
                      reads=[B_xbT, B_W[p_]], writes=[B_pb[2]])
            for kc in range(KC):
                sc.op("pe", lambda e, kc=kc, p_=p_: e.matmul(pbank[3][:], lhsT=xbT[:, kc, :], rhs=Wu[p_][:, kc, :],
                                                            start=(kc == 0), stop=(kc == KC - 1)),
                      reads=[B_xbT, B_W[p_]], writes=[B_pb[3]])
            sc.op("act", lambda e: e.activation(out=sgu, in_=pbank[2][:], func=AF.Silu), reads=[B_pb[2]], writes=[B_sgu])
            sc.op("dve", lambda e: e.tensor_tensor(out=ub, in0=sgu, in1=pbank[3][:], op=ALU.mult),
                  reads=[B_sgu, B_pb[3]], writes=[B_ub])
            ptu = pbank[0][:].bitcast(BF16)
            for fc in range(4):
                sc.op("pe", lambda e, fc=fc, ptu=ptu: e.transpose(out=ptu[:, fc * 128:(fc + 1) * 128],
                                                                 in_=ub[:, fc * 128:(fc + 1) * 128], identity=ident_b),
                      reads=[B_ub, B_const], writes=[B_pb[0]])
            sc.op("act", lambda e, ptu=ptu: e.activation(out=uT, in_=ptu[:, 0:512].rearrange("p (a b) -> p a b", b=128), func=AF.Copy),
                  reads=[B_pb[0]], writes=[B_uT])
            for dsl in range(4):
                bk = 4 + dsl
                for fc in range(4):
                    sc.op("pe", lambda e, fc=fc, dsl=dsl, bk=bk, p_=p_: e.matmul(
                        pbank[bk][:], lhsT=uT[:, fc, :], rhs=Wd[p_][:, fc, dsl * 512:(dsl + 1) * 512],
                        start=(fc == 0), stop=(fc == 3)),
                        reads=[B_uT, B_W[p_]], writes=[B_pb[bk]])
                if dsl % 2:
                    sc.op("act", lambda e, dsl=dsl, bk=bk, p_=p_: e.activation(out=yeb[p_][:, dsl * 512:(dsl + 1) * 512],
                                                                              in_=pbank[bk][:], func=AF.Copy),
                          reads=[B_pb[bk]], writes=[B_yeb[p_]])
                else:
                    sc.op("dve", lambda e, dsl=dsl, bk=bk, p_=p_: e.tensor_copy(out=yeb[p_][:, dsl * 512:(dsl + 1) * 512],
                                                                               in_=pbank[bk][:]),
                          reads=[B_pb[bk]], writes=[B_yeb[p_]])
            sc.dma("sp", lambda e, ex=ex, p_=p_: e.dma_start(out=ye_dram[ex * 128:(ex + 1) * 128, :], in_=yeb[p_]),
                   f"d_yeb{p_}", reads=[B_yeb[p_]], writes=[B_yed])

        sc.barrier()
        top[0] = slab_base - 16384
        fx = [alloc(2048) for _ in range(2)]
        fa = [alloc(1024, BF16) for _ in range(2)]
        fb_ = [alloc(1024, BF16) for _ in range(2)]
        fo = [alloc(2048) for _ in range(2)]
        B_fx, B_fa, B_fb, B_fo = ([Buf(f"{n}{k}") for k in range(2)] for n in ("fx", "fa", "fb", "fo"))
        fss = alloc(2)
        B_fs = [Buf("fs0"), Buf("fs1")]
        B_out = Buf("out")
        for i in range(NT):
            p_ = i % 2
            ts = slice(i * 128, (i + 1) * 128)
            sc.dma("sp", lambda e, ts=ts, p_=p_: e.dma_start(out=fx[p_], in_=x2_dram[ts, :]), f"d_fx{p_}",
                   reads=[B_x2d], writes=[B_fx[p_]])
            sc.dma("pool", lambda e, i=i, p_=p_: e.indirect_dma_start(
                out=fa[p_], out_offset=None, in_=ye_dram, in_offset=bass.IndirectOffsetOnAxis(ap=rix[:, i, 0:1], axis=0)),
                f"d_fa{p_}", reads=[B_yed, B_r], writes=[B_fa[p_]])
            sc.dma("pool", lambda e, i=i, p_=p_: e.indirect_dma_start(
                out=fb_[p_], out_offset=None, in_=ye_dram, in_offset=bass.IndirectOffsetOnAxis(ap=rix[:, i, 1:2], axis=0)),
                f"d_fb{p_}", reads=[B_yed, B_r], writes=[B_fb[p_]])
            sc.op("dve", lambda e, i=i, p_=p_: e.scalar_tensor_tensor(out=fx[p_], in0=fa[p_], scalar=gates[:, i, 0:1], in1=fx[p_],
                                                                      op0=ALU.mult, op1=ALU.add),
                  reads=[B_fa[p_], B_fx[p_], B_r], writes=[B_fx[p_]])
            sc.op("dve", lambda e, i=i, p_=p_: e.scalar_tensor_tensor(out=fx[p_], in0=fb_[p_], scalar=gates[:, i, 1:2], in1=fx[p_],
                                                                      op0=ALU.mult, op1=ALU.add),
                  reads=[B_fb[p_], B_fx[p_], B_r], writes=[B_fx[p_]])
            sc.op("act", lambda e, p_=p_: e.activation(out=fo[p_], in_=fx[p_], func=AF.Square, accum_out=fss[:, p_:p_ + 1]),
                  reads=[B_fx[p_]], writes=[B_fo[p_], B_fs[p_]])
            sc.op("act", lambda e, p_=p_: e.activation(out=fss[:, p_:p_ + 1], in_=fss[:, p_:p_ + 1], func=AF.Sqrt, scale=1.0 / D,
                                                       bias=epsc[:, 0:1]),
                  reads=[B_fs[p_], B_const], writes=[B_fs[p_]])
            sc.op("dve", lambda e, p_=p_: e.reciprocal(out=fss[:, p_:p_ + 1], in_=fss[:, p_:p_ + 1]), reads=[B_fs[p_]], writes=[B_fs[p_]])
            sc.op("dve", lambda e, p_=p_: e.scalar_tensor_tensor(out=fo[p_], in0=fx[p_], scalar=fss[:, p_:p_ + 1], in1=gfb,
                                                                 op0=ALU.mult, op1=ALU.mult),
                  reads=[B_fx[p_], B_fs[p_], B_gb], writes=[B_fo[p_]])
            sc.dma("sp", lambda e, ts=ts, p_=p_: e.dma_start(out=out[ts, :], in_=fo[p_]), f"d_fo{p_}",
                   reads=[B_fo[p_]], writes=[B_out])
        fin = [B_out]

    sc.wait_all("sp", fin)

    names = sc.sem_names()
    sem_cms = [nc.semaphore(n) for n in names]
    for n, cm in zip(names, sem_cms):
        sc.sems[n] = cm.__enter__()
    with nc.Block() as block:
        sc.emit(block)
    for cm in reversed(sem_cms):
        cm.__exit__(None, None, None)
    for cm in reversed(ctxs):
        cm.__exit__(None, None, None)
    print("instr counts:", sc.cnt, "dma:", sc.ndma, "sems:", len(names), "arena top:", top[0])
    return nc


_IN_NAMES = ("norm1_gain", "w_in", "hg_norm_gain", "w_branch_a", "w_branch_b", "w_out", "norm2_gain",
             "w_router_group", "b_router_group", "w_router_expert", "b_router_expert",
             "w_exp_gate", "w_exp_up", "w_exp_down")


def make_in_map(inputs, c):
    m = {"x": np.ascontiguousarray(inputs["x"][c])}
    for k in _IN_NAMES:
        m[k] = np.ascontiguousarray(np.asarray(inputs[k])[0])
    m["hg_lb_logits"] = np.ascontiguousarray(inputs["hg_lb_logits"])
    m["rel_bias"] = np.ascontiguousarray(inputs["rel_bias"])
    m["final_norm_gain"] = np.ascontiguousarray(inputs["final_norm_gain"])
    m["att_oh"] = _att_onehot()
    return m


def kernel(**inputs):
    n = 8
    nc = build_nc()
    inputs = {k: np.asarray(v) for k, v in inputs.items()}
    in_maps = [make_in_map(inputs, c) for c in range(n)]
    res = run_bass_kernel_spmd(nc, in_maps, core_ids=list(range(n)))
    return np.stack([np.asarray(r["out"]) for r in res.results], axis=0).astype(np.float32)
```

```python
import numpy as np
import concourse.bass as bass
import concourse.mybir as mybir
from concourse.bass_utils import run_bass_kernel_spmd

F32 = mybir.dt.float32
BF16 = mybir.dt.bfloat16
I32 = mybir.dt.int32
AF = mybir.ActivationFunctionType
ALU = mybir.AluOpType
AX = mybir.AxisListType

D = 2048
S = 2048
NT = S // 128
KC = D // 128
INW = 16896
EPS = 1e-6
NEG = -30000.0
ENGS = ("pe", "act", "dve", "pool", "sp")


class Buf:
    __slots__ = ("name", "w", "r")

    def __init__(self, name):
        self.name = name
        self.w = None
        self.r = {}


class Sched:
    def __init__(self, nc):
        self.nc = nc
        self.q = {e: [] for e in ENGS}
        self.cnt = {e: 0 for e in ENGS}
        self.seen = {e: {} for e in ENGS}
        self.dma_cnt = {}
        self.sems = {}
        self.ndma = 0

    def _deps(self, eng, reads, writes):
        deps = {}
        def add(ev):
            if ev is None:
                return
            k, v = ev
            if deps.get(k, 0) < v:
                deps[k] = v
        for b in reads:
            add(b.w)
        for b in writes:
            add(b.w)
            for k, v in b.r.items():
                add((k, v))
        for k, v in deps.items():
            if k == "pe" and eng == "pe":
                continue
            if self.seen[eng].get(k, 0) >= v:
                continue
            self.seen[eng][k] = v
            self.q[eng].append(("wait", k, v))

    def _mark(self, ev, reads, writes):
        for b in writes:
            b.w = ev
            b.r = {}
        for b in reads:
            if b.r.get(ev[0], 0) < ev[1]:
                b.r[ev[0]] = ev[1]

    def op(self, eng, fn, reads=(), writes=()):
        self._deps(eng, reads, writes)
        self.cnt[eng] += 1
        ev = (eng, self.cnt[eng])
        self.q[eng].append(("op", fn, eng, 1))
        self._mark(ev, reads, writes)
        return ev

    def dma(self, eng, fn, sem, reads=(), writes=()):
        self._deps(eng, reads, writes)
        self.dma_cnt[sem] = self.dma_cnt.get(sem, 0) + 16
        ev = (sem, self.dma_cnt[sem])
        self.q[eng].append(("op", fn, sem, 16))
        self._mark(ev, reads, writes)
        self.ndma += 1
        return ev

    def barrier(self):
        keys = list(ENGS) + sorted(self.dma_cnt.keys())
        for eng in ENGS:
            for k in keys:
                if k == eng:
                    continue
                v = self.cnt[k] if k in self.cnt else self.dma_cnt[k]
                if v > self.seen[eng].get(k, 0):
                    self.seen[eng][k] = v
                    self.q[eng].append(("wait", k, v))

    def wait_all(self, eng, bufs):
        self._deps(eng, bufs, ())

    def sem_names(self):
        return list(ENGS) + sorted(self.dma_cnt.keys())

    def emit(self, block):
        nc = self.nc
        sems = self.sems

        def run(engname):
            def body(e):
                for it in self.q[engname]:
                    if it[0] == "wait":
                        e.wait_ge(sems[it[1]], it[2])
                    else:
                        ins = it[1](e)
                        ins.then_inc(sems[it[2]], it[3])
            return body
        block.tensor(run("pe"))
        block.scalar(run("act"))
        block.vector(run("dve"))
        block.gpsimd(run("pool"))
        block.sync(run("sp"))


def _t5_bucket_np(dist):
    dist = np.asarray(dist, dtype=np.int64)
    exact = 16
    d_f = np.maximum(dist, 1).astype(np.float32)
    lb = exact + (np.log(d_f / np.float32(exact)) / np.float32(np.log(2048 / exact)) * np.float32(32 - exact)).astype(np.int32)
    return np.where(dist < exact, dist, np.minimum(lb, 31))


def _att_onehot():
    groups = ((128, 1), (512, 4), (2048, 16))
    JW = (256, 640, 2048)
    cols = []
    for (win, dil), J in zip(groups, JW):
        W = J + 127
        dist = np.arange(W) - 127
        valid = (dist >= 0) & (dist % dil == 0) & (dist <= win)
        b = _t5_bucket_np(np.maximum(dist, 0))
        oh = np.zeros((33, W), np.float32)
        oh[b[valid], np.nonzero(valid)[0]] = 1.0
        oh[32, ~valid] = 1.0
        cols.append(oh)
    return np.ascontiguousarray(np.concatenate(cols, axis=1))
def build_nc(stage="full"):
    nc = bass.Bass("TRN2", target_bir_lowering=False)
    dr = lambda name, shape, dt, kind: nc.dram_tensor(name, shape, dt, kind=kind).ap()
    x = dr("x", [S, D], F32, "ExternalInput")
    norm1_gain = dr("norm1_gain", [D], F32, "ExternalInput")
    w_in = dr("w_in", [D, INW], F32, "ExternalInput")
    hg_lb_logits = dr("hg_lb_logits", [2, D], F32, "ExternalInput")
    hg_norm_gain = dr("hg_norm_gain", [D], F32, "ExternalInput")
    rel_bias = dr("rel_bias", [32, 12], F32, "ExternalInput")
    att_oh = dr("att_oh", [33, 3325], F32, "ExternalInput")
    w_branch_a = dr("w_branch_a", [D, D], F32, "ExternalInput")
    w_branch_b = dr("w_branch_b", [512, D], F32, "ExternalInput")
    w_out = dr("w_out", [D, D], F32, "ExternalInput")
    norm2_gain = dr("norm2_gain", [D], F32, "ExternalInput")
    final_norm_gain = dr("final_norm_gain", [D], F32, "ExternalInput")
    w_router_group = dr("w_router_group", [D, 8], F32, "ExternalInput")
    b_router_group = dr("b_router_group", [1, 8], F32, "ExternalInput")
    w_router_expert = dr("w_router_expert", [D, 64], F32, "ExternalInput")
    b_router_expert = dr("b_router_expert", [1, 64], F32, "ExternalInput")
    w_exp_gate = dr("w_exp_gate", [64, D, 512], F32, "ExternalInput")
    w_exp_up = dr("w_exp_up", [64, D, 512], F32, "ExternalInput")
    w_exp_down = dr("w_exp_down", [64, 512, D], F32, "ExternalInput")
    out = dr("out", [S, D], F32, "ExternalOutput")

    def scratch(name, shape, dt):
        kind = "ExternalOutput" if stage == "dbg_" + name else "Internal"
        return dr(name, shape, dt, kind)
    sg_dram = scratch("sg", [2 * D, S], BF16)
    ya_dram = scratch("ya", [D, S], BF16)

    yb_dram = scratch("yb", [512, S], BF16)
    zrow_dram = scratch("zrow", [12 * 128 * 2176], BF16)
    x2_dram = scratch("x2", [S, D], F32)
    h2_dram = scratch("h2", [S + 1, D], BF16)
    ye_dram = scratch("ye", [8192 + 128, D], BF16)
    tab_dram = scratch("tab", [8320], I32)
    wbf_parts = [scratch(f"wbf{k}", [16, 3, 2048 * 512], BF16) for k in range(4)]

    class _Wbf:
        def __getitem__(self, key):
            e_, m_ = key
            return wbf_parts[e_ // 16][e_ % 16, m_]
    wbf_dram = _Wbf()
    sc = Sched(nc)
    ctxs = []

    def sb(name, shape, dt):
        cm = nc.sbuf_tensor(name, shape, dt)
        t = cm.__enter__()
        ctxs.append(cm)
        return t

    def ps(name, shape, dt):
        cm = nc.psum_tensor(name, shape, dt)
        t = cm.__enter__()
        ctxs.append(cm)
        return t

    ARENA = 53100
    arena = sb("arena", [128, ARENA], F32)
    top = [0]

    def alloc(words, dt=F32, shape=None):
        o = top[0]
        top[0] += words
        assert top[0] <= ARENA, ("arena overflow", top[0])
        a = arena[:, o:o + words]
        if dt != F32:
            a = a.bitcast(dt)
        if shape is not None and len(shape) == 3:
            a = a.rearrange("p (a b) -> p a b", b=shape[2])
        return a

    pbank = [ps(f"pb{i}", [128, 512], F32) for i in range(8)]
    B_pb = [Buf(f"pb{i}") for i in range(8)]

    ident_f = alloc(128)
    ident_b = alloc(64, BF16)
    mask_ut = alloc(128)
    B_const = Buf("const")
    sc.op("pool", lambda e: e.memset(ident_f, 1.0), writes=[B_const])
    sc.op("pool", lambda e: e.affine_select(out=ident_f, in_=ident_f, pattern=[[-1, 128]],
                                            compare_op=ALU.is_equal, fill=0.0, base=0, channel_multiplier=1),
          reads=[B_const], writes=[B_const])
    sc.op("dve", lambda e: e.tensor_copy(out=ident_b, in_=ident_f), reads=[B_const], writes=[B_const])
    sc.op("pool", lambda e: e.memset(mask_ut, 1.0), writes=[B_const])
    sc.op("pool", lambda e: e.affine_select(out=mask_ut, in_=mask_ut, pattern=[[1, 128]],
                                            compare_op=ALU.is_ge, fill=0.0, base=0, channel_multiplier=-1),
          reads=[B_const], writes=[B_const])
    g1 = alloc(KC)
    epsc = alloc(1)
    zeros512 = alloc(512)
    lbl = alloc(32, F32, [128, 2, 16])
    lbT = alloc(16)
    omlb = alloc(16)
    nomlb = alloc(16)
    gnT = alloc(16)
    sc.dma("sp", lambda e: e.dma_start(out=g1, in_=norm1_gain.rearrange("(kc p) -> p kc", p=128),
                                       allow_slow_non_contiguous=True), "d_const", writes=[B_const])
    sc.dma("sp", lambda e: e.dma_start(out=gnT, in_=hg_norm_gain.rearrange("(kc p) -> p kc", p=128),
                                       allow_slow_non_contiguous=True), "d_const", writes=[B_const])
    sc.dma("sp", lambda e: e.dma_start(out=lbl, in_=hg_lb_logits.rearrange("r (h p) -> p r h", p=128),
                                       allow_slow_non_contiguous=True), "d_const", writes=[B_const])
    sc.op("dve", lambda e: e.memset(epsc, EPS), writes=[B_const])
    sc.op("dve", lambda e: e.memset(zeros512, 0.0), writes=[B_const])
    sc.op("dve", lambda e: e.tensor_tensor(out=lbT, in0=lbl[:, 0, :], in1=lbl[:, 1, :], op=ALU.subtract),
          reads=[B_const], writes=[B_const])
    sc.op("act", lambda e: e.activation(out=omlb, in_=lbT, func=AF.Sigmoid, scale=-1.0), reads=[B_const], writes=[B_const])
    sc.op("act", lambda e: e.activation(out=lbT, in_=lbT, func=AF.Sigmoid), reads=[B_const], writes=[B_const])
    sc.op("dve", lambda e: e.tensor_scalar(out=nomlb, in0=omlb, scalar1=-1.0, scalar2=None, op0=ALU.mult),
          reads=[B_const], writes=[B_const])

    hT = alloc(16384, BF16, [128, KC, S])
    B_hT = [Buf(f"hT{i}") for i in range(NT)]
    NSLAB = 3
    slab_base = top[0]
    slab = [alloc(4096, BF16, [128, KC, 512]) for i in range(NSLAB)]
    B_slab = [Buf(f"slab{i}") for i in range(NSLAB)]
    phase_base = top[0]

    xt = [alloc(2048) for i in range(2)]
    B_xt = [Buf(f"xt{i}") for i in range(2)]
    junk = alloc(1024, BF16)
    B_junk = Buf("junk")
    ssq = alloc(2)
    rstd = alloc(2)
    B_st = [Buf("st0"), Buf("st1")]
    xn = [alloc(1024, BF16) for i in range(2)]
    B_xn = [Buf("xn0"), Buf("xn1")]

    for i in range(NT):
        s_ = i % 2
        sc.dma("sp", lambda e, i=i, s_=s_: e.dma_start(out=xt[s_], in_=x[i * 128:(i + 1) * 128, :]),
               f"d_xt{s_}", writes=[B_xt[s_]])
        sc.op("act", lambda e, s_=s_: e.activation(out=junk, in_=xt[s_], func=AF.Square,
                                                   accum_out=ssq[:, s_:s_ + 1]),
              reads=[B_xt[s_]], writes=[B_junk, B_st[s_]])
        sc.op("act", lambda e, s_=s_: e.activation(out=rstd[:, s_:s_ + 1], in_=ssq[:, s_:s_ + 1], func=AF.Sqrt,
                                                   scale=1.0 / D, bias=epsc[:, 0:1]),
              reads=[B_st[s_], B_const], writes=[B_st[s_]])
        sc.op("dve", lambda e, s_=s_: e.reciprocal(out=rstd[:, s_:s_ + 1], in_=rstd[:, s_:s_ + 1]),
              reads=[B_st[s_]], writes=[B_st[s_]])
        sc.op("dve", lambda e, s_=s_: e.tensor_scalar(out=xn[s_], in0=xt[s_], scalar1=rstd[:, s_:s_ + 1],
                                                      scalar2=None, op0=ALU.mult),
              reads=[B_xt[s_], B_st[s_]], writes=[B_xn[s_]])
        for q4 in range(4):
            bk = (i * 4 + q4) % 4
            pt = pbank[bk][:].bitcast(BF16)
            for j in range(4):
                kc = q4 * 4 + j
                sc.op("pe", lambda e, pt=pt, j=j, kc=kc, s_=s_: e.transpose(
                    out=pt[:, j * 128:(j + 1) * 128], in_=xn[s_][:, kc * 128:(kc + 1) * 128], identity=ident_b),
                    reads=[B_xn[s_], B_const], writes=[B_pb[bk]])
            for j in range(4):
                kc = q4 * 4 + j
                sc.op("act", lambda e, pt=pt, j=j, kc=kc, i=i: e.activation(
                    out=hT[:, kc, i * 128:(i + 1) * 128], in_=pt[:, j * 128:(j + 1) * 128],
                    func=AF.Copy, scale=g1[:, kc:kc + 1]),
                    reads=[B_pb[bk], B_const], writes=[B_hT[i]])

    slab_ctr = [0]
    nslab = [NSLAB]

    def load_slab(pieces, kc_n=KC):
        s_ = slab_ctr[0] % nslab[0]
        slab_ctr[0] += 1
        for ap_, c0, n in pieces:
            sc.dma("pool", lambda e, ap_=ap_, c0=c0, n=n: e.dma_start(
                out=slab[s_][:, 0:kc_n, c0:c0 + n], in_=ap_.rearrange("(kc p) n -> p kc n", p=128)),
                f"d_slab{s_}", writes=[B_slab[s_]])
        return s_

    cvt_list = [(e_, m_) for e_ in range(64) for m_ in range(3)] if stage == "full" else []
    B_wbf = Buf("wbf")
    w_exp = (w_exp_gate, w_exp_up, w_exp_down)

    CVT_DEPTH = 5

    def issue_cvt(n):
        for _ in range(n):
            if not cvt_list:
                return
            e_, m_ = cvt_list.pop(0)
            n_issued = sc.dma_cnt.get("d_cvt", 0) // 16
            if n_issued >= CVT_DEPTH:
                v_ = 16 * (n_issued - CVT_DEPTH + 1)
                if sc.seen["pool"].get("d_cvt", 0) < v_:
                    sc.seen["pool"]["d_cvt"] = v_
                    sc.q["pool"].append(("wait", "d_cvt", v_))
            sc.dma("pool", lambda e, e_=e_, m_=m_: e.dma_start(
                out=wbf_dram[e_, m_].rearrange("(a b) -> a b", b=w_exp[m_].shape[2]), in_=w_exp[m_][e_]),
                "d_cvt", writes=[B_wbf])

    pb_ctr = [0]

    def next_bank(lo=0, n=4):
        b = lo + pb_ctr[0] % n
        pb_ctr[0] += 1
        return b

    def mm_fm(s_, c0, tc, bk):
        for kc in range(KC):
            sc.op("pe", lambda e, kc=kc: e.matmul(
                pbank[bk][:], lhsT=slab[s_][:, kc, c0:c0 + 128],
                rhs=hT[:, kc, tc * 512:(tc + 1) * 512], start=(kc == 0), stop=(kc == KC - 1)),
                reads=[B_slab[s_]] + B_hT[tc * 4:(tc + 1) * 4], writes=[B_pb[bk]])

    def mm_tm(s_, c0, n, i, bk):
        for kc in range(KC):
            sc.op("pe", lambda e, kc=kc: e.matmul(
                pbank[bk][:, 0:n], lhsT=hT[:, kc, i * 128:(i + 1) * 128],
                rhs=slab[s_][:, kc, c0:c0 + n], start=(kc == 0), stop=(kc == KC - 1)),
                reads=[B_slab[s_], B_hT[i]], writes=[B_pb[bk]])

    sc.barrier()
    top[0] = phase_base
    GA0 = 4 * 2048 + 3 * 1536
    sgrow = [alloc(1024, BF16) for i in range(2)]
    B_sgrow = [Buf("sgrow0"), Buf("sgrow1")]
    B_sgd = Buf("sg_dram")
    rowctr = 0
    if stage not in ("dbg_ya", "dbg_yb"):
        for sl in range(8):
            s_ = load_slab([(w_in[:, GA0 + sl * 512:GA0 + (sl + 1) * 512], 0, 512)])
            issue_cvt(3)
            for fb in range(4):
                r_ = rowctr % 2
                rowctr += 1
                for tc in range(4):
                    bk = next_bank()
                    mm_fm(s_, fb * 128, tc, bk)
                    sc.op("act", lambda e, bk=bk, r_=r_, tc=tc: e.activation(
                        out=sgrow[r_][:, tc * 512:(tc + 1) * 512], in_=pbank[bk][:], func=AF.Sigmoid),
                        reads=[B_pb[bk]], writes=[B_sgrow[r_]])
                n0 = sl * 512 + fb * 128
                sc.dma("sp", lambda e, r_=r_, n0=n0: e.dma_start(out=sg_dram[n0:n0 + 128, :], in_=sgrow[r_]),
                       f"d_sgrow{r_}", reads=[B_sgrow[r_]], writes=[B_sgd])

    sc.barrier()
    top[0] = phase_base
    NH = 16 if stage not in ("dbg_ya", "dbg_yb") else (3 if stage == "dbg_ya" else 0)
    qT = [alloc(1024, BF16) for _ in range(2)]
    ktT = [alloc(1024, BF16) for _ in range(2)]
    itok = [alloc(1024, BF16, [128, NT, 128]) for _ in range(2)]
    sgtok = [alloc(1024, BF16, [128, NT, 128]) for _ in range(2)]
    B_qT = [[Buf(f"qT{s}_{c}") for c in range(4)] for s in range(2)]
    B_ktT = [[Buf(f"ktT{s}_{c}") for c in range(4)] for s in range(2)]
    B_itok = [[Buf(f"itok{s}_{c}") for c in range(NT)] for s in range(2)]
    B_sgtok = [[Buf(f"sgtok{s}_{c}") for c in range(NT)] for s in range(2)]
    Bc = alloc(2048)
    B_Bc = Buf("Bc")
    t_sig, t_kk, t_lf, t_b, t_eq, t_ek = [alloc(512) for _ in range(6)]
    B_t = {n: Buf(n) for n in ("sig", "kk", "lf", "b", "eq", "ek")}
    Dm = [alloc(16) for _ in range(2)]
    B_Dm = [Buf("Dm0"), Buf("Dm1")]
    dref = alloc(16)
    Sf = alloc(128)
    Stmp = alloc(128)
    Sbf = alloc(64, BF16)
    B_S = Buf("S")
    B_Sbf = Buf("Sbf")
    sctmp = alloc(128)
    scT = alloc(64, BF16)
    B_scT = Buf("scT")
    ktok = alloc(64, BF16)
    B_ktok = Buf("ktok")
    hss = alloc(1)
    hrs = alloc(1)
    B_hs = Buf("hs")
    hjunk = alloc(64, BF16)
    ytok = alloc(64, BF16)
    B_ytok = Buf("ytok")
    yaT = [alloc(1024, BF16) for _ in range(2)]
    B_yaT = [Buf("yaT0"), Buf("yaT1")]
    B_yad = Buf("ya_dram")
    pS_sc = pbank[4][:, 0:128]
    pS_tr = pbank[5][:].bitcast(BF16)
    pS_o = pbank[6][:, 0:128]
    pS_st = pbank[7][:, 0:128]
    B_psc, B_ptr1, B_ptr2, B_po, B_pst = Buf("psc"), Buf("ptr1"), Buf("ptr2"), Buf("po"), Buf("pst")

    def hg_inproj_units(h):
        sl = h % 2
        units = []
        holder = {}

        def u_load():
            holder["s"] = load_slab([(w_in[:, h * 128:(h + 1) * 128], 0, 128),
                                     (w_in[:, 2048 + h * 128:2048 + (h + 1) * 128], 128, 128),
                                     (w_in[:, 4096 + h * 128:4096 + (h + 1) * 128], 256, 128),
                                     (w_in[:, 6144 + h * 128:6144 + (h + 1) * 128], 384, 128)])
            issue_cvt(6)
        units.append(u_load)

        def u_q(tc):
            def f():
                s_ = holder["s"]
                bk = next_bank()
                mm_fm(s_, 0, tc, bk)
                sc.op("act", lambda e: e.activation(out=qT[sl][:, tc * 512:(tc + 1) * 512], in_=pbank[bk][:],
                                                    func=AF.Copy),
                      reads=[B_pb[bk]], writes=[B_qT[sl][tc]])
            return f

        def u_f(tc):
            def f():
                s_ = holder["s"]
                bk = next_bank()
                mm_fm(s_, 128, tc, bk)
                cs = slice(tc * 512, (tc + 1) * 512)
                sc.op("act", lambda e: e.activation(out=t_sig, in_=pbank[bk][:], func=AF.Exp, scale=-1.0),
                      reads=[B_pb[bk]], writes=[B_t["sig"]])
                sc.op("dve", lambda e: e.tensor_scalar(out=t_sig, in0=t_sig, scalar1=1.0, scalar2=None, op0=ALU.add),
                      reads=[B_t["sig"]], writes=[B_t["sig"]])
                sc.op("dve", lambda e: e.reciprocal(out=t_sig, in_=t_sig), reads=[B_t["sig"]], writes=[B_t["sig"]])
                sc.op("dve", lambda e: e.tensor_scalar(out=t_kk, in0=t_sig, scalar1=nomlb[:, h:h + 1],
                                                       scalar2=omlb[:, h:h + 1], op0=ALU.mult, op1=ALU.add),
                      reads=[B_t["sig"], B_const], writes=[B_t["kk"]])
                sc.op("act", lambda e: e.activation(out=t_lf, in_=t_sig, func=AF.Ln, scale=omlb[:, h:h + 1],
                                                    bias=lbT[:, h:h + 1]),
                      reads=[B_t["sig"], B_const], writes=[B_t["lf"]])
                init = 0.0 if tc == 0 else Bc[:, tc * 512 - 1:tc * 512]
                sc.op("dve", lambda e: e.tensor_tensor_scan(out=Bc[:, cs], data0=t_lf, data1=zeros512,
                                                            initial=init, op0=ALU.add, op1=ALU.add),
                      reads=[B_t["lf"], B_const, B_Bc], writes=[B_Bc])
                bview = Bc[:, cs].rearrange("p (a b) -> p a b", b=128)
                sc.op("dve", lambda e: e.tensor_tensor(
                    out=t_b.rearrange("p (a b) -> p a b", b=128), in0=bview,
                    in1=bview[:, :, 63:64].to_broadcast([128, 4, 128]), op=ALU.subtract),
                    reads=[B_Bc], writes=[B_t["b"]])
                sc.op("act", lambda e: e.activation(out=t_eq, in_=t_b, func=AF.Exp), reads=[B_t["b"]], writes=[B_t["eq"]])
                sc.op("act", lambda e: e.activation(out=t_ek, in_=t_b, func=AF.Exp, scale=-1.0),
                      reads=[B_t["b"]], writes=[B_t["ek"]])
                sc.op("dve", lambda e: e.tensor_tensor(out=qT[sl][:, cs], in0=qT[sl][:, cs], in1=t_eq, op=ALU.mult),
                      reads=[B_t["eq"], B_qT[sl][tc]], writes=[B_qT[sl][tc]])
                sc.op("dve", lambda e: e.tensor_tensor(out=ktT[sl][:, cs], in0=t_kk, in1=t_ek, op=ALU.mult),
                      reads=[B_t["ek"], B_t["kk"]], writes=[B_ktT[sl][tc]])
                if tc == 3:
                    refs = Bc.rearrange("p (a b) -> p a b", b=128)[:, :, 63]
                    sc.op("dve", lambda e: e.tensor_tensor(out=dref[:, 0:15], in0=refs[:, 1:16], in1=refs[:, 0:15],
                                                           op=ALU.subtract),
                          reads=[B_Bc], writes=[B_t["b"]])
                    sc.op("act", lambda e: e.activation(out=Dm[sl][:, 0:15], in_=dref[:, 0:15], func=AF.Exp),
                          reads=[B_t["b"]], writes=[B_Dm[sl]])
            return f

        def u_ig(i):
            def f():
                s_ = holder["s"]
                bk = next_bank()
                mm_tm(s_, 256, 256, i, bk)
                sc.op("act", lambda e: e.activation(out=itok[sl][:, i, :], in_=pbank[bk][:, 0:128], func=AF.Copy),
                      reads=[B_pb[bk]], writes=[B_itok[sl][i]])
                sc.op("act", lambda e: e.activation(out=sgtok[sl][:, i, :], in_=pbank[bk][:, 128:256], func=AF.Exp, scale=-1.0),
                      reads=[B_pb[bk]], writes=[B_sgtok[sl][i]])
                if i == NT - 1:
                    sc.op("dve", lambda e: e.tensor_scalar(out=sgtok[sl], in0=sgtok[sl], scalar1=1.0, scalar2=None, op0=ALU.add),
                          reads=B_sgtok[sl], writes=B_sgtok[sl])
                    def _rcp(e):
                        with nc.allow_low_precision("bf16 storage of the sigmoid gate"):
                            return e.reciprocal(out=sgtok[sl], in_=sgtok[sl])
                    sc.op("dve", _rcp, reads=B_sgtok[sl], writes=B_sgtok[sl])
            return f
        for tc in range(4):
            units.append(u_q(tc))
        for tc in range(4):
            units.append(u_f(tc))
        for i in range(NT):
            units.append(u_ig(i))
        return units

    scT2 = [scT, alloc(64, BF16)]
    ktok2 = [ktok, alloc(64, BF16)]
    ytok2 = [ytok, alloc(64, BF16)]
    sctmp2 = [sctmp, alloc(128)]
    hss2 = [hss, alloc(1)]
    hrs2 = [hrs, alloc(1)]
    B_scT2 = [Buf("scT0"), Buf("scT1")]
    B_ktok2 = [Buf("ktok0"), Buf("ktok1")]
    B_ytok2 = [Buf("ytok0"), Buf("ytok1")]
    B_hs2 = [Buf("hs0"), Buf("hs1")]
    pSsc2 = [pbank[4][:, 0:128], pbank[4][:, 128:256]]
    pStr_k = [pS_tr[:, 0:128], pS_tr[:, 128:256]]
    pStr_y = [pS_tr[:, 256:384], pS_tr[:, 384:512]]
    pSo2 = [pbank[6][:, 0:128], pbank[6][:, 128:256]]
    pSst2 = [pbank[7][:, 0:128], pbank[7][:, 128:256]]
    pStr_y = [pbank[7][:].bitcast(BF16)[:, 0:128], pbank[7][:].bitcast(BF16)[:, 128:256]]
    pSst2 = [pbank[6][:, 0:128], pbank[6][:, 128:256]]
    pSo2 = [pbank[5][:, 0:128], pbank[5][:, 128:256]]
    pStr_k = [pbank[4][:].bitcast(BF16)[:, 512:640], pbank[4][:].bitcast(BF16)[:, 640:768]]
    B_psc2 = [B_pb[4], B_pb[4]]
    B_ptrk = [B_pb[4], B_pb[4]]
    B_po2 = [B_pb[5], B_pb[5]]
    B_pst2 = [B_pb[6], B_pb[6]]
    B_ptry = [B_pb[7], B_pb[7]]

    def hg_rec_steps(h):
        sl = h % 2

        def stA(i):
            d = i % 2
            ts = slice(i * 128, (i + 1) * 128)
            tc = i // 4
            sc.op("pe", lambda e: e.matmul(pSsc2[d], lhsT=ktT[sl][:, ts], rhs=qT[sl][:, ts], start=True, stop=True),
                  reads=[B_ktT[sl][tc], B_qT[sl][tc]], writes=[B_psc2[d]])
            if i < NT - 1:
                sc.op("pe", lambda e: e.transpose(out=pStr_k[d], in_=ktT[sl][:, ts], identity=ident_b),
                      reads=[B_ktT[sl][tc], B_const], writes=[B_ptrk[d]])
            sc.op("dve", lambda e: e.tensor_scalar(out=sctmp2[d], in0=pSsc2[d], scalar1=1e30, scalar2=-1e30,
                                                   op0=ALU.min, op1=ALU.max),
                  reads=[B_psc2[d]], writes=[B_scT2[d], B_psc2[d]])
            sc.op("dve", lambda e: e.tensor_tensor(out=scT2[d], in0=sctmp2[d], in1=mask_ut, op=ALU.mult),
                  reads=[B_scT2[d], B_const], writes=[B_scT2[d]])
            if i < NT - 1:
                sc.op("act", lambda e: e.activation(out=ktok2[d], in_=pStr_k[d], func=AF.Copy),
                      reads=[B_ptrk[d]], writes=[B_ktok2[d], B_ptrk[d]])

        def stB(i):
            d = i % 2
            ts = slice(i * 128, (i + 1) * 128)
            tc = i // 4
            sc.op("pe", lambda e: e.matmul(pSo2[d], lhsT=scT2[d], rhs=itok[sl][:, i, :], start=True, stop=(i == 0)),
                  reads=[B_scT2[d], B_itok[sl][i]], writes=[B_po2[d]])
            if i > 0:
                sc.op("pe", lambda e: e.matmul(pSo2[d], lhsT=qT[sl][:, ts], rhs=Sbf, start=False, stop=True),
                      reads=[B_qT[sl][tc], B_Sbf], writes=[B_po2[d]])
            if i < NT - 1:
                sc.op("pe", lambda e: e.matmul(pSst2[d], lhsT=ktok2[d], rhs=itok[sl][:, i, :], start=True, stop=True),
                      reads=[B_ktok2[d], B_itok[sl][i]], writes=[B_pst2[d]])
                if i == 0:
                    sc.op("dve", lambda e: e.tensor_scalar(out=Sf, in0=pSst2[d], scalar1=Dm[sl][:, 0:1], scalar2=None,
                                                           op0=ALU.mult),
                          reads=[B_pst2[d], B_Dm[sl]], writes=[B_S, B_pst2[d]])
                else:
                    sc.op("dve", lambda e: e.tensor_scalar(out=Stmp, in0=Sf, scalar1=Dm[sl][:, i:i + 1], scalar2=None,
                                                           op0=ALU.mult),
                          reads=[B_S, B_Dm[sl]], writes=[B_S])
                    sc.op("dve", lambda e: e.scalar_tensor_tensor(out=Sf, in0=pSst2[d], scalar=Dm[sl][:, i:i + 1],
                                                                  in1=Stmp, op0=ALU.mult, op1=ALU.add),
                          reads=[B_pst2[d], B_S, B_Dm[sl]], writes=[B_S, B_pst2[d]])
                sc.op("act", lambda e: e.activation(out=Sbf, in_=Sf, func=AF.Copy), reads=[B_S], writes=[B_Sbf])
            sc.op("act", lambda e: e.activation(out=hjunk, in_=pSo2[d], func=AF.Square, accum_out=hss2[d]),
                  reads=[B_po2[d]], writes=[B_hs2[d], B_po2[d]])
            sc.op("act", lambda e: e.activation(out=hrs2[d], in_=hss2[d], func=AF.Ln, scale=1.0 / 128, bias=epsc[:, 0:1]),
                  reads=[B_hs2[d], B_const], writes=[B_hs2[d]])
            sc.op("act", lambda e: e.activation(out=hrs2[d], in_=hrs2[d], func=AF.Exp, scale=-0.5),
                  reads=[B_hs2[d]], writes=[B_hs2[d]])
            sc.op("dve", lambda e: e.scalar_tensor_tensor(out=ytok2[d], in0=pSo2[d], scalar=hrs2[d][:, 0:1],
                                                          in1=sgtok[sl][:, i, :], op0=ALU.mult, op1=ALU.mult),
                  reads=[B_po2[d], B_hs2[d], B_sgtok[sl][i]], writes=[B_ytok2[d], B_po2[d]])

        def stC(i):
            d = i % 2
            ts = slice(i * 128, (i + 1) * 128)
            sc.op("pe", lambda e: e.transpose(out=pStr_y[d], in_=ytok2[d], identity=ident_b),
                  reads=[B_ytok2[d], B_const], writes=[B_ptry[d]])
            sc.op("act", lambda e: e.activation(out=yaT[sl][:, ts], in_=pStr_y[d], func=AF.Copy,
                                                scale=gnT[:, h:h + 1]),
                  reads=[B_ptry[d], B_const], writes=[B_yaT[sl], B_ptry[d]])
            if i == NT - 1:
                sc.dma("sp", lambda e: e.dma_start(out=ya_dram[h * 128:(h + 1) * 128, :], in_=yaT[sl]),
                       f"d_yaT{sl}", reads=[B_yaT[sl]], writes=[B_yad])

        def slot(u):
            def f():
                if u < NT:
                    stA(u)
                if 0 <= u - 1 < NT:
                    stB(u - 1)
                if 0 <= u - 2 < NT:
                    stC(u - 2)
            return f
        return [slot(u) for u in range(NT + 2)]

    prev_steps = []
    for h in range(NH + 1):
        units = hg_inproj_units(h) if h < NH else []
        n = max(len(units), len(prev_steps))
        for u in range(n):
            if u < len(units):
                units[u]()
            if u < len(prev_steps):
                prev_steps[u]()
        prev_steps = hg_rec_steps(h) if h < NH else []

    sc.barrier()
    top[0] = phase_base
    ATT_G = ((128, 1), (512, 4), (2048, 16))
    DMAX = (1, 4, 15)
    JW = (256, 640, 2048)
    WW = tuple(j + 127 for j in JW)
    WOFF = (0, WW[0], WW[0] + WW[1])
    NSLOT = 4 if stage != "dbg_yb" else 1
    rb33 = alloc(12)
    rbx = alloc(12 * 128, F32, [128, 12, 128])
    ohs = alloc(WW[0] + WW[1] + WW[2])
    zst = alloc(1088, BF16)
    B_att = Buf("attc")
    B_zst = Buf("zst")
    B_zd = Buf("zrow_dram")
    Btoe = [[alloc(JW[g] // 2, BF16) for s in range(4)] for g in range(3)]
    B_toe = Buf("toe")
    sc.dma("sp", lambda e: e.dma_start(out=rb33[0:32, :], in_=rel_bias), "d_const", writes=[B_att])
    sc.op("dve", lambda e: e.memset(rb33[32:33, :], NEG), writes=[B_att])
    sc.dma("sp", lambda e: e.dma_start(out=ohs[0:33, :], in_=att_oh), "d_const", writes=[B_att])
    sc.op("dve", lambda e: e.tensor_copy(out=rbx[0:33, :, :], in_=rb33[0:33, :].unsqueeze(2).to_broadcast([33, 12, 128])),
          reads=[B_att], writes=[B_att])
    for g in range(3):
        for s in range(NSLOT):
            hd = g * 4 + s
            W = WW[g]
            for c0 in range(0, W, 512):
                n = min(512, W - c0)
                sc.op("pe", lambda e, c0=c0, n=n, hd=hd, g=g: e.matmul(
                    pbank[7][:, 0:n], lhsT=rbx[0:33, hd, :], rhs=ohs[0:33, WOFF[g] + c0:WOFF[g] + c0 + n],
                    start=True, stop=True), reads=[B_att], writes=[B_pb[7]])
                sc.op("act", lambda e, c0=c0, n=n: e.activation(out=zst[:, c0:c0 + n], in_=pbank[7][:, 0:n], func=AF.Copy),
                      reads=[B_pb[7]], writes=[B_zst])
            zoff = hd * 128 * 2176
            sc.dma("sp", lambda e, W=W, zoff=zoff: e.dma_start(
                out=zrow_dram[zoff:zoff + 128 * W].rearrange("(c w) -> c w", w=W), in_=zst[:, 0:W]),
                "d_zst", reads=[B_zst], writes=[B_zd])
            src = bass.AP(tensor=zrow_dram.tensor, offset=zrow_dram.offset + zoff + 127,
                          ap=[[W - 1, 128], [1, JW[g]]])
            sc.dma("sp", lambda e, src=src, g=g, s=s: e.dma_start(out=Btoe[g][s], in_=src),
                   "d_toe", reads=[B_zd], writes=[B_toe])

    qTs = [alloc(1024, BF16) for g in range(3)]
    kTs = [alloc(1024, BF16) for g in range(3)]
    vtok = alloc(16 * 3 * 130 // 2, BF16).rearrange("p (t g c) -> p t g c", g=3, c=130)
    B_qTs = [[Buf(f"qTs{g}_{c}") for c in range(4)] for g in range(3)]
    B_kTs = [[Buf(f"kTs{g}_{c}") for c in range(4)] for g in range(3)]
    B_vtok = [Buf(f"vtok{i}") for i in range(NT)]
    PT = [alloc(256, BF16) for _ in range(2)]
    B_PT = [Buf("PT0"), Buf("PT1")]
    rden = alloc(1)
    B_rden = Buf("rden")
    obt = alloc(64, BF16)
    B_obt = Buf("obt")
    ybT = alloc(1024, BF16)
    B_ybT = Buf("ybT")
    B_ybd = Buf("yb_dram")
    sc.op("dve", lambda e: e.memset(vtok[:, :, :, 128:130], 1.0), writes=B_vtok)
    po_ap = [pbank[4 + j][:, 0:129] for j in range(4)]
    B_poa = [B_pb[4 + j] for j in range(4)]
    pt_ctr = 0
    AQ0 = 8192
    for s in range(NSLOT):
        pieces_q = [(w_in[:, AQ0 + (g * 4 + s) * 128:AQ0 + (g * 4 + s + 1) * 128], g * 128, 128) for g in range(3)]
        pieces_k = [(w_in[:, AQ0 + 1536 + (g * 4 + s) * 128:AQ0 + 1536 + (g * 4 + s + 1) * 128], g * 128, 128) for g in range(3)]
        pieces_v = [(w_in[:, AQ0 + 3072 + (g * 4 + s) * 128:AQ0 + 3072 + (g * 4 + s + 1) * 128], g * 128, 128) for g in range(3)]
        s_q = load_slab(pieces_q)
        s_k = load_slab(pieces_k)
        s_v = load_slab(pieces_v)
        issue_cvt(18)

        def inproj_units(tc, s_q=s_q, s_k=s_k, s_v=s_v):
            us = []
            for g in range(3):
                def uq(g=g):
                    bk = next_bank()
                    mm_fm(s_q, g * 128, tc, bk)
                    sc.op("act", lambda e: e.activation(
                        out=qTs[g][:, tc * 512:(tc + 1) * 512], in_=pbank[bk][:], func=AF.Copy, scale=128.0 ** -0.5),
                        reads=[B_pb[bk]], writes=[B_qTs[g][tc]])
                us.append(uq)

                def uk(g=g):
                    bk = next_bank()
                    mm_fm(s_k, g * 128, tc, bk)
                    sc.op("dve", lambda e: e.tensor_copy(out=kTs[g][:, tc * 512:(tc + 1) * 512], in_=pbank[bk][:]),
                          reads=[B_pb[bk]], writes=[B_kTs[g][tc]])
                us.append(uk)
            for i in range(4 * tc, 4 * tc + 4):
                def uv(i=i):
                    bk = next_bank()
                    mm_tm(s_v, 0, 384, i, bk)
                    sc.op("act", lambda e: e.activation(
                        out=vtok[:, i, :, 0:128], in_=pbank[bk][:, 0:384].rearrange("p (g c) -> p g c", c=128), func=AF.Copy),
                        reads=[B_pb[bk]], writes=[B_vtok[i]])
                us.append(uv)
            return us

        for u_ in inproj_units(0):
            u_()
        for Q in range(4):
            pend = inproj_units(Q + 1) if Q < 3 else []
            contribs = []
            for g in range(3):
                for m in range(max(0, 4 * Q - DMAX[g]), 4 * Q + 4):
                    T_lo = max(m, 4 * Q)
                    T_hi = min(4 * Q + 3, m + DMAX[g])
                    if T_hi >= T_lo:
                        contribs.append((g, m, T_lo, T_hi))
            firstT, lastT = {}, {}
            for ci, (g, m, T_lo, T_hi) in enumerate(contribs):
                for T in range(T_lo, T_hi + 1):
                    firstT.setdefault(T, ci)
                    lastT[T] = ci
            every = max(1, len(contribs) // (len(pend) + 1)) if pend else 0
            for ci, (g, m, T_lo, T_hi) in enumerate(contribs):
                if pend and ci % every == every - 1:
                    pend.pop(0)()
                ncols = (T_hi - T_lo + 1) * 128
                bk = next_bank()
                qbufs = [B_qTs[g][T // 4] for T in range(T_lo, T_hi + 1)]
                sc.op("pe", lambda e, g=g, m=m, T_lo=T_lo, T_hi=T_hi, ncols=ncols, bk=bk: e.matmul(
                    pbank[bk][:, 0:ncols], lhsT=kTs[g][:, m * 128:(m + 1) * 128],
                    rhs=qTs[g][:, T_lo * 128:(T_hi + 1) * 128], start=True, stop=False),
                    reads=[B_kTs[g][m // 4]] + qbufs, writes=[B_pb[bk]])
                sc.op("pe", lambda e, g=g, m=m, T_lo=T_lo, T_hi=T_hi, ncols=ncols, bk=bk, s=s: e.matmul(
                    pbank[bk][:, 0:ncols], lhsT=ident_b,
                    rhs=Btoe[g][s][:, (T_lo - m) * 128:(T_hi - m + 1) * 128], start=False, stop=True),
                    reads=[B_toe, B_const], writes=[B_pb[bk]])
                p_ = pt_ctr % 2
                pt_ctr += 1
                sc.op("act", lambda e, p_=p_, ncols=ncols, bk=bk: e.activation(
                    out=PT[p_][:, 0:ncols], in_=pbank[bk][:, 0:ncols], func=AF.Exp),
                    reads=[B_pb[bk]], writes=[B_PT[p_]])
                for T in range(T_lo, T_hi + 1):
                    j = T - 4 * Q
                    st_, sp_ = (firstT[T] == ci), (lastT[T] == ci)
                    sc.op("pe", lambda e, p_=p_, T=T, T_lo=T_lo, j=j, m=m, g=g, st_=st_, sp_=sp_: e.matmul(
                        po_ap[j], lhsT=PT[p_][:, (T - T_lo) * 128:(T - T_lo + 1) * 128], rhs=vtok[:, m, g, 0:129],
                        start=st_, stop=sp_),
                        reads=[B_PT[p_], B_vtok[m]], writes=[B_poa[j]])
            while pend:
                pend.pop(0)()
            for j in range(4):
                T = 4 * Q + j
                sc.op("dve", lambda e, j=j: e.reciprocal(out=rden, in_=po_ap[j][:, 128:129]),
                      reads=[B_poa[j]], writes=[B_rden])
                sc.op("dve", lambda e, j=j: e.tensor_scalar(out=obt, in0=po_ap[j][:, 0:128], scalar1=rden[:, 0:1],
                                                            scalar2=None, op0=ALU.mult),
                      reads=[B_poa[j], B_rden], writes=[B_obt])
                bk = next_bank()
                ptr_att = pbank[bk][:].bitcast(BF16)
                sc.op("pe", lambda e, ptr_att=ptr_att: e.transpose(out=ptr_att[:, 0:128], in_=obt, identity=ident_b),
                      reads=[B_obt, B_const], writes=[B_pb[bk]])
                sc.op("act", lambda e, T=T, ptr_att=ptr_att: e.activation(out=ybT[:, T * 128:(T + 1) * 128], in_=ptr_att[:, 0:128],
                                                         func=AF.Copy),
                      reads=[B_pb[bk]], writes=[B_ybT])
        sc.dma("sp", lambda e, s=s: e.dma_start(out=yb_dram[s * 128:(s + 1) * 128, :], in_=ybT),
               "d_ybT", reads=[B_ybT], writes=[B_ybd])
    fin = [B_sgd, B_yad, B_ybd]
    if stage in ('full', 'dbg_full'):
        sc.barrier()
        top[0] = phase_base
        yaT_all = hT
        sc.dma("sp", lambda e: e.dma_start(out=yaT_all[:, 0:8, :], in_=ya_dram[0:1024, :].rearrange("(c p) t -> p c t", p=128)),
               "d_big", reads=[B_yad], writes=B_hT)
        sc.dma("sp", lambda e: e.dma_start(out=yaT_all[:, 8:16, :], in_=ya_dram[1024:2048, :].rearrange("(c p) t -> p c t", p=128)),
               "d_big", reads=[B_yad], writes=B_hT)
        mergedT = alloc(16384, BF16, [128, KC, S])
        ybT_all = alloc(4096, BF16, [128, 4, S])
        B_ybTa = Buf("ybT_all")
        sc.dma("sp", lambda e: e.dma_start(out=ybT_all, in_=yb_dram.rearrange("(c p) t -> p c t", p=128)),
               "d_big", reads=[B_ybd], writes=[B_ybTa])
        B_mT = [Buf(f"mT{c}") for c in range(4)]
        sga = alloc(1024, BF16)
        sgb = alloc(1024, BF16)
        B_sga, B_sgb = Buf("sga"), Buf("sgb")
        tmpA = slab[2][:, 0:2, :].rearrange("p a b -> p (a b)").bitcast(F32)
        tmpB = slab[2][:, 2:4, :].rearrange("p a b -> p (a b)").bitcast(F32)
        B_tA, B_tB = Buf("tmpA"), Buf("tmpB")
        nslab[0] = 2
        slab_ctr[0] = 0
        for ns in range(4):
            s_a = load_slab([(w_branch_a[:, ns * 512:(ns + 1) * 512], 0, 512)])
            s_b = load_slab([(w_branch_b[:, ns * 512:(ns + 1) * 512], 0, 512)], kc_n=4)
            issue_cvt(200)
            for fb in range(4):
                nb = ns * 4 + fb
                sc.dma("sp", lambda e, nb=nb: e.dma_start(out=sga, in_=sg_dram[nb * 128:(nb + 1) * 128, :]),
                       "d_sga", reads=[B_sgd], writes=[B_sga])
                sc.dma("sp", lambda e, nb=nb: e.dma_start(out=sgb, in_=sg_dram[2048 + nb * 128:2048 + (nb + 1) * 128, :]),
                       "d_sgb", reads=[B_sgd], writes=[B_sgb])
                for tc in range(4):
                    bka = next_bank()
                    for kc in range(KC):
                        sc.op("pe", lambda e, kc=kc, bka=bka, s_a=s_a, fb=fb, tc=tc: e.matmul(
                            pbank[bka][:], lhsT=slab[s_a][:, kc, fb * 128:(fb + 1) * 128],
                            rhs=yaT_all[:, kc, tc * 512:(tc + 1) * 512], start=(kc == 0), stop=(kc == KC - 1)),
                            reads=[B_slab[s_a]] + B_hT[tc * 4:(tc + 1) * 4], writes=[B_pb[bka]])
                    bkb = next_bank()
                    for kc in range(4):
                        sc.op("pe", lambda e, kc=kc, bkb=bkb, s_b=s_b, fb=fb, tc=tc: e.matmul(
                            pbank[bkb][:], lhsT=slab[s_b][:, kc, fb * 128:(fb + 1) * 128],
                            rhs=ybT_all[:, kc, tc * 512:(tc + 1) * 512], start=(kc == 0), stop=(kc == 3)),
                            reads=[B_slab[s_b], B_ybTa], writes=[B_pb[bkb]])
                    cs = slice(tc * 512, (tc + 1) * 512)
                    sc.op("dve", lambda e, bka=bka, cs=cs: e.tensor_tensor(out=tmpA, in0=pbank[bka][:], in1=sga[:, cs], op=ALU.mult),
                          reads=[B_pb[bka], B_sga], writes=[B_tA])
                    sc.op("dve", lambda e, bkb=bkb, cs=cs: e.tensor_tensor(out=tmpB, in0=pbank[bkb][:], in1=sgb[:, cs], op=ALU.mult),
                          reads=[B_pb[bkb], B_sgb], writes=[B_tB])
                    sc.op("pool", lambda e, nb=nb, cs=cs: e.tensor_tensor(out=mergedT[:, nb, cs], in0=tmpA, in1=tmpB, op=ALU.add),
                          reads=[B_tA, B_tB], writes=[B_mT[tc]])

        sc.barrier()
        wo = hT
        B_wo = Buf("wo")
        for c in range(4):
            sc.dma("pool", lambda e, c=c: e.dma_start(
                out=wo[:, c * 4:(c + 1) * 4, :], in_=w_out[c * 512:(c + 1) * 512, :].rearrange("(kc p) n -> p kc n", p=128)),
                "d_big", writes=[B_wo])
        top[0] = phase_base + 16384
        g2b = alloc(2048)
        gfb = alloc(2048)
        B_gb = Buf("gb")
        bcast = lambda ap_: bass.AP(tensor=ap_.tensor, offset=ap_.offset, ap=[[0, 128], [1, ap_.shape[0]]])
        sc.dma("sp", lambda e: e.dma_start(out=g2b, in_=bcast(norm2_gain)), "d_const", writes=[B_gb])
        sc.dma("sp", lambda e: e.dma_start(out=gfb, in_=bcast(final_norm_gain)), "d_const", writes=[B_gb])
        top2 = [slab_base]

        def alloc2(words, dt=F32, shape=None):
            save = top[0]
            top[0] = top2[0]
            a = alloc(words, dt, shape)
            top2[0] = top[0]
            top[0] = save
            assert top2[0] <= slab_base + 12288
            return a
        xt2 = alloc2(2048)
        x2t = alloc2(2048)
        h2f = alloc2(2048)
        h2T = alloc2(2048, F32, [128, KC, 128])
        h2b = alloc2(1024, BF16)
        wr = alloc2(16 * 72, F32, [128, KC, 72])
        lg_all = alloc2(16 * 72, F32, [128, NT, 72])
        brow = alloc2(72)
        onesrow = alloc2(128)
        ss2 = alloc2(1)
        rs2 = alloc2(1)
        B_xt2, B_x2t, B_h2f, B_h2T, B_h2b, B_wr, B_lg, B_s2 = (Buf(n) for n in ("xt2", "x2t", "h2f", "h2T", "h2b", "wr", "lg", "s2"))
        B_x2d, B_h2d = Buf("x2_dram"), Buf("h2_dram")
        sc.dma("sp", lambda e: e.dma_start(out=wr[:, :, 0:8], in_=w_router_group.rearrange("(kc p) n -> p kc n", p=128),
                                           allow_slow_non_contiguous=True), "d_const", writes=[B_wr])
        sc.dma("sp", lambda e: e.dma_start(out=wr[:, :, 8:72], in_=w_router_expert.rearrange("(kc p) n -> p kc n", p=128),
                                           allow_slow_non_contiguous=True), "d_const", writes=[B_wr])
        sc.dma("sp", lambda e: e.dma_start(out=brow[0:1, 0:8], in_=b_router_group), "d_const", writes=[B_wr])
        sc.dma("sp", lambda e: e.dma_start(out=brow[0:1, 8:72], in_=b_router_expert), "d_const", writes=[B_wr])
        sc.op("dve", lambda e: e.memset(onesrow[0:1, :], 1.0), writes=[B_wr])
        sc.op("dve", lambda e: e.memset(h2f, 0.0), writes=[B_h2f])
        sc.op("dve", lambda e: e.memset(h2b, 0.0), writes=[B_h2b])
        sc.dma("sp", lambda e: e.dma_start(out=h2_dram[2048:2049, :], in_=h2b[0:1, :]), "d_h2b", reads=[B_h2b], writes=[B_h2d])
        B_yed = Buf("ye_dram")
        sc.dma("sp", lambda e: e.dma_start(out=ye_dram[8192:8320, :], in_=h2b), "d_h2b", reads=[B_h2b], writes=[B_yed])
        for i in range(NT):
            ts = slice(i * 128, (i + 1) * 128)
            sc.dma("sp", lambda e, ts=ts: e.dma_start(out=xt2, in_=x[ts, :]), "d_xt2", writes=[B_xt2])
            for dsl in range(4):
                for kc in range(KC):
                    sc.op("pe", lambda e, kc=kc, dsl=dsl, ts=ts: e.matmul(
                        pbank[dsl][:], lhsT=mergedT[:, kc, ts], rhs=wo[:, kc, dsl * 512:(dsl + 1) * 512],
                        start=(kc == 0), stop=(kc == KC - 1)),
                        reads=[B_mT[i // 4], B_wo], writes=[B_pb[dsl]])
                sc.op("dve", lambda e, dsl=dsl: e.tensor_tensor(out=x2t[:, dsl * 512:(dsl + 1) * 512], in0=pbank[dsl][:],
                                                               in1=xt2[:, dsl * 512:(dsl + 1) * 512], op=ALU.add),
                      reads=[B_pb[dsl], B_xt2], writes=[B_x2t])
            sc.dma("sp", lambda e, ts=ts: e.dma_start(out=x2_dram[ts, :], in_=x2t), "d_x2t", reads=[B_x2t], writes=[B_x2d])
            sc.op("act", lambda e: e.activation(out=h2f, in_=x2t, func=AF.Square, accum_out=ss2), reads=[B_x2t], writes=[B_h2f, B_s2])
            sc.op("act", lambda e: e.activation(out=rs2, in_=ss2, func=AF.Sqrt, scale=1.0 / D, bias=epsc[:, 0:1]),
                  reads=[B_s2, B_const], writes=[B_s2])
            sc.op("dve", lambda e: e.reciprocal(out=rs2, in_=rs2), reads=[B_s2], writes=[B_s2])
            sc.op("dve", lambda e: e.scalar_tensor_tensor(out=h2f, in0=x2t, scalar=rs2[:, 0:1], in1=g2b, op0=ALU.mult, op1=ALU.mult),
                  reads=[B_x2t, B_s2, B_gb], writes=[B_h2f])
            sc.op("act", lambda e: e.activation(out=h2b, in_=h2f, func=AF.Copy), reads=[B_h2f], writes=[B_h2b])
            sc.dma("sp", lambda e, ts=ts: e.dma_start(out=h2_dram[ts, :], in_=h2b), "d_h2b", reads=[B_h2b], writes=[B_h2d])
            for q4 in range(4):
                bk = 4 + q4
                for j in range(4):
                    kc = q4 * 4 + j
                    sc.op("pe", lambda e, bk=bk, j=j, kc=kc: e.transpose(
                        out=pbank[bk][:, j * 128:(j + 1) * 128], in_=h2f[:, kc * 128:(kc + 1) * 128], identity=ident_f),
                        reads=[B_h2f, B_const], writes=[B_pb[bk]])
                sc.op("act" if q4 % 2 else "dve", (lambda e, bk=bk, q4=q4: e.activation(
                    out=h2T[:, q4 * 4:(q4 + 1) * 4, :], in_=pbank[bk][:].rearrange("p (a b) -> p a b", b=128), func=AF.Copy))
                    if q4 % 2 else (lambda e, bk=bk, q4=q4: e.tensor_copy(
                        out=h2T[:, q4 * 4:(q4 + 1) * 4, :], in_=pbank[bk][:].rearrange("p (a b) -> p a b", b=128))),
                    reads=[B_pb[bk]], writes=[B_h2T])
            for kc in range(KC):
                sc.op("pe", lambda e, kc=kc: e.matmul(pbank[0][:, 0:72], lhsT=h2T[:, kc, :], rhs=wr[:, kc, :],
                                                      start=(kc == 0), stop=False),
                      reads=[B_h2T, B_wr], writes=[B_pb[0]])
            sc.op("pe", lambda e: e.matmul(pbank[0][:, 0:72], lhsT=onesrow[0:1, :], rhs=brow[0:1, :], start=False, stop=True),
                  reads=[B_wr], writes=[B_pb[0]])
            sc.op("dve", lambda e, i=i: e.tensor_copy(out=lg_all[:, i, :], in_=pbank[0][:, 0:72]), reads=[B_pb[0]], writes=[B_lg])

        sc.barrier()
        top[0] = phase_base
        B_r = Buf("route")

        def R(eng, fn):
            sc.op(eng, fn, reads=[B_r, B_lg, B_const], writes=[B_r])
        lgG = lg_all[:, :, 0:8]
        lgE = lg_all[:, :, 8:72].rearrange("p t (g e) -> p t g e", e=8)
        mG, sumG, pg, m1, m2, w1 = (alloc(16) for _ in range(6))
        ohG, eG, lgin, oh1, oh2, msk = (alloc(128, F32, [128, 16, 8]) for _ in range(6))
        sel = alloc(1024).rearrange("p (t g e) -> p t g e", g=8, e=8)
        O1 = alloc(1024).rearrange("p (t g e) -> p t g e", g=8, e=8)
        O2 = alloc(1024).rearrange("p (t g e) -> p t g e", g=8, e=8)
        Osum = alloc(1024, F32, [128, 16, 64])
        Ob = alloc(512, BF16, [128, 16, 64])
        cum = alloc(1024, F32, [128, 16, 64])
        tmp64 = alloc(1024, F32, [128, 16, 64])
        gates = alloc(32, F32, [128, 16, 2])
        slot_ = alloc(32, F32, [128, 16, 2])
        eid_ = alloc(32, F32, [128, 16, 2])
        tixf = alloc(32, F32, [128, 16, 2])
        rixf = alloc(32, F32, [128, 16, 2])
        tix = alloc(32, I32, [128, 16, 2])
        rix = alloc(32, I32, [128, 16, 2])
        iota_i = alloc(64, I32)
        iota_e = alloc(64)
        tokid = alloc(16, I32)
        fill_i = alloc(65, I32)
        ones_b = alloc(64, BF16)
        mst_b = alloc(64, BF16)
        mstf = alloc(128)
        tabs = alloc(64, I32)
        bc3 = lambda a: a.unsqueeze(2).to_broadcast([128, 16, 8])
        R("dve", lambda e: e.tensor_reduce(out=mG, in_=lgG, axis=AX.X, op=ALU.max))
        R("dve", lambda e: e.tensor_tensor(out=ohG, in0=lgG, in1=bc3(mG), op=ALU.is_equal))
        R("dve", lambda e: e.tensor_tensor(out=eG, in0=lgG, in1=bc3(mG), op=ALU.subtract))
        R("act", lambda e: e.activation(out=eG, in_=eG, func=AF.Exp))
        R("dve", lambda e: e.tensor_reduce(out=sumG, in_=eG, axis=AX.X, op=ALU.add))
        R("dve", lambda e: e.reciprocal(out=pg, in_=sumG))
        R("dve", lambda e: e.tensor_tensor(out=sel, in0=lgE, in1=ohG.unsqueeze(3).to_broadcast([128, 16, 8, 8]), op=ALU.mult))
        R("dve", lambda e: e.tensor_reduce(out=lgin, in_=sel.rearrange("p t g e -> p t e g"), axis=AX.X, op=ALU.add))
        R("dve", lambda e: e.tensor_reduce(out=m1, in_=lgin, axis=AX.X, op=ALU.max))
        R("dve", lambda e: e.tensor_tensor(out=oh1, in0=lgin, in1=bc3(m1), op=ALU.is_equal))
        R("dve", lambda e: e.scalar_tensor_tensor(out=msk, in0=oh1, scalar=-1e30, in1=lgin, op0=ALU.mult, op1=ALU.add))
        R("dve", lambda e: e.tensor_reduce(out=m2, in_=msk, axis=AX.X, op=ALU.max))
        R("dve", lambda e: e.tensor_tensor(out=oh2, in0=msk, in1=bc3(m2), op=ALU.is_equal))
        R("dve", lambda e: e.tensor_tensor(out=w1, in0=m2, in1=m1, op=ALU.subtract))
        R("act", lambda e: e.activation(out=w1, in_=w1, func=AF.Exp))
        R("dve", lambda e: e.tensor_scalar(out=w1, in0=w1, scalar1=1.0, scalar2=None, op0=ALU.add))
        R("dve", lambda e: e.reciprocal(out=w1, in_=w1))
        R("dve", lambda e: e.tensor_tensor(out=gates[:, :, 0], in0=pg, in1=w1, op=ALU.mult))
        R("dve", lambda e: e.tensor_tensor(out=gates[:, :, 1], in0=pg, in1=gates[:, :, 0], op=ALU.subtract))
        for O_, oh_ in ((O1, oh1), (O2, oh2)):
            R("dve", lambda e, O_=O_: e.tensor_copy(out=O_, in_=ohG.unsqueeze(3).to_broadcast([128, 16, 8, 8])))
            R("dve", lambda e, O_=O_, oh_=oh_: e.tensor_tensor(out=O_, in0=O_, in1=oh_.unsqueeze(2).to_broadcast([128, 16, 8, 8]),
                                                             op=ALU.mult))
        O1f = O1.rearrange("p t g e -> p t (g e)")
        O2f = O2.rearrange("p t g e -> p t (g e)")
        R("dve", lambda e: e.tensor_tensor(out=Osum, in0=O1f, in1=O2f, op=ALU.add))
        R("dve", lambda e: e.tensor_copy(out=Ob, in_=Osum))
        R("dve", lambda e: e.memset(ones_b, 1.0))
        R("dve", lambda e: e.tensor_tensor(out=mstf, in0=mask_ut, in1=ident_f, op=ALU.subtract))
        R("dve", lambda e: e.tensor_copy(out=mst_b, in_=mstf))
        for i in range(NT):
            bk = 4 + (i // 8)
            cols = slice((i % 8) * 64, (i % 8 + 1) * 64)
            for j in range(i):
                sc.op("pe", lambda e, bk=bk, cols=cols, j=j: e.matmul(pbank[bk][:, cols], lhsT=ones_b, rhs=Ob[:, j, :],
                                                                     start=(j == 0), stop=False),
                      reads=[B_r], writes=[B_pb[bk]])
            sc.op("pe", lambda e, bk=bk, cols=cols, i=i: e.matmul(pbank[bk][:, cols], lhsT=mst_b, rhs=Ob[:, i, :],
                                                                 start=(i == 0), stop=True),
                  reads=[B_r], writes=[B_pb[bk]])
        sc.op("dve", lambda e: e.tensor_copy(out=cum[:, 0:8, :], in_=pbank[4][:].rearrange("p (t e) -> p t e", e=64)),
              reads=[B_pb[4], B_r], writes=[B_r])
        sc.op("dve", lambda e: e.tensor_copy(out=cum[:, 8:16, :], in_=pbank[5][:].rearrange("p (t e) -> p t e", e=64)),
              reads=[B_pb[5], B_r], writes=[B_r])
        R("pool", lambda e: e.iota(iota_i, pattern=[[1, 64]], base=0, channel_multiplier=0))
        R("pool", lambda e: e.iota(tokid, pattern=[[128, 16]], base=0, channel_multiplier=1))
        R("pool", lambda e: e.iota(fill_i, pattern=[[0, 65]], base=2048, channel_multiplier=0))
        R("dve", lambda e: e.tensor_copy(out=iota_e, in_=iota_i))
        for jj, Of in ((0, O1f), (1, O2f)):
            R("dve", lambda e, Of=Of: e.tensor_tensor(out=tmp64, in0=Of, in1=cum, op=ALU.mult))
            R("dve", lambda e, jj=jj: e.tensor_reduce(out=slot_[:, :, jj], in_=tmp64, axis=AX.X, op=ALU.add))
            R("dve", lambda e, Of=Of: e.tensor_tensor(out=tmp64, in0=Of, in1=iota_e.unsqueeze(1).to_broadcast([128, 16, 64]),
                                                     op=ALU.mult))
            R("dve", lambda e, jj=jj: e.tensor_reduce(out=eid_[:, :, jj], in_=tmp64, axis=AX.X, op=ALU.add))
        R("dve", lambda e: e.tensor_scalar(out=tixf, in0=slot_, scalar1=128.0, scalar2=None, op0=ALU.min))
        R("dve", lambda e: e.scalar_tensor_tensor(out=tixf, in0=eid_, scalar=129.0, in1=tixf, op0=ALU.mult, op1=ALU.add))
        R("dve", lambda e: e.tensor_scalar(out=rixf, in0=slot_, scalar1=128.0, scalar2=1e6, op0=ALU.is_ge, op1=ALU.mult))
        R("dve", lambda e: e.tensor_tensor(out=rixf, in0=rixf, in1=slot_, op=ALU.add))
        R("dve", lambda e: e.scalar_tensor_tensor(out=rixf, in0=eid_, scalar=128.0, in1=rixf, op0=ALU.mult, op1=ALU.add))
        R("dve", lambda e: e.tensor_scalar(out=rixf, in0=rixf, scalar1=8192.0, scalar2=None, op0=ALU.min))
        R("dve", lambda e: e.tensor_copy(out=tix, in_=tixf))
        R("dve", lambda e: e.tensor_copy(out=rix, in_=rixf))
        B_tabd = Buf("tab_dram")
        sc.dma("sp", lambda e: e.dma_start(out=tab_dram.rearrange("(p c) -> p c", c=65), in_=fill_i), "d_tab",
               reads=[B_r], writes=[B_tabd])
        tab2 = tab_dram.rearrange("(r c) -> r c", c=1)
        for i in range(NT):
            for jj in range(2):
                sc.dma("pool", lambda e, i=i, jj=jj: e.indirect_dma_start(
                    out=tab2, out_offset=bass.IndirectOffsetOnAxis(ap=tix[:, i, jj:jj + 1], axis=0),
                    in_=tokid[:, i:i + 1], in_offset=None), "d_tabs", reads=[B_r, B_tabd], writes=[B_tabd])
        B_tabs = Buf("tabs")
        tab_src = bass.AP(tensor=tab_dram.tensor, offset=tab_dram.offset, ap=[[1, 128], [129, 64]])
        sc.dma("sp", lambda e: e.dma_start(out=tabs, in_=tab_src, allow_slow_non_contiguous=True), "d_tab2",
               reads=[B_tabd], writes=[B_tabs])

        sc.barrier()
        wreg = [slab_base - 16384 + 4096 * k for k in range(6)]
        def wview(k):
            a = arena[:, wreg[k]:wreg[k] + 4096].bitcast(BF16)
            return a
        Wg = [wview(0).rearrange("p (a b) -> p a b", b=512), wview(3).rearrange("p (a b) -> p a b", b=512)]
        Wu = [wview(1).rearrange("p (a b) -> p a b", b=512), wview(4).rearrange("p (a b) -> p a b", b=512)]
        Wd = [wview(2).rearrange("p (a b) -> p a b", b=2048), wview(5).rearrange("p (a b) -> p a b", b=2048)]
        B_W = [Buf("W0"), Buf("W1")]
        xb = [alloc(1024, BF16) for _ in range(2)]
        B_xb = [Buf("xb0"), Buf("xb1")]
        xbT = alloc(1024, BF16, [128, KC, 128])
        B_xbT = Buf("xbT")
        sgu = alloc(512)
        ub = alloc(256, BF16)
        uT = alloc(256, BF16, [128, 4, 128])
        B_sgu, B_ub, B_uT = Buf("sgu"), Buf("ub"), Buf("uT")
        yeb = [alloc(1024, BF16) for _ in range(2)]
        B_yeb = [Buf("yeb0"), Buf("yeb1")]
        NE = 64 if stage == "full" else 2
        for ex in range(NE):
            p_ = ex % 2
            sc.dma("sp", lambda e, ex=ex, p_=p_: e.dma_start(
                out=Wg[p_], in_=wbf_dram[ex, 0].rearrange("(kc p n) -> p kc n", p=128, n=512)),
                f"d_W{p_}", reads=[B_wbf], writes=[B_W[p_]])
            sc.dma("sp", lambda e, ex=ex, p_=p_: e.dma_start(
                out=Wu[p_], in_=wbf_dram[ex, 1].rearrange("(kc p n) -> p kc n", p=128, n=512)),
                f"d_W{p_}", reads=[B_wbf], writes=[B_W[p_]])
            sc.dma("sp", lambda e, ex=ex, p_=p_: e.dma_start(
                out=Wd[p_], in_=wbf_dram[ex, 2].rearrange("(kc p n) -> p kc n", p=128, n=2048)),
                f"d_W{p_}", reads=[B_wbf], writes=[B_W[p_]])
            sc.dma("pool", lambda e, ex=ex, p_=p_: e.indirect_dma_start(
                out=xb[p_], out_offset=None, in_=h2_dram,
                in_offset=bass.IndirectOffsetOnAxis(ap=tabs[:, ex:ex + 1], axis=0)),
                f"d_xb{p_}", reads=[B_tabs, B_h2d], writes=[B_xb[p_]])
            for q2 in range(2):
                bk = q2
                pt = pbank[bk][:].bitcast(BF16)
                for j in range(8):
                    kc = q2 * 8 + j
                    sc.op("pe", lambda e, pt=pt, j=j, kc=kc, p_=p_: e.transpose(
                        out=pt[:, j * 128:(j + 1) * 128], in_=xb[p_][:, kc * 128:(kc + 1) * 128], identity=ident_b),
                        reads=[B_xb[p_], B_const], writes=[B_pb[bk]])
                sc.op("act" if q2 else "dve", (lambda e, pt=pt, q2=q2: e.activation(
                    out=xbT[:, q2 * 8:(q2 + 1) * 8, :], in_=pt.rearrange("p (a b) -> p a b", b=128), func=AF.Copy))
                    if q2 else (lambda e, pt=pt, q2=q2: e.tensor_copy(
                        out=xbT[:, q2 * 8:(q2 + 1) * 8, :], in_=pt.rearrange("p (a b) -> p a b", b=128))),
                    reads=[B_pb[bk]], writes=[B_xbT])
            for kc in range(KC):
                sc.op("pe", lambda e, kc=kc, p_=p_: e.matmul(pbank[2][:], lhsT=xbT[:, kc, :], rhs=Wg[p_][:, kc, :],
                                                            start=(kc == 0), stop=(kc == KC - 1)),
                      reads=[B_xbT, B_W[p_]], writes=[B_pb[2]])
            for kc in range(KC):
                sc.op("pe", lambda e, kc=kc, p_=p_: e.matmul(pbank[3][:], lhsT=xbT[:, kc, :], rhs=Wu[p_][:, kc, :],
                                                            start=(kc == 0), stop=(kc == KC - 1)),
                      reads=[B_xbT, B_W[p_]], writes=[B_pb[3]])
            sc.op("act", lambda e: e.activation(out=sgu, in_=pbank[2][:], func=AF.Silu), reads=[B_pb[2]], writes=[B_sgu])
            sc.op("dve", lambda e: e.tensor_tensor(out=ub, in0=sgu, in1=pbank[3][:], op=ALU.mult),
                  reads=[B_sgu, B_pb[3]], writes=[B_ub])
            ptu = pbank[0][:].bitcast(BF16)
            for fc in range(4):
                sc.op("pe", lambda e, fc=fc, ptu=ptu: e.transpose(out=ptu[:, fc * 128:(fc + 1) * 128],
                                                                 in_=ub[:, fc * 128:(fc + 1) * 128], identity=ident_b),
                      reads=[B_ub, B_const], writes=[B_pb[0]])
            sc.op("act", lambda e, ptu=ptu: e.activation(out=uT, in_=ptu[:, 0:512].rearrange("p (a b) -> p a b", b=128), func=AF.Copy),
                  reads=[B_pb[0]], writes=[B_uT])
            for dsl in range(4):
                bk = 4 + dsl
                for fc in range(4):
                    sc.op("pe", lambda e, fc=fc, dsl=dsl, bk=bk, p_=p_: e.matmul(
                        pbank[bk][:], lhsT=uT[:, fc, :], rhs=Wd[p_][:, fc, dsl * 512:(dsl + 1) * 512],
                        start=(fc == 0), stop=(fc == 3)),
                        reads=[B_uT, B_W[p_]], writes=[B_pb[bk]])
                if dsl % 2:
                    sc.op("act", lambda e, dsl=dsl, bk=bk, p_=p_: e.activation(out=yeb[p_][:, dsl * 512:(dsl + 1) * 512],
                                                                              in_=pbank[bk][:], func=AF.Copy),
                          reads=[B_pb[bk]], writes=[B_yeb[p_]])
                else:
                    sc.op("dve", lambda e, dsl=dsl, bk=bk, p_=p_: e.tensor_copy(out=yeb[p_][:, dsl * 512:(dsl + 1) * 512],
                                                                               in_=pbank[bk][:]),
                          reads=[B_pb[bk]], writes=[B_yeb[p_]])
            sc.dma("act", lambda e, ex=ex, p_=p_: e.dma_start(out=ye_dram[ex * 128:(ex + 1) * 128, :], in_=yeb[p_]),
                   f"d_yeb{p_}", reads=[B_yeb[p_]], writes=[B_yed])

        sc.barrier()
        top[0] = slab_base - 16384
        fx = [alloc(2048) for _ in range(2)]
        fa = [alloc(1024, BF16) for _ in range(2)]
        fb_ = [alloc(1024, BF16) for _ in range(2)]
        fo = [alloc(2048) for _ in range(2)]
        B_fx, B_fa, B_fb, B_fo = ([Buf(f"{n}{k}") for k in range(2)] for n in ("fx", "fa", "fb", "fo"))
        fss = alloc(2)
        B_fs = [Buf("fs0"), Buf("fs1")]
        B_out = Buf("out")
        for i in range(NT):
            p_ = i % 2
            ts = slice(i * 128, (i + 1) * 128)
            sc.dma("sp", lambda e, ts=ts, p_=p_: e.dma_start(out=fx[p_], in_=x2_dram[ts, :]), f"d_fx{p_}",
                   reads=[B_x2d], writes=[B_fx[p_]])
            sc.dma("pool", lambda e, i=i, p_=p_: e.indirect_dma_start(
                out=fa[p_], out_offset=None, in_=ye_dram, in_offset=bass.IndirectOffsetOnAxis(ap=rix[:, i, 0:1], axis=0)),
                f"d_fa{p_}", reads=[B_yed, B_r], writes=[B_fa[p_]])
            sc.dma("pool", lambda e, i=i, p_=p_: e.indirect_dma_start(
                out=fb_[p_], out_offset=None, in_=ye_dram, in_offset=bass.IndirectOffsetOnAxis(ap=rix[:, i, 1:2], axis=0)),
                f"d_fb{p_}", reads=[B_yed, B_r], writes=[B_fb[p_]])
            sc.op("dve", lambda e, i=i, p_=p_: e.scalar_tensor_tensor(out=fx[p_], in0=fa[p_], scalar=gates[:, i, 0:1], in1=fx[p_],
                                                                      op0=ALU.mult, op1=ALU.add),
                  reads=[B_fa[p_], B_fx[p_], B_r], writes=[B_fx[p_]])
            sc.op("dve", lambda e, i=i, p_=p_: e.scalar_tensor_tensor(out=fx[p_], in0=fb_[p_], scalar=gates[:, i, 1:2], in1=fx[p_],
                                                                      op0=ALU.mult, op1=ALU.add),
                  reads=[B_fb[p_], B_fx[p_], B_r], writes=[B_fx[p_]])
            sc.op("act", lambda e, p_=p_: e.activation(out=fo[p_], in_=fx[p_], func=AF.Square, accum_out=fss[:, p_:p_ + 1]),
                  reads=[B_fx[p_]], writes=[B_fo[p_], B_fs[p_]])
            sc.op("act", lambda e, p_=p_: e.activation(out=fss[:, p_:p_ + 1], in_=fss[:, p_:p_ + 1], func=AF.Sqrt, scale=1.0 / D,
                                                       bias=epsc[:, 0:1]),
                  reads=[B_fs[p_], B_const], writes=[B_fs[p_]])
            sc.op("dve", lambda e, p_=p_: e.reciprocal(out=fss[:, p_:p_ + 1], in_=fss[:, p_:p_ + 1]), reads=[B_fs[p_]], writes=[B_fs[p_]])
            sc.op("dve", lambda e, p_=p_: e.scalar_tensor_tensor(out=fo[p_], in0=fx[p_], scalar=fss[:, p_:p_ + 1], in1=gfb,
                                                                 op0=ALU.mult, op1=ALU.mult),
                  reads=[B_fx[p_], B_fs[p_], B_gb], writes=[B_fo[p_]])
            sc.dma("sp", lambda e, ts=ts, p_=p_: e.dma_start(out=out[ts, :], in_=fo[p_]), f"d_fo{p_}",
                   reads=[B_fo[p_]], writes=[B_out])
        fin = [B_out]

    sc.wait_all("sp", fin)

    names = sc.sem_names()
    sem_cms = [nc.semaphore(n) for n in names]
    for n, cm in zip(names, sem_cms):
        sc.sems[n] = cm.__enter__()
    with nc.Block() as block:
        sc.emit(block)
    for cm in reversed(sem_cms):
        cm.__exit__(None, None, None)
    for cm in reversed(ctxs):
        cm.__exit__(None, None, None)
    print("instr counts:", sc.cnt, "dma:", sc.ndma, "sems:", len(names), "arena top:", top[0])
    return nc


_IN_NAMES = ("norm1_gain", "w_in", "hg_norm_gain", "w_branch_a", "w_branch_b", "w_out", "norm2_gain",
             "w_router_group", "b_router_group", "w_router_expert", "b_router_expert",
             "w_exp_gate", "w_exp_up", "w_exp_down")


def make_in_map(inputs, c):
    m = {"x": np.ascontiguousarray(inputs["x"][c])}
    for k in _IN_NAMES:
        m[k] = np.ascontiguousarray(np.asarray(inputs[k])[0])
    m["hg_lb_logits"] = np.ascontiguousarray(inputs["hg_lb_logits"])
    m["rel_bias"] = np.ascontiguousarray(inputs["rel_bias"])
    m["final_norm_gain"] = np.ascontiguousarray(inputs["final_norm_gain"])
    m["att_oh"] = _att_onehot()
    return m


def kernel(**inputs):
    n = 8
    nc = build_nc()
    inputs = {k: np.asarray(v) for k, v in inputs.items()}
    in_maps = [make_in_map(inputs, c) for c in range(n)]
    res = run_bass_kernel_spmd(nc, in_maps, core_ids=list(range(n)))
    return np.stack([np.asarray(r["out"]) for r in res.results], axis=0).astype(np.float32)
```

```python
import numpy as np
import concourse.bass as bass
import concourse.mybir as mybir
from concourse.bass_utils import run_bass_kernel_spmd

F32 = mybir.dt.float32
BF16 = mybir.dt.bfloat16
I32 = mybir.dt.int32
AF = mybir.ActivationFunctionType
ALU = mybir.AluOpType
AX = mybir.AxisListType

D = 2048
S = 2048
NT = S // 128
KC = D // 128
INW = 16896
EPS = 1e-6
NEG = -30000.0
ENGS = ("pe", "act", "dve", "pool", "sp")


class Buf:
    __slots__ = ("name", "w", "r")

    def __init__(self, name):
        self.name = name
        self.w = None
        self.r = {}


class Sched:
    def __init__(self, nc):
        self.nc = nc
        self.q = {e: [] for e in ENGS}
        self.cnt = {e: 0 for e in ENGS}
        self.seen = {e: {} for e in ENGS}
        self.dma_cnt = {}
        self.sems = {}
        self.ndma = 0

    def _deps(self, eng, reads, writes):
        deps = {}
        def add(ev):
            if ev is None:
                return
            k, v = ev
            if deps.get(k, 0) < v:
                deps[k] = v
        for b in reads:
            add(b.w)
        for b in writes:
            add(b.w)
            for k, v in b.r.items():
                add((k, v))
        for k, v in deps.items():
            if k == "pe" and eng == "pe":
                continue
            if self.seen[eng].get(k, 0) >= v:
                continue
            self.seen[eng][k] = v
            self.q[eng].append(("wait", k, v))

    def _mark(self, ev, reads, writes):
        for b in writes:
            b.w = ev
            b.r = {}
        for b in reads:
            if b.r.get(ev[0], 0) < ev[1]:
                b.r[ev[0]] = ev[1]

    def op(self, eng, fn, reads=(), writes=()):
        self._deps(eng, reads, writes)
        self.cnt[eng] += 1
        ev = (eng, self.cnt[eng])
        self.q[eng].append(("op", fn, eng, 1))
        self._mark(ev, reads, writes)
        return ev

    def dma(self, eng, fn, sem, reads=(), writes=()):
        self._deps(eng, reads, writes)
        self.dma_cnt[sem] = self.dma_cnt.get(sem, 0) + 16
        ev = (sem, self.dma_cnt[sem])
        self.q[eng].append(("op", fn, sem, 16))
        self._mark(ev, reads, writes)
        self.ndma += 1
        return ev

    def barrier(self):
        keys = list(ENGS) + sorted(self.dma_cnt.keys())
        for eng in ENGS:
            for k in keys:
                if k == eng:
                    continue
                v = self.cnt[k] if k in self.cnt else self.dma_cnt[k]
                if v > self.seen[eng].get(k, 0):
                    self.seen[eng][k] = v
                    self.q[eng].append(("wait", k, v))

    def wait_all(self, eng, bufs):
        self._deps(eng, bufs, ())

    def sem_names(self):
        return list(ENGS) + sorted(self.dma_cnt.keys())

    def emit(self, block):
        nc = self.nc
        sems = self.sems

        def run(engname):
            def body(e):
                for it in self.q[engname]:
                    if it[0] == "wait":
                        e.wait_ge(sems[it[1]], it[2])
                    else:
                        ins = it[1](e)
                        ins.then_inc(sems[it[2]], it[3])
            return body
        block.tensor(run("pe"))
        block.scalar(run("act"))
        block.vector(run("dve"))
        block.gpsimd(run("pool"))
        block.sync(run("sp"))


def _t5_bucket_np(dist):
    dist = np.asarray(dist, dtype=np.int64)
    exact = 16
    d_f = np.maximum(dist, 1).astype(np.float32)
    lb = exact + (np.log(d_f / np.float32(exact)) / np.float32(np.log(2048 / exact)) * np.float32(32 - exact)).astype(np.int32)
    return np.where(dist < exact, dist, np.minimum(lb, 31))


def _att_onehot():
    groups = ((128, 1), (512, 4), (2048, 16))
    JW = (256, 640, 2048)
    cols = []
    for (win, dil), J in zip(groups, JW):
        W = J + 127
        dist = np.arange(W) - 127
        valid = (dist >= 0) & (dist % dil == 0) & (dist <= win)
        b = _t5_bucket_np(np.maximum(dist, 0))
        oh = np.zeros((33, W), np.float32)
        oh[b[valid], np.nonzero(valid)[0]] = 1.0
        oh[32, ~valid] = 1.0
        cols.append(oh)
    return np.ascontiguousarray(np.concatenate(cols, axis=1))
def build_nc(stage="full"):
    nc = bass.Bass("TRN2", target_bir_lowering=False)
    dr = lambda name, shape, dt, kind: nc.dram_tensor(name, shape, dt, kind=kind).ap()
    x = dr("x", [S, D], F32, "ExternalInput")
    norm1_gain = dr("norm1_gain", [D], F32, "ExternalInput")
    w_in = dr("w_in", [D, INW], F32, "ExternalInput")
    hg_lb_logits = dr("hg_lb_logits", [2, D], F32, "ExternalInput")
    hg_norm_gain = dr("hg_norm_gain", [D], F32, "ExternalInput")
    rel_bias = dr("rel_bias", [32, 12], F32, "ExternalInput")
    att_oh = dr("att_oh", [33, 3325], F32, "ExternalInput")
    w_branch_a = dr("w_branch_a", [D, D], F32, "ExternalInput")
    w_branch_b = dr("w_branch_b", [512, D], F32, "ExternalInput")
    w_out = dr("w_out", [D, D], F32, "ExternalInput")
    norm2_gain = dr("norm2_gain", [D], F32, "ExternalInput")
    final_norm_gain = dr("final_norm_gain", [D], F32, "ExternalInput")
    w_router_group = dr("w_router_group", [D, 8], F32, "ExternalInput")
    b_router_group = dr("b_router_group", [1, 8], F32, "ExternalInput")
    w_router_expert = dr("w_router_expert", [D, 64], F32, "ExternalInput")
    b_router_expert = dr("b_router_expert", [1, 64], F32, "ExternalInput")
    w_exp_gate = dr("w_exp_gate", [64, D, 512], F32, "ExternalInput")
    w_exp_up = dr("w_exp_up", [64, D, 512], F32, "ExternalInput")
    w_exp_down = dr("w_exp_down", [64, 512, D], F32, "ExternalInput")
    out = dr("out", [S, D], F32, "ExternalOutput")

    def scratch(name, shape, dt):
        kind = "ExternalOutput" if stage == "dbg_" + name else "Internal"
        return dr(name, shape, dt, kind)
    sg_dram = scratch("sg", [2 * D, S], BF16)
    ya_dram = scratch("ya", [D, S], BF16)

    yb_dram = scratch("yb", [512, S], BF16)
    zrow_dram = scratch("zrow", [12 * 128 * 2176], BF16)
    x2_dram = scratch("x2", [S, D], F32)
    h2_dram = scratch("h2", [S + 1, D], BF16)
    ye_dram = scratch("ye", [8192 + 128, D], BF16)
    tab_dram = scratch("tab", [8320], I32)
    wbf_parts = [scratch(f"wbf{k}", [16, 3, 2048 * 512], BF16) for k in range(4)]

    class _Wbf:
        def __getitem__(self, key):
            e_, m_ = key
            return wbf_parts[e_ // 16][e_ % 16, m_]
    wbf_dram = _Wbf()
    sc = Sched(nc)
    ctxs = []

    def sb(name, shape, dt):
        cm = nc.sbuf_tensor(name, shape, dt)
        t = cm.__enter__()
        ctxs.append(cm)
        return t

    def ps(name, shape, dt):
        cm = nc.psum_tensor(name, shape, dt)
        t = cm.__enter__()
        ctxs.append(cm)
        return t

    ARENA = 53100
    arena = sb("arena", [128, ARENA], F32)
    top = [0]

    def alloc(words, dt=F32, shape=None):
        o = top[0]
        top[0] += words
        assert top[0] <= ARENA, ("arena overflow", top[0])
        a = arena[:, o:o + words]
        if dt != F32:
            a = a.bitcast(dt)
        if shape is not None and len(shape) == 3:
            a = a.rearrange("p (a b) -> p a b", b=shape[2])
        return a

    pbank = [ps(f"pb{i}", [128, 512], F32) for i in range(8)]
    B_pb = [Buf(f"pb{i}") for i in range(8)]

    ident_f = alloc(128)
    ident_b = alloc(64, BF16)
    mask_ut = alloc(128)
    B_const = Buf("const")
    sc.op("pool", lambda e: e.memset(ident_f, 1.0), writes=[B_const])
    sc.op("pool", lambda e: e.affine_select(out=ident_f, in_=ident_f, pattern=[[-1, 128]],
                                            compare_op=ALU.is_equal, fill=0.0, base=0, channel_multiplier=1),
          reads=[B_const], writes=[B_const])
    sc.op("dve", lambda e: e.tensor_copy(out=ident_b, in_=ident_f), reads=[B_const], writes=[B_const])
    sc.op("pool", lambda e: e.memset(mask_ut, 1.0), writes=[B_const])
    sc.op("pool", lambda e: e.affine_select(out=mask_ut, in_=mask_ut, pattern=[[1, 128]],
                                            compare_op=ALU.is_ge, fill=0.0, base=0, channel_multiplier=-1),
          reads=[B_const], writes=[B_const])
    g1 = alloc(KC)
    epsc = alloc(1)
    zeros512 = alloc(512)
    lbl = alloc(32, F32, [128, 2, 16])
    lbT = alloc(16)
    omlb = alloc(16)
    nomlb = alloc(16)
    gnT = alloc(16)
    sc.dma("sp", lambda e: e.dma_start(out=g1, in_=norm1_gain.rearrange("(kc p) -> p kc", p=128),
                                       allow_slow_non_contiguous=True), "d_const", writes=[B_const])
    sc.dma("sp", lambda e: e.dma_start(out=gnT, in_=hg_norm_gain.rearrange("(kc p) -> p kc", p=128),
                                       allow_slow_non_contiguous=True), "d_const", writes=[B_const])
    sc.dma("sp", lambda e: e.dma_start(out=lbl, in_=hg_lb_logits.rearrange("r (h p) -> p r h", p=128),
                                       allow_slow_non_contiguous=True), "d_const", writes=[B_const])
    sc.op("dve", lambda e: e.memset(epsc, EPS), writes=[B_const])
    sc.op("dve", lambda e: e.memset(zeros512, 0.0), writes=[B_const])
    sc.op("dve", lambda e: e.tensor_tensor(out=lbT, in0=lbl[:, 0, :], in1=lbl[:, 1, :], op=ALU.subtract),
          reads=[B_const], writes=[B_const])
    sc.op("act", lambda e: e.activation(out=omlb, in_=lbT, func=AF.Sigmoid, scale=-1.0), reads=[B_const], writes=[B_const])
    sc.op("act", lambda e: e.activation(out=lbT, in_=lbT, func=AF.Sigmoid), reads=[B_const], writes=[B_const])
    sc.op("dve", lambda e: e.tensor_scalar(out=nomlb, in0=omlb, scalar1=-1.0, scalar2=None, op0=ALU.mult),
          reads=[B_const], writes=[B_const])

    hT = alloc(16384, BF16, [128, KC, S])
    B_hT = [Buf(f"hT{i}") for i in range(NT)]
    NSLAB = 3
    slab_base = top[0]
    slab = [alloc(4096, BF16, [128, KC, 512]) for i in range(NSLAB)]
    B_slab = [Buf(f"slab{i}") for i in range(NSLAB)]
    phase_base = top[0]

    xt = [alloc(2048) for i in range(2)]
    B_xt = [Buf(f"xt{i}") for i in range(2)]
    junk = alloc(1024, BF16)
    B_junk = Buf("junk")
    ssq = alloc(2)
    rstd = alloc(2)
    B_st = [Buf("st0"), Buf("st1")]
    xn = [alloc(1024, BF16) for i in range(2)]
    B_xn = [Buf("xn0"), Buf("xn1")]

    for i in range(NT):
        s_ = i % 2
        sc.dma("sp", lambda e, i=i, s_=s_: e.dma_start(out=xt[s_], in_=x[i * 128:(i + 1) * 128, :]),
               f"d_xt{s_}", writes=[B_xt[s_]])
        sc.op("act", lambda e, s_=s_: e.activation(out=junk, in_=xt[s_], func=AF.Square,
                                                   accum_out=ssq[:, s_:s_ + 1]),
              reads=[B_xt[s_]], writes=[B_junk, B_st[s_]])
        sc.op("act", lambda e, s_=s_: e.activation(out=rstd[:, s_:s_ + 1], in_=ssq[:, s_:s_ + 1], func=AF.Sqrt,
                                                   scale=1.0 / D, bias=epsc[:, 0:1]),
              reads=[B_st[s_], B_const], writes=[B_st[s_]])
        sc.op("dve", lambda e, s_=s_: e.reciprocal(out=rstd[:, s_:s_ + 1], in_=rstd[:, s_:s_ + 1]),
              reads=[B_st[s_]], writes=[B_st[s_]])
        sc.op("dve", lambda e, s_=s_: e.tensor_scalar(out=xn[s_], in0=xt[s_], scalar1=rstd[:, s_:s_ + 1],
                                                      scalar2=None, op0=ALU.mult),
              reads=[B_xt[s_], B_st[s_]], writes=[B_xn[s_]])
        for q4 in range(4):
            bk = (i * 4 + q4) % 4
            pt = pbank[bk][:].bitcast(BF16)
            for j in range(4):
                kc = q4 * 4 + j
                sc.op("pe", lambda e, pt=pt, j=j, kc=kc, s_=s_: e.transpose(
                    out=pt[:, j * 128:(j + 1) * 128], in_=xn[s_][:, kc * 128:(kc + 1) * 128], identity=ident_b),
                    reads=[B_xn[s_], B_const], writes=[B_pb[bk]])
            for j in range(4):
                kc = q4 * 4 + j
                sc.op("act", lambda e, pt=pt, j=j, kc=kc, i=i: e.activation(
                    out=hT[:, kc, i * 128:(i + 1) * 128], in_=pt[:, j * 128:(j + 1) * 128],
                    func=AF.Copy, scale=g1[:, kc:kc + 1]),
                    reads=[B_pb[bk], B_const], writes=[B_hT[i]])

    slab_ctr = [0]
    nslab = [NSLAB]

    def load_slab(pieces, kc_n=KC):
        s_ = slab_ctr[0] % nslab[0]
        slab_ctr[0] += 1
        for ap_, c0, n in pieces:
            sc.dma("pool", lambda e, ap_=ap_, c0=c0, n=n: e.dma_start(
                out=slab[s_][:, 0:kc_n, c0:c0 + n], in_=ap_.rearrange("(kc p) n -> p kc n", p=128)),
                f"d_slab{s_}", writes=[B_slab[s_]])
        return s_

    cvt_list = [(e_, m_) for e_ in range(64) for m_ in range(3)] if stage == "full" else []
    B_wbf = Buf("wbf")
    w_exp = (w_exp_gate, w_exp_up, w_exp_down)

    CVT_DEPTH = 5

    def issue_cvt(n):
        for _ in range(n):
            if not cvt_list:
                return
            e_, m_ = cvt_list.pop(0)
            n_issued = sc.dma_cnt.get("d_cvt", 0) // 16
            if n_issued >= CVT_DEPTH:
                v_ = 16 * (n_issued - CVT_DEPTH + 1)
                if sc.seen["pool"].get("d_cvt", 0) < v_:
                    sc.seen["pool"]["d_cvt"] = v_
                    sc.q["pool"].append(("wait", "d_cvt", v_))
            sc.dma("pool", lambda e, e_=e_, m_=m_: e.dma_start(
                out=wbf_dram[e_, m_].rearrange("(a b) -> a b", b=w_exp[m_].shape[2]), in_=w_exp[m_][e_]),
                "d_cvt", writes=[B_wbf])

    pb_ctr = [0]

    def next_bank(lo=0, n=4):
        b = lo + pb_ctr[0] % n
        pb_ctr[0] += 1
        return b

    def mm_fm(s_, c0, tc, bk):
        for kc in range(KC):
            sc.op("pe", lambda e, kc=kc: e.matmul(
                pbank[bk][:], lhsT=slab[s_][:, kc, c0:c0 + 128],
                rhs=hT[:, kc, tc * 512:(tc + 1) * 512], start=(kc == 0), stop=(kc == KC - 1)),
                reads=[B_slab[s_]] + B_hT[tc * 4:(tc + 1) * 4], writes=[B_pb[bk]])

    def mm_tm(s_, c0, n, i, bk):
        for kc in range(KC):
            sc.op("pe", lambda e, kc=kc: e.matmul(
                pbank[bk][:, 0:n], lhsT=hT[:, kc, i * 128:(i + 1) * 128],
                rhs=slab[s_][:, kc, c0:c0 + n], start=(kc == 0), stop=(kc == KC - 1)),
                reads=[B_slab[s_], B_hT[i]], writes=[B_pb[bk]])

    sc.barrier()
    top[0] = phase_base
    GA0 = 4 * 2048 + 3 * 1536
    sgrow = [alloc(1024, BF16) for i in range(2)]
    B_sgrow = [Buf("sgrow0"), Buf("sgrow1")]
    B_sgd = Buf("sg_dram")
    rowctr = 0
    if stage not in ("dbg_ya", "dbg_yb"):
        for sl in range(8):
            s_ = load_slab([(w_in[:, GA0 + sl * 512:GA0 + (sl + 1) * 512], 0, 512)])
            issue_cvt(3)
            for fb in range(4):
                r_ = rowctr % 2
                rowctr += 1
                for tc in range(4):
                    bk = next_bank()
                    mm_fm(s_, fb * 128, tc, bk)
                    sc.op("act", lambda e, bk=bk, r_=r_, tc=tc: e.activation(
                        out=sgrow[r_][:, tc * 512:(tc + 1) * 512], in_=pbank[bk][:], func=AF.Sigmoid),
                        reads=[B_pb[bk]], writes=[B_sgrow[r_]])
                n0 = sl * 512 + fb * 128
                sc.dma("sp", lambda e, r_=r_, n0=n0: e.dma_start(out=sg_dram[n0:n0 + 128, :], in_=sgrow[r_]),
                       f"d_sgrow{r_}", reads=[B_sgrow[r_]], writes=[B_sgd])

    sc.barrier()
    top[0] = phase_base
    NH = 16 if stage not in ("dbg_ya", "dbg_yb") else (3 if stage == "dbg_ya" else 0)
    qT = [alloc(1024, BF16) for _ in range(2)]
    ktT = [alloc(1024, BF16) for _ in range(2)]
    itok = [alloc(1024, BF16, [128, NT, 128]) for _ in range(2)]
    sgtok = [alloc(1024, BF16, [128, NT, 128]) for _ in range(2)]
    B_qT = [[Buf(f"qT{s}_{c}") for c in range(4)] for s in range(2)]
    B_ktT = [[Buf(f"ktT{s}_{c}") for c in range(4)] for s in range(2)]
    B_itok = [[Buf(f"itok{s}_{c}") for c in range(NT)] for s in range(2)]
    B_sgtok = [[Buf(f"sgtok{s}_{c}") for c in range(NT)] for s in range(2)]
    Bc = alloc(2048)
    B_Bc = Buf("Bc")
    t_sig, t_kk, t_lf, t_b, t_eq, t_ek = [alloc(512) for _ in range(6)]
    fX = [t_sig, t_lf, t_b, alloc(512)]
    fK = [t_kk, alloc(512), alloc(512), alloc(512)]
    fE = [t_eq, t_ek, alloc(512), alloc(512)]
    B_fX = [Buf(f"fX{c}") for c in range(4)]
    B_fK = [Buf(f"fK{c}") for c in range(4)]
    B_fE = [Buf(f"fE{c}") for c in range(4)]
    B_t = {n: Buf(n) for n in ("sig", "kk", "lf", "b", "eq", "ek")}
    Dm = [alloc(16) for _ in range(2)]
    B_Dm = [Buf("Dm0"), Buf("Dm1")]
    dref = alloc(16)
    Sf = alloc(128)
    Stmp = alloc(128)
    Sbf = alloc(64, BF16)
    B_S = Buf("S")
    B_Sbf = Buf("Sbf")
    sctmp = alloc(128)
    scT = alloc(64, BF16)
    B_scT = Buf("scT")
    ktok = alloc(64, BF16)
    B_ktok = Buf("ktok")
    hss = alloc(1)
    hrs = alloc(1)
    B_hs = Buf("hs")
    hjunk = alloc(64, BF16)
    ytok = alloc(64, BF16)
    B_ytok = Buf("ytok")
    yaT = [alloc(1024, BF16) for _ in range(2)]
    B_yaT = [Buf("yaT0"), Buf("yaT1")]
    B_yad = Buf("ya_dram")
    pS_sc = pbank[4][:, 0:128]
    pS_tr = pbank[5][:].bitcast(BF16)
    pS_o = pbank[6][:, 0:128]
    pS_st = pbank[7][:, 0:128]
    B_psc, B_ptr1, B_ptr2, B_po, B_pst = Buf("psc"), Buf("ptr1"), Buf("ptr2"), Buf("po"), Buf("pst")

    def hg_inproj_units(h):
        sl = h % 2
        units = []
        holder = {}

        def u_load():
            holder["s"] = load_slab([(w_in[:, h * 128:(h + 1) * 128], 0, 128),
                                     (w_in[:, 2048 + h * 128:2048 + (h + 1) * 128], 128, 128),
                                     (w_in[:, 4096 + h * 128:4096 + (h + 1) * 128], 256, 128),
                                     (w_in[:, 6144 + h * 128:6144 + (h + 1) * 128], 384, 128)])
            issue_cvt(6)
        units.append(u_load)

        def u_q(tc):
            def f():
                s_ = holder["s"]
                bk = next_bank()
                mm_fm(s_, 0, tc, bk)
                sc.op("act", lambda e: e.activation(out=qT[sl][:, tc * 512:(tc + 1) * 512], in_=pbank[bk][:],
                                                    func=AF.Copy),
                      reads=[B_pb[bk]], writes=[B_qT[sl][tc]])
            return f

        def u_f(tc):
            def f():
                s_ = holder["s"]
                bk = next_bank()
                mm_fm(s_, 128, tc, bk)
                sc.op("act", lambda e: e.activation(out=fX[tc], in_=pbank[bk][:], func=AF.Exp, scale=-1.0),
                      reads=[B_pb[bk]], writes=[B_fX[tc]])
            return f

        def f_stage(tc, j):
            cs = slice(tc * 512, (tc + 1) * 512)
            X, Kb, Eb = fX[tc], fK[tc], fE[tc]
            BX, BK, BE = B_fX[tc], B_fK[tc], B_fE[tc]

            def s1():
                sc.op("dve", lambda e: e.tensor_scalar(out=X, in0=X, scalar1=1.0, scalar2=None, op0=ALU.add),
                      reads=[BX], writes=[BX])
                sc.op("dve", lambda e: e.reciprocal(out=X, in_=X), reads=[BX], writes=[BX])
                sc.op("dve", lambda e: e.tensor_scalar(out=Kb, in0=X, scalar1=nomlb[:, h:h + 1],
                                                       scalar2=omlb[:, h:h + 1], op0=ALU.mult, op1=ALU.add),
                      reads=[BX, B_const], writes=[BK])

            def s2():
                sc.op("act", lambda e: e.activation(out=X, in_=X, func=AF.Ln, scale=omlb[:, h:h + 1],
                                                    bias=lbT[:, h:h + 1]),
                      reads=[BX, B_const], writes=[BX])

            def s3():
                init = 0.0 if tc == 0 else Bc[:, tc * 512 - 1:tc * 512]
                sc.op("dve", lambda e: e.tensor_tensor_scan(out=Bc[:, cs], data0=X, data1=zeros512,
                                                            initial=init, op0=ALU.add, op1=ALU.add),
                      reads=[BX, B_const, B_Bc], writes=[B_Bc])
                bview = Bc[:, cs].rearrange("p (a b) -> p a b", b=128)
                sc.op("dve", lambda e: e.tensor_tensor(
                    out=X.rearrange("p (a b) -> p a b", b=128), in0=bview,
                    in1=bview[:, :, 63:64].to_broadcast([128, 4, 128]), op=ALU.subtract),
                    reads=[B_Bc, BX], writes=[BX])
                if tc == 3:
                    refs = Bc.rearrange("p (a b) -> p a b", b=128)[:, :, 63]
                    sc.op("dve", lambda e: e.tensor_tensor(out=dref[:, 0:15], in0=refs[:, 1:16], in1=refs[:, 0:15],
                                                           op=ALU.subtract),
                          reads=[B_Bc], writes=[B_t["b"]])

            def s4():
                sc.op("act", lambda e: e.activation(out=Eb, in_=X, func=AF.Exp), reads=[BX], writes=[BE])
                sc.op("act", lambda e: e.activation(out=X, in_=X, func=AF.Exp, scale=-1.0), reads=[BX], writes=[BX])
                if tc == 3:
                    sc.op("act", lambda e: e.activation(out=Dm[sl][:, 0:15], in_=dref[:, 0:15], func=AF.Exp),
                          reads=[B_t["b"]], writes=[B_Dm[sl]])

            def s5():
                sc.op("dve", lambda e: e.tensor_tensor(out=qT[sl][:, cs], in0=qT[sl][:, cs], in1=Eb, op=ALU.mult),
                      reads=[BE, B_qT[sl][tc]], writes=[B_qT[sl][tc]])
                sc.op("dve", lambda e: e.tensor_tensor(out=ktT[sl][:, cs], in0=Kb, in1=X, op=ALU.mult),
                      reads=[BX, BK], writes=[B_ktT[sl][tc]])
            return (s1, s2, s3, s4, s5)[j]

        def u_ig(i):
            def f():
                s_ = holder["s"]
                bk = next_bank()
                mm_tm(s_, 256, 256, i, bk)
                sc.op("act", lambda e: e.activation(out=itok[sl][:, i, :], in_=pbank[bk][:, 0:128], func=AF.Copy),
                      reads=[B_pb[bk]], writes=[B_itok[sl][i]])
                sc.op("act", lambda e: e.activation(out=sgtok[sl][:, i, :], in_=pbank[bk][:, 128:256], func=AF.Exp, scale=-1.0),
                      reads=[B_pb[bk]], writes=[B_sgtok[sl][i]])
                if i == NT - 1:
                    sc.op("dve", lambda e: e.tensor_scalar(out=sgtok[sl], in0=sgtok[sl], scalar1=1.0, scalar2=None, op0=ALU.add),
                          reads=B_sgtok[sl], writes=B_sgtok[sl])
                    def _rcp(e):
                        with nc.allow_low_precision("bf16 storage of the sigmoid gate"):
                            return e.reciprocal(out=sgtok[sl], in_=sgtok[sl])
                    sc.op("dve", _rcp, reads=B_sgtok[sl], writes=B_sgtok[sl])
            return f
        for tc in range(4):
            units.append(u_q(tc))
        for tc in range(4):
            units.append(u_f(tc))
        for i in range(NT):
            units.append(u_ig(i))
        def ride(u0, st):
            def g():
                u0()
                st()
            return g
        for tc in range(4):
            for j in range(5):
                idx = 6 + tc + j
                units[idx] = ride(units[idx], f_stage(tc, j))
        return units

    scT2 = [scT, alloc(64, BF16)]
    ktok2 = [ktok, alloc(64, BF16)]
    ytok2 = [ytok, alloc(64, BF16)]
    sctmp2 = [sctmp, alloc(128)]
    hss2 = [hss, alloc(1)]
    hrs2 = [hrs, alloc(1)]
    B_scT2 = [Buf("scT0"), Buf("scT1")]
    B_ktok2 = [Buf("ktok0"), Buf("ktok1")]
    B_ytok2 = [Buf("ytok0"), Buf("ytok1")]
    B_hs2 = [Buf("hs0"), Buf("hs1")]
    pSsc2 = [pbank[4][:, 0:128], pbank[4][:, 128:256]]
    pStr_k = [pS_tr[:, 0:128], pS_tr[:, 128:256]]
    pStr_y = [pS_tr[:, 256:384], pS_tr[:, 384:512]]
    pSo2 = [pbank[6][:, 0:128], pbank[6][:, 128:256]]
    pSst2 = [pbank[7][:, 0:128], pbank[7][:, 128:256]]
    pStr_y = [pbank[7][:].bitcast(BF16)[:, 0:128], pbank[7][:].bitcast(BF16)[:, 128:256]]
    pSst2 = [pbank[6][:, 0:128], pbank[6][:, 128:256]]
    pSo2 = [pbank[5][:, 0:128], pbank[5][:, 128:256]]
    pStr_k = [pbank[4][:].bitcast(BF16)[:, 512:640], pbank[4][:].bitcast(BF16)[:, 640:768]]
    B_psc2 = [B_pb[4], B_pb[4]]
    B_ptrk = [B_pb[4], B_pb[4]]
    B_po2 = [B_pb[5], B_pb[5]]
    B_pst2 = [B_pb[6], B_pb[6]]
    B_ptry = [B_pb[7], B_pb[7]]

    def hg_rec_steps(h):
        sl = h % 2

        def stA(i):
            d = i % 2
            ts = slice(i * 128, (i + 1) * 128)
            tc = i // 4
            sc.op("pe", lambda e: e.matmul(pSsc2[d], lhsT=ktT[sl][:, ts], rhs=qT[sl][:, ts], start=True, stop=True),
                  reads=[B_ktT[sl][tc], B_qT[sl][tc]], writes=[B_psc2[d]])
            if i < NT - 1:
                sc.op("pe", lambda e: e.transpose(out=pStr_k[d], in_=ktT[sl][:, ts], identity=ident_b),
                      reads=[B_ktT[sl][tc], B_const], writes=[B_ptrk[d]])
            sc.op("dve", lambda e: e.tensor_scalar(out=sctmp2[d], in0=pSsc2[d], scalar1=1e30, scalar2=-1e30,
                                                   op0=ALU.min, op1=ALU.max),
                  reads=[B_psc2[d]], writes=[B_scT2[d], B_psc2[d]])
            sc.op("dve", lambda e: e.tensor_tensor(out=scT2[d], in0=sctmp2[d], in1=mask_ut, op=ALU.mult),
                  reads=[B_scT2[d], B_const], writes=[B_scT2[d]])
            if i < NT - 1:
                sc.op("act", lambda e: e.activation(out=ktok2[d], in_=pStr_k[d], func=AF.Copy),
                      reads=[B_ptrk[d]], writes=[B_ktok2[d], B_ptrk[d]])

        def stB(i):
            d = i % 2
            ts = slice(i * 128, (i + 1) * 128)
            tc = i // 4
            sc.op("pe", lambda e: e.matmul(pSo2[d], lhsT=scT2[d], rhs=itok[sl][:, i, :], start=True, stop=(i == 0)),
                  reads=[B_scT2[d], B_itok[sl][i]], writes=[B_po2[d]])
            if i > 0:
                sc.op("pe", lambda e: e.matmul(pSo2[d], lhsT=qT[sl][:, ts], rhs=Sbf, start=False, stop=True),
                      reads=[B_qT[sl][tc], B_Sbf], writes=[B_po2[d]])
            if i < NT - 1:
                sc.op("pe", lambda e: e.matmul(pSst2[d], lhsT=ktok2[d], rhs=itok[sl][:, i, :], start=True, stop=True),
                      reads=[B_ktok2[d], B_itok[sl][i]], writes=[B_pst2[d]])
                if i == 0:
                    sc.op("dve", lambda e: e.tensor_scalar(out=Sf, in0=pSst2[d], scalar1=Dm[sl][:, 0:1], scalar2=None,
                                                           op0=ALU.mult),
                          reads=[B_pst2[d], B_Dm[sl]], writes=[B_S, B_pst2[d]])
                else:
                    sc.op("dve", lambda e: e.tensor_scalar(out=Stmp, in0=Sf, scalar1=Dm[sl][:, i:i + 1], scalar2=None,
                                                           op0=ALU.mult),
                          reads=[B_S, B_Dm[sl]], writes=[B_S])
                    sc.op("dve", lambda e: e.scalar_tensor_tensor(out=Sf, in0=pSst2[d], scalar=Dm[sl][:, i:i + 1],
                                                                  in1=Stmp, op0=ALU.mult, op1=ALU.add),
                          reads=[B_pst2[d], B_S, B_Dm[sl]], writes=[B_S, B_pst2[d]])
                sc.op("act", lambda e: e.activation(out=Sbf, in_=Sf, func=AF.Copy), reads=[B_S], writes=[B_Sbf])
            sc.op("act", lambda e: e.activation(out=hjunk, in_=pSo2[d], func=AF.Square, accum_out=hss2[d]),
                  reads=[B_po2[d]], writes=[B_hs2[d], B_po2[d]])
            sc.op("act", lambda e: e.activation(out=hrs2[d], in_=hss2[d], func=AF.Ln, scale=1.0 / 128, bias=epsc[:, 0:1]),
                  reads=[B_hs2[d], B_const], writes=[B_hs2[d]])
            sc.op("act", lambda e: e.activation(out=hrs2[d], in_=hrs2[d], func=AF.Exp, scale=-0.5),
                  reads=[B_hs2[d]], writes=[B_hs2[d]])
            sc.op("dve", lambda e: e.scalar_tensor_tensor(out=ytok2[d], in0=pSo2[d], scalar=hrs2[d][:, 0:1],
                                                          in1=sgtok[sl][:, i, :], op0=ALU.mult, op1=ALU.mult),
                  reads=[B_po2[d], B_hs2[d], B_sgtok[sl][i]], writes=[B_ytok2[d], B_po2[d]])

        def stC(i):
            d = i % 2
            ts = slice(i * 128, (i + 1) * 128)
            sc.op("pe", lambda e: e.transpose(out=pStr_y[d], in_=ytok2[d], identity=ident_b),
                  reads=[B_ytok2[d], B_const], writes=[B_ptry[d]])
            sc.op("act", lambda e: e.activation(out=yaT[sl][:, ts], in_=pStr_y[d], func=AF.Copy,
                                                scale=gnT[:, h:h + 1]),
                  reads=[B_ptry[d], B_const], writes=[B_yaT[sl], B_ptry[d]])
            if i == NT - 1:
                sc.dma("sp", lambda e: e.dma_start(out=ya_dram[h * 128:(h + 1) * 128, :], in_=yaT[sl]),
                       f"d_yaT{sl}", reads=[B_yaT[sl]], writes=[B_yad])

        def slot(u):
            def f():
                if u < NT:
                    stA(u)
                if 0 <= u - 1 < NT:
                    stB(u - 1)
                if 0 <= u - 2 < NT:
                    stC(u - 2)
            return f
        return [slot(u) for u in range(NT + 2)]

    prev_steps = []
    for h in range(NH + 1):
        units = hg_inproj_units(h) if h < NH else []
        n = max(len(units), len(prev_steps))
        for u in range(n):
            if u < len(units):
                units[u]()
            if u < len(prev_steps):
                prev_steps[u]()
        prev_steps = hg_rec_steps(h) if h < NH else []

    sc.barrier()
    top[0] = phase_base
    ATT_G = ((128, 1), (512, 4), (2048, 16))
    DMAX = (1, 4, 15)
    JW = (256, 640, 2048)
    WW = tuple(j + 127 for j in JW)
    WOFF = (0, WW[0], WW[0] + WW[1])
    NSLOT = 4 if stage != "dbg_yb" else 1
    rb33 = alloc(12)
    rbx = alloc(12 * 128, F32, [128, 12, 128])
    ohs = alloc(WW[0] + WW[1] + WW[2])
    zst = alloc(1088, BF16)
    B_att = Buf("attc")
    B_zst = Buf("zst")
    B_zd = Buf("zrow_dram")
    Btoe = [[alloc(JW[g] // 2, BF16) for s in range(4)] for g in range(3)]
    B_toe = Buf("toe")
    sc.dma("sp", lambda e: e.dma_start(out=rb33[0:32, :], in_=rel_bias), "d_const", writes=[B_att])
    sc.op("dve", lambda e: e.memset(rb33[32:33, :], NEG), writes=[B_att])
    sc.dma("sp", lambda e: e.dma_start(out=ohs[0:33, :], in_=att_oh), "d_const", writes=[B_att])
    sc.op("dve", lambda e: e.tensor_copy(out=rbx[0:33, :, :], in_=rb33[0:33, :].unsqueeze(2).to_broadcast([33, 12, 128])),
          reads=[B_att], writes=[B_att])
    for g in range(3):
        for s in range(NSLOT):
            hd = g * 4 + s
            W = WW[g]
            for c0 in range(0, W, 512):
                n = min(512, W - c0)
                sc.op("pe", lambda e, c0=c0, n=n, hd=hd, g=g: e.matmul(
                    pbank[7][:, 0:n], lhsT=rbx[0:33, hd, :], rhs=ohs[0:33, WOFF[g] + c0:WOFF[g] + c0 + n],
                    start=True, stop=True), reads=[B_att], writes=[B_pb[7]])
                sc.op("act", lambda e, c0=c0, n=n: e.activation(out=zst[:, c0:c0 + n], in_=pbank[7][:, 0:n], func=AF.Copy),
                      reads=[B_pb[7]], writes=[B_zst])
            zoff = hd * 128 * 2176
            sc.dma("sp", lambda e, W=W, zoff=zoff: e.dma_start(
                out=zrow_dram[zoff:zoff + 128 * W].rearrange("(c w) -> c w", w=W), in_=zst[:, 0:W]),
                "d_zst", reads=[B_zst], writes=[B_zd])
            src = bass.AP(tensor=zrow_dram.tensor, offset=zrow_dram.offset + zoff + 127,
                          ap=[[W - 1, 128], [1, JW[g]]])
            sc.dma("sp", lambda e, src=src, g=g, s=s: e.dma_start(out=Btoe[g][s], in_=src),
                   "d_toe", reads=[B_zd], writes=[B_toe])

    qTs = [alloc(1024, BF16) for g in range(3)]
    kTs = [alloc(1024, BF16) for g in range(3)]
    vtok = alloc(16 * 3 * 130 // 2, BF16).rearrange("p (t g c) -> p t g c", g=3, c=130)
    B_qTs = [[Buf(f"qTs{g}_{c}") for c in range(4)] for g in range(3)]
    B_kTs = [[Buf(f"kTs{g}_{c}") for c in range(4)] for g in range(3)]
    B_vtok = [Buf(f"vtok{i}") for i in range(NT)]
    PT = [alloc(256, BF16) for _ in range(2)]
    B_PT = [Buf("PT0"), Buf("PT1")]
    rden = alloc(1)
    B_rden = Buf("rden")
    obt = alloc(64, BF16)
    B_obt = Buf("obt")
    ybT = alloc(1024, BF16)
    B_ybT = Buf("ybT")
    B_ybd = Buf("yb_dram")
    sc.op("dve", lambda e: e.memset(vtok[:, :, :, 128:130], 1.0), writes=B_vtok)
    po_ap = [pbank[4 + j][:, 0:129] for j in range(4)]
    B_poa = [B_pb[4 + j] for j in range(4)]
    pt_ctr = 0
    AQ0 = 8192
    for s in range(NSLOT):
        pieces_q = [(w_in[:, AQ0 + (g * 4 + s) * 128:AQ0 + (g * 4 + s + 1) * 128], g * 128, 128) for g in range(3)]
        pieces_k = [(w_in[:, AQ0 + 1536 + (g * 4 + s) * 128:AQ0 + 1536 + (g * 4 + s + 1) * 128], g * 128, 128) for g in range(3)]
        pieces_v = [(w_in[:, AQ0 + 3072 + (g * 4 + s) * 128:AQ0 + 3072 + (g * 4 + s + 1) * 128], g * 128, 128) for g in range(3)]
        s_q = load_slab(pieces_q)
        s_k = load_slab(pieces_k)
        s_v = load_slab(pieces_v)
        issue_cvt(18)

        def inproj_units(tc, s_q=s_q, s_k=s_k, s_v=s_v):
            us = []
            for g in range(3):
                def uq(g=g):
                    bk = next_bank()
                    mm_fm(s_q, g * 128, tc, bk)
                    sc.op("act", lambda e: e.activation(
                        out=qTs[g][:, tc * 512:(tc + 1) * 512], in_=pbank[bk][:], func=AF.Copy, scale=128.0 ** -0.5),
                        reads=[B_pb[bk]], writes=[B_qTs[g][tc]])
                us.append(uq)

                def uk(g=g):
                    bk = next_bank()
                    mm_fm(s_k, g * 128, tc, bk)
                    sc.op("dve", lambda e: e.tensor_copy(out=kTs[g][:, tc * 512:(tc + 1) * 512], in_=pbank[bk][:]),
                          reads=[B_pb[bk]], writes=[B_kTs[g][tc]])
                us.append(uk)
            for i in range(4 * tc, 4 * tc + 4):
                def uv(i=i):
                    bk = next_bank()
                    mm_tm(s_v, 0, 384, i, bk)
                    sc.op("act", lambda e: e.activation(
                        out=vtok[:, i, :, 0:128], in_=pbank[bk][:, 0:384].rearrange("p (g c) -> p g c", c=128), func=AF.Copy),
                        reads=[B_pb[bk]], writes=[B_vtok[i]])
                us.append(uv)
            return us

        for u_ in inproj_units(0):
            u_()
        for Q in range(4):
            pend = inproj_units(Q + 1) if Q < 3 else []
            contribs = []
            for g in range(3):
                for m in range(max(0, 4 * Q - DMAX[g]), 4 * Q + 4):
                    T_lo = max(m, 4 * Q)
                    T_hi = min(4 * Q + 3, m + DMAX[g])
                    if T_hi >= T_lo:
                        contribs.append((g, m, T_lo, T_hi))
            firstT, lastT = {}, {}
            for ci, (g, m, T_lo, T_hi) in enumerate(contribs):
                for T in range(T_lo, T_hi + 1):
                    firstT.setdefault(T, ci)
                    lastT[T] = ci
            every = max(1, len(contribs) // (len(pend) + 1)) if pend else 0
            for ci, (g, m, T_lo, T_hi) in enumerate(contribs):
                if pend and ci % every == every - 1:
                    pend.pop(0)()
                ncols = (T_hi - T_lo + 1) * 128
                bk = next_bank()
                qbufs = [B_qTs[g][T // 4] for T in range(T_lo, T_hi + 1)]
                sc.op("pe", lambda e, g=g, m=m, T_lo=T_lo, T_hi=T_hi, ncols=ncols, bk=bk: e.matmul(
                    pbank[bk][:, 0:ncols], lhsT=kTs[g][:, m * 128:(m + 1) * 128],
                    rhs=qTs[g][:, T_lo * 128:(T_hi + 1) * 128], start=True, stop=False),
                    reads=[B_kTs[g][m // 4]] + qbufs, writes=[B_pb[bk]])
                sc.op("pe", lambda e, g=g, m=m, T_lo=T_lo, T_hi=T_hi, ncols=ncols, bk=bk, s=s: e.matmul(
                    pbank[bk][:, 0:ncols], lhsT=ident_b,
                    rhs=Btoe[g][s][:, (T_lo - m) * 128:(T_hi - m + 1) * 128], start=False, stop=True),
                    reads=[B_toe, B_const], writes=[B_pb[bk]])
                p_ = pt_ctr % 2
                pt_ctr += 1
                sc.op("act", lambda e, p_=p_, ncols=ncols, bk=bk: e.activation(
                    out=PT[p_][:, 0:ncols], in_=pbank[bk][:, 0:ncols], func=AF.Exp),
                    reads=[B_pb[bk]], writes=[B_PT[p_]])
                for T in range(T_lo, T_hi + 1):
                    j = T - 4 * Q
                    st_, sp_ = (firstT[T] == ci), (lastT[T] == ci)
                    sc.op("pe", lambda e, p_=p_, T=T, T_lo=T_lo, j=j, m=m, g=g, st_=st_, sp_=sp_: e.matmul(
                        po_ap[j], lhsT=PT[p_][:, (T - T_lo) * 128:(T - T_lo + 1) * 128], rhs=vtok[:, m, g, 0:129],
                        start=st_, stop=sp_),
                        reads=[B_PT[p_], B_vtok[m]], writes=[B_poa[j]])
            while pend:
                pend.pop(0)()
            for j in range(4):
                T = 4 * Q + j
                sc.op("dve", lambda e, j=j: e.reciprocal(out=rden, in_=po_ap[j][:, 128:129]),
                      reads=[B_poa[j]], writes=[B_rden])
                sc.op("dve", lambda e, j=j: e.tensor_scalar(out=obt, in0=po_ap[j][:, 0:128], scalar1=rden[:, 0:1],
                                                            scalar2=None, op0=ALU.mult),
                      reads=[B_poa[j], B_rden], writes=[B_obt])
                bk = next_bank()
                ptr_att = pbank[bk][:].bitcast(BF16)
                sc.op("pe", lambda e, ptr_att=ptr_att: e.transpose(out=ptr_att[:, 0:128], in_=obt, identity=ident_b),
                      reads=[B_obt, B_const], writes=[B_pb[bk]])
                sc.op("act", lambda e, T=T, ptr_att=ptr_att: e.activation(out=ybT[:, T * 128:(T + 1) * 128], in_=ptr_att[:, 0:128],
                                                         func=AF.Copy),
                      reads=[B_pb[bk]], writes=[B_ybT])
        sc.dma("sp", lambda e, s=s: e.dma_start(out=yb_dram[s * 128:(s + 1) * 128, :], in_=ybT),
               "d_ybT", reads=[B_ybT], writes=[B_ybd])
    fin = [B_sgd, B_yad, B_ybd]
    if stage in ('full', 'dbg_full'):
        sc.barrier()
        top[0] = phase_base
        yaT_all = hT
        sc.dma("sp", lambda e: e.dma_start(out=yaT_all[:, 0:8, :], in_=ya_dram[0:1024, :].rearrange("(c p) t -> p c t", p=128)),
               "d_big", reads=[B_yad], writes=B_hT)
        sc.dma("sp", lambda e: e.dma_start(out=yaT_all[:, 8:16, :], in_=ya_dram[1024:2048, :].rearrange("(c p) t -> p c t", p=128)),
               "d_big", reads=[B_yad], writes=B_hT)
        mergedT = alloc(16384, BF16, [128, KC, S])
        ybT_all = alloc(4096, BF16, [128, 4, S])
        B_ybTa = Buf("ybT_all")
        sc.dma("sp", lambda e: e.dma_start(out=ybT_all, in_=yb_dram.rearrange("(c p) t -> p c t", p=128)),
               "d_big", reads=[B_ybd], writes=[B_ybTa])
        B_mT = [Buf(f"mT{c}") for c in range(4)]
        sga = alloc(1024, BF16)
        sgb = alloc(1024, BF16)
        B_sga, B_sgb = Buf("sga"), Buf("sgb")
        tmpA = slab[2][:, 0:2, :].rearrange("p a b -> p (a b)").bitcast(F32)
        tmpB = slab[2][:, 2:4, :].rearrange("p a b -> p (a b)").bitcast(F32)
        B_tA, B_tB = Buf("tmpA"), Buf("tmpB")
        nslab[0] = 2
        slab_ctr[0] = 0
        for ns in range(4):
            s_a = load_slab([(w_branch_a[:, ns * 512:(ns + 1) * 512], 0, 512)])
            s_b = load_slab([(w_branch_b[:, ns * 512:(ns + 1) * 512], 0, 512)], kc_n=4)
            issue_cvt(200)
            for fb in range(4):
                nb = ns * 4 + fb
                sc.dma("sp", lambda e, nb=nb: e.dma_start(out=sga, in_=sg_dram[nb * 128:(nb + 1) * 128, :]),
                       "d_sga", reads=[B_sgd], writes=[B_sga])
                sc.dma("sp", lambda e, nb=nb: e.dma_start(out=sgb, in_=sg_dram[2048 + nb * 128:2048 + (nb + 1) * 128, :]),
                       "d_sgb", reads=[B_sgd], writes=[B_sgb])
                for tc in range(4):
                    bka = next_bank()
                    for kc in range(KC):
                        sc.op("pe", lambda e, kc=kc, bka=bka, s_a=s_a, fb=fb, tc=tc: e.matmul(
                            pbank[bka][:], lhsT=slab[s_a][:, kc, fb * 128:(fb + 1) * 128],
                            rhs=yaT_all[:, kc, tc * 512:(tc + 1) * 512], start=(kc == 0), stop=(kc == KC - 1)),
                            reads=[B_slab[s_a]] + B_hT[tc * 4:(tc + 1) * 4], writes=[B_pb[bka]])
                    bkb = next_bank()
                    for kc in range(4):
                        sc.op("pe", lambda e, kc=kc, bkb=bkb, s_b=s_b, fb=fb, tc=tc: e.matmul(
                            pbank[bkb][:], lhsT=slab[s_b][:, kc, fb * 128:(fb + 1) * 128],
                            rhs=ybT_all[:, kc, tc * 512:(tc + 1) * 512], start=(kc == 0), stop=(kc == 3)),
                            reads=[B_slab[s_b], B_ybTa], writes=[B_pb[bkb]])
                    cs = slice(tc * 512, (tc + 1) * 512)
                    sc.op("dve", lambda e, bka=bka, cs=cs: e.tensor_tensor(out=tmpA, in0=pbank[bka][:], in1=sga[:, cs], op=ALU.mult),
                          reads=[B_pb[bka], B_sga], writes=[B_tA])
                    sc.op("dve", lambda e, bkb=bkb, cs=cs: e.tensor_tensor(out=tmpB, in0=pbank[bkb][:], in1=sgb[:, cs], op=ALU.mult),
                          reads=[B_pb[bkb], B_sgb], writes=[B_tB])
                    sc.op("pool", lambda e, nb=nb, cs=cs: e.tensor_tensor(out=mergedT[:, nb, cs], in0=tmpA, in1=tmpB, op=ALU.add),
                          reads=[B_tA, B_tB], writes=[B_mT[tc]])

        sc.barrier()
        wo = hT
        B_wo = Buf("wo")
        for c in range(4):
            sc.dma("pool", lambda e, c=c: e.dma_start(
                out=wo[:, c * 4:(c + 1) * 4, :], in_=w_out[c * 512:(c + 1) * 512, :].rearrange("(kc p) n -> p kc n", p=128)),
                "d_big", writes=[B_wo])
        top[0] = phase_base + 16384
        g2b = alloc(2048)
        gfb = alloc(2048)
        B_gb = Buf("gb")
        bcast = lambda ap_: bass.AP(tensor=ap_.tensor, offset=ap_.offset, ap=[[0, 128], [1, ap_.shape[0]]])
        sc.dma("sp", lambda e: e.dma_start(out=g2b, in_=bcast(norm2_gain)), "d_const", writes=[B_gb])
        sc.dma("sp", lambda e: e.dma_start(out=gfb, in_=bcast(final_norm_gain)), "d_const", writes=[B_gb])
        top2 = [slab_base]

        def alloc2(words, dt=F32, shape=None):
            save = top[0]
            top[0] = top2[0]
            a = alloc(words, dt, shape)
            top2[0] = top[0]
            top[0] = save
            assert top2[0] <= slab_base + 12288
            return a
        xt2 = alloc2(2048)
        x2t = alloc2(2048)
        h2f = alloc2(2048)
        h2T = alloc2(2048, F32, [128, KC, 128])
        h2b = alloc2(1024, BF16)
        wr = alloc2(16 * 72, F32, [128, KC, 72])
        lg_all = alloc2(16 * 72, F32, [128, NT, 72])
        brow = alloc2(72)
        onesrow = alloc2(128)
        ss2 = alloc2(1)
        rs2 = alloc2(1)
        B_xt2, B_x2t, B_h2f, B_h2T, B_h2b, B_wr, B_lg, B_s2 = (Buf(n) for n in ("xt2", "x2t", "h2f", "h2T", "h2b", "wr", "lg", "s2"))
        B_x2d, B_h2d = Buf("x2_dram"), Buf("h2_dram")
        sc.dma("sp", lambda e: e.dma_start(out=wr[:, :, 0:8], in_=w_router_group.rearrange("(kc p) n -> p kc n", p=128),
                                           allow_slow_non_contiguous=True), "d_const", writes=[B_wr])
        sc.dma("sp", lambda e: e.dma_start(out=wr[:, :, 8:72], in_=w_router_expert.rearrange("(kc p) n -> p kc n", p=128),
                                           allow_slow_non_contiguous=True), "d_const", writes=[B_wr])
        sc.dma("sp", lambda e: e.dma_start(out=brow[0:1, 0:8], in_=b_router_group), "d_const", writes=[B_wr])
        sc.dma("sp", lambda e: e.dma_start(out=brow[0:1, 8:72], in_=b_router_expert), "d_const", writes=[B_wr])
        sc.op("dve", lambda e: e.memset(onesrow[0:1, :], 1.0), writes=[B_wr])
        sc.op("dve", lambda e: e.memset(h2f, 0.0), writes=[B_h2f])
        sc.op("dve", lambda e: e.memset(h2b, 0.0), writes=[B_h2b])
        sc.dma("sp", lambda e: e.dma_start(out=h2_dram[2048:2049, :], in_=h2b[0:1, :]), "d_h2b", reads=[B_h2b], writes=[B_h2d])
        B_yed = Buf("ye_dram")
        sc.dma("sp", lambda e: e.dma_start(out=ye_dram[8192:8320, :], in_=h2b), "d_h2b", reads=[B_h2b], writes=[B_yed])
        for i in range(NT):
            ts = slice(i * 128, (i + 1) * 128)
            sc.dma("sp", lambda e, ts=ts: e.dma_start(out=xt2, in_=x[ts, :]), "d_xt2", writes=[B_xt2])
            for dsl in range(4):
                for kc in range(KC):
                    sc.op("pe", lambda e, kc=kc, dsl=dsl, ts=ts: e.matmul(
                        pbank[dsl][:], lhsT=mergedT[:, kc, ts], rhs=wo[:, kc, dsl * 512:(dsl + 1) * 512],
                        start=(kc == 0), stop=(kc == KC - 1)),
                        reads=[B_mT[i // 4], B_wo], writes=[B_pb[dsl]])
                sc.op("dve", lambda e, dsl=dsl: e.tensor_tensor(out=x2t[:, dsl * 512:(dsl + 1) * 512], in0=pbank[dsl][:],
                                                               in1=xt2[:, dsl * 512:(dsl + 1) * 512], op=ALU.add),
                      reads=[B_pb[dsl], B_xt2], writes=[B_x2t])
            sc.dma("sp", lambda e, ts=ts: e.dma_start(out=x2_dram[ts, :], in_=x2t), "d_x2t", reads=[B_x2t], writes=[B_x2d])
            sc.op("act", lambda e: e.activation(out=h2f, in_=x2t, func=AF.Square, accum_out=ss2), reads=[B_x2t], writes=[B_h2f, B_s2])
            sc.op("act", lambda e: e.activation(out=rs2, in_=ss2, func=AF.Sqrt, scale=1.0 / D, bias=epsc[:, 0:1]),
                  reads=[B_s2, B_const], writes=[B_s2])
            sc.op("dve", lambda e: e.reciprocal(out=rs2, in_=rs2), reads=[B_s2], writes=[B_s2])
            sc.op("dve", lambda e: e.scalar_tensor_tensor(out=h2f, in0=x2t, scalar=rs2[:, 0:1], in1=g2b, op0=ALU.mult, op1=ALU.mult),
                  reads=[B_x2t, B_s2, B_gb], writes=[B_h2f])
            sc.op("act", lambda e: e.activation(out=h2b, in_=h2f, func=AF.Copy), reads=[B_h2f], writes=[B_h2b])
            sc.dma("sp", lambda e, ts=ts: e.dma_start(out=h2_dram[ts, :], in_=h2b), "d_h2b", reads=[B_h2b], writes=[B_h2d])
            for q4 in range(4):
                bk = 4 + q4
                for j in range(4):
                    kc = q4 * 4 + j
                    sc.op("pe", lambda e, bk=bk, j=j, kc=kc: e.transpose(
                        out=pbank[bk][:, j * 128:(j + 1) * 128], in_=h2f[:, kc * 128:(kc + 1) * 128], identity=ident_f),
                        reads=[B_h2f, B_const], writes=[B_pb[bk]])
                sc.op("act" if q4 % 2 else "dve", (lambda e, bk=bk, q4=q4: e.activation(
                    out=h2T[:, q4 * 4:(q4 + 1) * 4, :], in_=pbank[bk][:].rearrange("p (a b) -> p a b", b=128), func=AF.Copy))
                    if q4 % 2 else (lambda e, bk=bk, q4=q4: e.tensor_copy(
                        out=h2T[:, q4 * 4:(q4 + 1) * 4, :], in_=pbank[bk][:].rearrange("p (a b) -> p a b", b=128))),
                    reads=[B_pb[bk]], writes=[B_h2T])
            for kc in range(KC):
                sc.op("pe", lambda e, kc=kc: e.matmul(pbank[0][:, 0:72], lhsT=h2T[:, kc, :], rhs=wr[:, kc, :],
                                                      start=(kc == 0), stop=False),
                      reads=[B_h2T, B_wr], writes=[B_pb[0]])
            sc.op("pe", lambda e: e.matmul(pbank[0][:, 0:72], lhsT=onesrow[0:1, :], rhs=brow[0:1, :], start=False, stop=True),
                  reads=[B_wr], writes=[B_pb[0]])
            sc.op("dve", lambda e, i=i: e.tensor_copy(out=lg_all[:, i, :], in_=pbank[0][:, 0:72]), reads=[B_pb[0]], writes=[B_lg])

        sc.barrier()
        top[0] = phase_base
        B_r = Buf("route")

        def R(eng, fn):
            sc.op(eng, fn, reads=[B_r, B_lg, B_const], writes=[B_r])
        lgG = lg_all[:, :, 0:8]
        lgE = lg_all[:, :, 8:72].rearrange("p t (g e) -> p t g e", e=8)
        mG, sumG, pg, m1, m2, w1 = (alloc(16) for _ in range(6))
        ohG, eG, lgin, oh1, oh2, msk = (alloc(128, F32, [128, 16, 8]) for _ in range(6))
        sel = alloc(1024).rearrange("p (t g e) -> p t g e", g=8, e=8)
        O1 = alloc(1024).rearrange("p (t g e) -> p t g e", g=8, e=8)
        O2 = alloc(1024).rearrange("p (t g e) -> p t g e", g=8, e=8)
        Osum = alloc(1024, F32, [128, 16, 64])
        Ob = alloc(512, BF16, [128, 16, 64])
        cum = alloc(1024, F32, [128, 16, 64])
        tmp64 = alloc(1024, F32, [128, 16, 64])
        gates = alloc(32, F32, [128, 16, 2])
        slot_ = alloc(32, F32, [128, 16, 2])
        eid_ = alloc(32, F32, [128, 16, 2])
        tixf = alloc(32, F32, [128, 16, 2])
        rixf = alloc(32, F32, [128, 16, 2])
        tix = alloc(32, I32, [128, 16, 2])
        rix = alloc(32, I32, [128, 16, 2])
        iota_i = alloc(64, I32)
        iota_e = alloc(64)
        tokid = alloc(16, I32)
        fill_i = alloc(65, I32)
        ones_b = alloc(64, BF16)
        mst_b = alloc(64, BF16)
        mstf = alloc(128)
        tabs = alloc(64, I32)
        bc3 = lambda a: a.unsqueeze(2).to_broadcast([128, 16, 8])
        R("dve", lambda e: e.tensor_reduce(out=mG, in_=lgG, axis=AX.X, op=ALU.max))
        R("dve", lambda e: e.tensor_tensor(out=ohG, in0=lgG, in1=bc3(mG), op=ALU.is_equal))
        R("dve", lambda e: e.tensor_tensor(out=eG, in0=lgG, in1=bc3(mG), op=ALU.subtract))
        R("act", lambda e: e.activation(out=eG, in_=eG, func=AF.Exp))
        R("dve", lambda e: e.tensor_reduce(out=sumG, in_=eG, axis=AX.X, op=ALU.add))
        R("dve", lambda e: e.reciprocal(out=pg, in_=sumG))
        R("dve", lambda e: e.tensor_tensor(out=sel, in0=lgE, in1=ohG.unsqueeze(3).to_broadcast([128, 16, 8, 8]), op=ALU.mult))
        R("dve", lambda e: e.tensor_reduce(out=lgin, in_=sel.rearrange("p t g e -> p t e g"), axis=AX.X, op=ALU.add))
        R("dve", lambda e: e.tensor_reduce(out=m1, in_=lgin, axis=AX.X, op=ALU.max))
        R("dve", lambda e: e.tensor_tensor(out=oh1, in0=lgin, in1=bc3(m1), op=ALU.is_equal))
        R("dve", lambda e: e.scalar_tensor_tensor(out=msk, in0=oh1, scalar=-1e30, in1=lgin, op0=ALU.mult, op1=ALU.add))
        R("dve", lambda e: e.tensor_reduce(out=m2, in_=msk, axis=AX.X, op=ALU.max))
        R("dve", lambda e: e.tensor_tensor(out=oh2, in0=msk, in1=bc3(m2), op=ALU.is_equal))
        R("dve", lambda e: e.tensor_tensor(out=w1, in0=m2, in1=m1, op=ALU.subtract))
        R("act", lambda e: e.activation(out=w1, in_=w1, func=AF.Exp))
        R("dve", lambda e: e.tensor_scalar(out=w1, in0=w1, scalar1=1.0, scalar2=None, op0=ALU.add))
        R("dve", lambda e: e.reciprocal(out=w1, in_=w1))
        R("dve", lambda e: e.tensor_tensor(out=gates[:, :, 0], in0=pg, in1=w1, op=ALU.mult))
        R("dve", lambda e: e.tensor_tensor(out=gates[:, :, 1], in0=pg, in1=gates[:, :, 0], op=ALU.subtract))
        for O_, oh_ in ((O1, oh1), (O2, oh2)):
            R("dve", lambda e, O_=O_: e.tensor_copy(out=O_, in_=ohG.unsqueeze(3).to_broadcast([128, 16, 8, 8])))
            R("dve", lambda e, O_=O_, oh_=oh_: e.tensor_tensor(out=O_, in0=O_, in1=oh_.unsqueeze(2).to_broadcast([128, 16, 8, 8]),
                                                             op=ALU.mult))
        O1f = O1.rearrange("p t g e -> p t (g e)")
        O2f = O2.rearrange("p t g e -> p t (g e)")
        R("dve", lambda e: e.tensor_tensor(out=Osum, in0=O1f, in1=O2f, op=ALU.add))
        R("dve", lambda e: e.tensor_copy(out=Ob, in_=Osum))
        R("dve", lambda e: e.memset(ones_b, 1.0))
        R("dve", lambda e: e.tensor_tensor(out=mstf, in0=mask_ut, in1=ident_f, op=ALU.subtract))
        R("dve", lambda e: e.tensor_copy(out=mst_b, in_=mstf))
        for i in range(NT):
            bk = 4 + (i // 8)
            cols = slice((i % 8) * 64, (i % 8 + 1) * 64)
            for j in range(i):
                sc.op("pe", lambda e, bk=bk, cols=cols, j=j: e.matmul(pbank[bk][:, cols], lhsT=ones_b, rhs=Ob[:, j, :],
                                                                     start=(j == 0), stop=False),
                      reads=[B_r], writes=[B_pb[bk]])
            sc.op("pe", lambda e, bk=bk, cols=cols, i=i: e.matmul(pbank[bk][:, cols], lhsT=mst_b, rhs=Ob[:, i, :],
                                                                 start=(i == 0), stop=True),
                  reads=[B_r], writes=[B_pb[bk]])
        sc.op("dve", lambda e: e.tensor_copy(out=cum[:, 0:8, :], in_=pbank[4][:].rearrange("p (t e) -> p t e", e=64)),
              reads=[B_pb[4], B_r], writes=[B_r])
        sc.op("dve", lambda e: e.tensor_copy(out=cum[:, 8:16, :], in_=pbank[5][:].rearrange("p (t e) -> p t e", e=64)),
              reads=[B_pb[5], B_r], writes=[B_r])
        R("pool", lambda e: e.iota(iota_i, pattern=[[1, 64]], base=0, channel_multiplier=0))
        R("pool", lambda e: e.iota(tokid, pattern=[[128, 16]], base=0, channel_multiplier=1))
        R("pool", lambda e: e.iota(fill_i, pattern=[[0, 65]], base=2048, channel_multiplier=0))
        R("dve", lambda e: e.tensor_copy(out=iota_e, in_=iota_i))
        for jj, Of in ((0, O1f), (1, O2f)):
            R("dve", lambda e, Of=Of: e.tensor_tensor(out=tmp64, in0=Of, in1=cum, op=ALU.mult))
            R("dve", lambda e, jj=jj: e.tensor_reduce(out=slot_[:, :, jj], in_=tmp64, axis=AX.X, op=ALU.add))
            R("dve", lambda e, Of=Of: e.tensor_tensor(out=tmp64, in0=Of, in1=iota_e.unsqueeze(1).to_broadcast([128, 16, 64]),
                                                     op=ALU.mult))
            R("dve", lambda e, jj=jj: e.tensor_reduce(out=eid_[:, :, jj], in_=tmp64, axis=AX.X, op=ALU.add))
        R("dve", lambda e: e.tensor_scalar(out=tixf, in0=slot_, scalar1=128.0, scalar2=None, op0=ALU.min))
        R("dve", lambda e: e.scalar_tensor_tensor(out=tixf, in0=eid_, scalar=129.0, in1=tixf, op0=ALU.mult, op1=ALU.add))
        R("dve", lambda e: e.tensor_scalar(out=rixf, in0=slot_, scalar1=128.0, scalar2=1e6, op0=ALU.is_ge, op1=ALU.mult))
        R("dve", lambda e: e.tensor_tensor(out=rixf, in0=rixf, in1=slot_, op=ALU.add))
        R("dve", lambda e: e.scalar_tensor_tensor(out=rixf, in0=eid_, scalar=128.0, in1=rixf, op0=ALU.mult, op1=ALU.add))
        R("dve", lambda e: e.tensor_scalar(out=rixf, in0=rixf, scalar1=8192.0, scalar2=None, op0=ALU.min))
        R("dve", lambda e: e.tensor_copy(out=tix, in_=tixf))
        R("dve", lambda e: e.tensor_copy(out=rix, in_=rixf))
        B_tabd = Buf("tab_dram")
        sc.dma("sp", lambda e: e.dma_start(out=tab_dram.rearrange("(p c) -> p c", c=65), in_=fill_i), "d_tab",
               reads=[B_r], writes=[B_tabd])
        tab2 = tab_dram.rearrange("(r c) -> r c", c=1)
        for i in range(NT):
            for jj in range(2):
                sc.dma("pool", lambda e, i=i, jj=jj: e.indirect_dma_start(
                    out=tab2, out_offset=bass.IndirectOffsetOnAxis(ap=tix[:, i, jj:jj + 1], axis=0),
                    in_=tokid[:, i:i + 1], in_offset=None), "d_tabs", reads=[B_r, B_tabd], writes=[B_tabd])
        B_tabs = Buf("tabs")
        tab_src = bass.AP(tensor=tab_dram.tensor, offset=tab_dram.offset, ap=[[1, 128], [129, 64]])
        sc.dma("sp", lambda e: e.dma_start(out=tabs, in_=tab_src, allow_slow_non_contiguous=True), "d_tab2",
               reads=[B_tabd], writes=[B_tabs])

        sc.barrier()
        wreg = [slab_base - 16384 + 4096 * k for k in range(6)]
        def wview(k):
            a = arena[:, wreg[k]:wreg[k] + 4096].bitcast(BF16)
            return a
        Wg = [wview(0).rearrange("p (a b) -> p a b", b=512), wview(3).rearrange("p (a b) -> p a b", b=512)]
        Wu = [wview(1).rearrange("p (a b) -> p a b", b=512), wview(4).rearrange("p (a b) -> p a b", b=512)]
        Wd = [wview(2).rearrange("p (a b) -> p a b", b=2048), wview(5).rearrange("p (a b) -> p a b", b=2048)]
        B_W = [Buf("W0"), Buf("W1")]
        xb = [alloc(1024, BF16) for _ in range(2)]
        B_xb = [Buf("xb0"), Buf("xb1")]
        xbT = alloc(1024, BF16, [128, KC, 128])
        B_xbT = Buf("xbT")
        sgu = alloc(512)
        ub = alloc(256, BF16)
        uT = alloc(256, BF16, [128, 4, 128])
        B_sgu, B_ub, B_uT = Buf("sgu"), Buf("ub"), Buf("uT")
        yeb = [alloc(1024, BF16) for _ in range(2)]
        B_yeb = [Buf("yeb0"), Buf("yeb1")]
        NE = 64 if stage == "full" else 2
        for ex in range(NE):
            p_ = ex % 2
            sc.dma("sp", lambda e, ex=ex, p_=p_: e.dma_start(
                out=Wg[p_], in_=wbf_dram[ex, 0].rearrange("(kc p n) -> p kc n", p=128, n=512)),
                f"d_W{p_}", reads=[B_wbf], writes=[B_W[p_]])
            sc.dma("sp", lambda e, ex=ex, p_=p_: e.dma_start(
                out=Wu[p_], in_=wbf_dram[ex, 1].rearrange("(kc p n) -> p kc n", p=128, n=512)),
                f"d_W{p_}", reads=[B_wbf], writes=[B_W[p_]])
            sc.dma("sp", lambda e, ex=ex, p_=p_: e.dma_start(
                out=Wd[p_], in_=wbf_dram[ex, 2].rearrange("(kc p n) -> p kc n", p=128, n=2048)),
                f"d_W{p_}", reads=[B_wbf], writes=[B_W[p_]])
            sc.dma("pool", lambda e, ex=ex, p_=p_: e.indirect_dma_start(
                out=xb[p_], out_offset=None, in_=h2_dram,
                in_offset=bass.IndirectOffsetOnAxis(ap=tabs[:, ex:ex + 1], axis=0)),
                f"d_xb{p_}", reads=[B_tabs, B_h2d], writes=[B_xb[p_]])
            for q2 in range(2):
                bk = q2
                pt = pbank[bk][:].bitcast(BF16)
                for j in range(8):
                    kc = q2 * 8 + j
                    sc.op("pe", lambda e, pt=pt, j=j, kc=kc, p_=p_: e.transpose(
                        out=pt[:, j * 128:(j + 1) * 128], in_=xb[p_][:, kc * 128:(kc + 1) * 128], identity=ident_b),
                        reads=[B_xb[p_], B_const], writes=[B_pb[bk]])
                sc.op("act" if q2 else "dve", (lambda e, pt=pt, q2=q2: e.activation(
                    out=xbT[:, q2 * 8:(q2 + 1) * 8, :], in_=pt.rearrange("p (a b) -> p a b", b=128), func=AF.Copy))
                    if q2 else (lambda e, pt=pt, q2=q2: e.tensor_copy(
                        out=xbT[:, q2 * 8:(q2 + 1) * 8, :], in_=pt.rearrange("p (a b) -> p a b", b=128))),
                    reads=[B_pb[bk]], writes=[B_xbT])
            for kc in range(KC):
                sc.op("pe", lambda e, kc=kc, p_=p_: e.matmul(pbank[2][:], lhsT=xbT[:, kc, :], rhs=Wg[p_][:, kc, :],
                                                            start=(kc == 0), stop=(kc == KC - 1)),
                      reads=[B_xbT, B_W[p_]], writes=[B_pb[2]])
            for kc in range(KC):
                sc.op("pe", lambda e, kc=kc, p_=p_: e.matmul(pbank[3][:], lhsT=xbT[:, kc, :], rhs=Wu[p_][:, kc, :],
                                                            start=(kc == 0), stop=(kc == KC - 1)),
                      reads=[B_xbT, B_W[p_]], writes=[B_pb[3]])
            sc.op("act", lambda e: e.activation(out=sgu, in_=pbank[2][:], func=AF.Silu), reads=[B_pb[2]], writes=[B_sgu])
            sc.op("dve", lambda e: e.tensor_tensor(out=ub, in0=sgu, in1=pbank[3][:], op=ALU.mult),
                  reads=[B_sgu, B_pb[3]], writes=[B_ub])
            ptu = pbank[0][:].bitcast(BF16)
            for fc in range(4):
                sc.op("pe", lambda e, fc=fc, ptu=ptu: e.transpose(out=ptu[:, fc * 128:(fc + 1) * 128],
                                                                 in_=ub[:, fc * 128:(fc + 1) * 128], identity=ident_b),
                      reads=[B_ub, B_const], writes=[B_pb[0]])
            sc.op("act", lambda e, ptu=ptu: e.activation(out=uT, in_=ptu[:, 0:512].rearrange("p (a b) -> p a b", b=128), func=AF.Copy),
                  reads=[B_pb[0]], writes=[B_uT])
            for dsl in range(4):
                bk = 4 + dsl
                for fc in range(4):
                    sc.op("pe", lambda e, fc=fc, dsl=dsl, bk=bk, p_=p_: e.matmul(
                        pbank[bk][:], lhsT=uT[:, fc, :], rhs=Wd[p_][:, fc, dsl * 512:(dsl + 1) * 512],
                        start=(fc == 0), stop=(fc == 3)),
                        reads=[B_uT, B_W[p_]], writes=[B_pb[bk]])
                if dsl % 2:
                    sc.op("act", lambda e, dsl=dsl, bk=bk, p_=p_: e.activation(out=yeb[p_][:, dsl * 512:(dsl + 1) * 512],
                                                                              in_=pbank[bk][:], func=AF.Copy),
                          reads=[B_pb[bk]], writes=[B_yeb[p_]])
                else:
                    sc.op("dve", lambda e, dsl=dsl, bk=bk, p_=p_: e.tensor_copy(out=yeb[p_][:, dsl * 512:(dsl + 1) * 512],
                                                                               in_=pbank[bk][:]),
                          reads=[B_pb[bk]], writes=[B_yeb[p_]])
            sc.dma("act", lambda e, ex=ex, p_=p_: e.dma_start(out=ye_dram[ex * 128:(ex + 1) * 128, :], in_=yeb[p_]),
                   f"d_yeb{p_}", reads=[B_yeb[p_]], writes=[B_yed])

        sc.barrier()
        top[0] = slab_base - 16384
        fx = [alloc(2048) for _ in range(2)]
        fa = [alloc(1024, BF16) for _ in range(2)]
        fb_ = [alloc(1024, BF16) for _ in range(2)]
        fo = [alloc(2048) for _ in range(2)]
        B_fx, B_fa, B_fb, B_fo = ([Buf(f"{n}{k}") for k in range(2)] for n in ("fx", "fa", "fb", "fo"))
        fss = alloc(2)
        B_fs = [Buf("fs0"), Buf("fs1")]
        B_out = Buf("out")
        for i in range(NT):
            p_ = i % 2
            ts = slice(i * 128, (i + 1) * 128)
            sc.dma("sp", lambda e, ts=ts, p_=p_: e.dma_start(out=fx[p_], in_=x2_dram[ts, :]), f"d_fx{p_}",
                   reads=[B_x2d], writes=[B_fx[p_]])
            sc.dma("pool", lambda e, i=i, p_=p_: e.indirect_dma_start(
                out=fa[p_], out_offset=None, in_=ye_dram, in_offset=bass.IndirectOffsetOnAxis(ap=rix[:, i, 0:1], axis=0)),
                f"d_fa{p_}", reads=[B_yed, B_r], writes=[B_fa[p_]])
            sc.dma("pool", lambda e, i=i, p_=p_: e.indirect_dma_start(
                out=fb_[p_], out_offset=None, in_=ye_dram, in_offset=bass.IndirectOffsetOnAxis(ap=rix[:, i, 1:2], axis=0)),
                f"d_fb{p_}", reads=[B_yed, B_r], writes=[B_fb[p_]])
            sc.op("dve", lambda e, i=i, p_=p_: e.scalar_tensor_tensor(out=fx[p_], in0=fa[p_], scalar=gates[:, i, 0:1], in1=fx[p_],
                                                                      op0=ALU.mult, op1=ALU.add),
                  reads=[B_fa[p_], B_fx[p_], B_r], writes=[B_fx[p_]])
            sc.op("dve", lambda e, i=i, p_=p_: e.scalar_tensor_tensor(out=fx[p_], in0=fb_[p_], scalar=gates[:, i, 1:2], in1=fx[p_],
                                                                      op0=ALU.mult, op1=ALU.add),
                  reads=[B_fb[p_], B_fx[p_], B_r], writes=[B_fx[p_]])
            sc.op("act", lambda e, p_=p_: e.activation(out=fo[p_], in_=fx[p_], func=AF.Square, accum_out=fss[:, p_:p_ + 1]),
                  reads=[B_fx[p_]], writes=[B_fo[p_], B_fs[p_]])
            sc.op("act", lambda e, p_=p_: e.activation(out=fss[:, p_:p_ + 1], in_=fss[:, p_:p_ + 1], func=AF.Sqrt, scale=1.0 / D,
                                                       bias=epsc[:, 0:1]),
                  reads=[B_fs[p_], B_const], writes=[B_fs[p_]])
            sc.op("dve", lambda e, p_=p_: e.reciprocal(out=fss[:, p_:p_ + 1], in_=fss[:, p_:p_ + 1]), reads=[B_fs[p_]], writes=[B_fs[p_]])
            sc.op("dve", lambda e, p_=p_: e.scalar_tensor_tensor(out=fo[p_], in0=fx[p_], scalar=fss[:, p_:p_ + 1], in1=gfb,
                                                                 op0=ALU.mult, op1=ALU.mult),
                  reads=[B_fx[p_], B_fs[p_], B_gb], writes=[B_fo[p_]])
            sc.dma("sp", lambda e, ts=ts, p_=p_: e.dma_start(out=out[ts, :], in_=fo[p_]), f"d_fo{p_}",
                   reads=[B_fo[p_]], writes=[B_out])
        fin = [B_out]

    sc.wait_all("sp", fin)

    names = sc.sem_names()
    sem_cms = [nc.semaphore(n) for n in names]
    for n, cm in zip(names, sem_cms):
        sc.sems[n] = cm.__enter__()
    with nc.Block() as block:
        sc.emit(block)
    for cm in reversed(sem_cms):
        cm.__exit__(None, None, None)
    for cm in reversed(ctxs):
        cm.__exit__(None, None, None)
    print("instr counts:", sc.cnt, "dma:", sc.ndma, "sems:", len(names), "arena top:", top[0])
    return nc


_IN_NAMES = ("norm1_gain", "w_in", "hg_norm_gain", "w_branch_a", "w_branch_b", "w_out", "norm2_gain",
             "w_router_group", "b_router_group", "w_router_expert", "b_router_expert",
             "w_exp_gate", "w_exp_up", "w_exp_down")


def make_in_map(inputs, c):
    m = {"x": np.ascontiguousarray(inputs["x"][c])}
    for k in _IN_NAMES:
        m[k] = np.ascontiguousarray(np.asarray(inputs[k])[0])
    m["hg_lb_logits"] = np.ascontiguousarray(inputs["hg_lb_logits"])
    m["rel_bias"] = np.ascontiguousarray(inputs["rel_bias"])
    m["final_norm_gain"] = np.ascontiguousarray(inputs["final_norm_gain"])
    m["att_oh"] = _att_onehot()
    return m


def kernel(**inputs):
    n = 8
    nc = build_nc()
    inputs = {k: np.asarray(v) for k, v in inputs.items()}
    in_maps = [make_in_map(inputs, c) for c in range(n)]
    res = run_bass_kernel_spmd(nc, in_maps, core_ids=list(range(n)))
    return np.stack([np.asarray(r["out"]) for r in res.results], axis=0).astype(np.float32)
```
